# Optimizing a Trainium2 kernel written in Bass

```python
import jax, jax.numpy as jnp
from jax import lax
import numpy as np

D_MODEL = 2048
BATCH = 4
SEQ = 4096
DEPTH = 2

CTX_LEN = 256
GRID_W = 64
ROPE_BASE = 10000.0
Q_BLOCK = 128
TOKEN_BLOCK = 128
NORM_EPS = 1e-5
RMS_EPS = 1e-6

MLA_HEADS = 8
MLA_Q_LORA = 512
MLA_KV_LORA = 256
MLA_NOPE = 64
MLA_ROPE = 32
MLA_V = 64
MLA_SCALE = (MLA_NOPE + MLA_ROPE) ** -0.5

RET_HEADS = 8
RET_DK = 32
RET_DV = 64
RET_CHUNK = 128
RET_K_SCALE = RET_DK ** -0.5

GQA_HEADS = 8
GQA_KV_HEADS = 2
GQA_GROUP = GQA_HEADS // GQA_KV_HEADS
GQA_DH = 128
GQA_SCALE = GQA_DH ** -0.5

PEER_HEADS = 8
PEER_DQ = 128
PEER_N_KEYS = 128
PEER_TOPK = 16
PEER_N_EXPERTS = PEER_N_KEYS * PEER_N_KEYS

IN_SPLITS = (MLA_Q_LORA, MLA_KV_LORA, MLA_ROPE,
             RET_HEADS * RET_DK, RET_HEADS * RET_DK, RET_HEADS * RET_DV, RET_HEADS * RET_DV,
             GQA_HEADS * GQA_DH, GQA_KV_HEADS * GQA_DH, GQA_KV_HEADS * GQA_DH,
             D_MODEL, D_MODEL, D_MODEL)
N_IN = sum(IN_SPLITS)

DEEPNORM_ALPHA = (2.0 * DEPTH) ** 0.25
DEEPNORM_BETA = (8.0 * DEPTH) ** -0.25

kernel_name = 'hybrid_mla_retention_gqa_peer_dit_block'


def split_in(p):
    idx = np.cumsum(IN_SPLITS)[:-1].tolist()
    return jnp.split(p, idx, axis=-1)


def layer_norm(x, g, b):
    xf = x.astype(jnp.float32)
    mu = xf.mean(-1, keepdims=True)
    var = jnp.square(xf - mu).mean(-1, keepdims=True)
    return ((xf - mu) * lax.rsqrt(var + NORM_EPS) * g + b).astype(x.dtype)


def rms_norm(x, g):
    xf = x.astype(jnp.float32)
    return (xf * lax.rsqrt(jnp.square(xf).mean(-1, keepdims=True) + RMS_EPS) * g).astype(x.dtype)


def axial_rope(x, row, col):
    r = x.shape[-1]
    ra = r // 2
    nf = ra // 2
    inv_freq = ROPE_BASE ** (-jnp.arange(nf, dtype=jnp.float32) / nf)

    def rotate(xa, pos):
        ang = pos.astype(jnp.float32)[:, None] * inv_freq[None, :]
        cos = jnp.cos(ang)[None, :, None, :]
        sin = jnp.sin(ang)[None, :, None, :]
        x1 = xa[..., :nf].astype(jnp.float32)
        x2 = xa[..., nf:].astype(jnp.float32)
        return jnp.concatenate([x1 * cos - x2 * sin, x1 * sin + x2 * cos], axis=-1)

    out = jnp.concatenate([rotate(x[..., :ra], row), rotate(x[..., ra:], col)], axis=-1)
    return out.astype(x.dtype)


def block_attention(q, k, v, scale):
    B, S, HK, G, DQ = q.shape
    nb = S // Q_BLOCK
    qb = jnp.moveaxis(q.reshape(B, nb, Q_BLOCK, HK, G, DQ), 1, 0)

    def one_block(qx):
        s = jnp.einsum('bqhgd,bthd->bhgqt', qx, k).astype(jnp.float32) * scale
        pr = jax.nn.softmax(s, axis=-1).astype(v.dtype)
        return jnp.einsum('bhgqt,bthe->bqhge', pr, v)

    o = lax.map(one_block, qb)
    return jnp.moveaxis(o, 0, 1).reshape(B, S, HK * G * v.shape[-1])


def mla_q(cq, p, pos):
    B, T, _ = cq.shape
    q = (rms_norm(cq, p['mla_q_norm']) @ p['mla_w_qup']).reshape(B, T, MLA_HEADS, MLA_NOPE + MLA_ROPE)
    if pos is not None:
        q = jnp.concatenate([q[..., :MLA_NOPE], axial_rope(q[..., MLA_NOPE:], *pos)], axis=-1)
    return q[:, :, :, None, :]


def mla_kv(ckv, kr, p, pos):
    B, T, _ = ckv.shape
    kv = (rms_norm(ckv, p['mla_kv_norm']) @ p['mla_w_kvup']).reshape(B, T, MLA_HEADS, MLA_NOPE + MLA_V)
    k_rope = kr[:, :, None, :]
    if pos is not None:
        k_rope = axial_rope(k_rope, *pos)
    k = jnp.concatenate([kv[..., :MLA_NOPE], jnp.broadcast_to(k_rope, (B, T, MLA_HEADS, MLA_ROPE))], axis=-1)
    return k, kv[..., MLA_NOPE:]


def ret_heads(a, d, pos):
    B, T, _ = a.shape
    a = a.reshape(B, T, RET_HEADS, d)
    if pos is not None:
        a = axial_rope(a, *pos)
    return jnp.moveaxis(a, 1, 2).astype(jnp.float32)


def retention_scan(q, k, v, log_g, r0):
    B, H, T, _ = q.shape
    dv = v.shape[-1]
    n = T // RET_CHUNK
    chunk = lambda a: jnp.moveaxis(a.reshape(B, H, n, RET_CHUNK, a.shape[-1]), 2, 0)
    i = jnp.arange(RET_CHUNK, dtype=jnp.float32)
    diff = i[:, None] - i[None, :]
    dmask = jnp.where(diff >= 0, jnp.exp(log_g[:, None, None] * jnp.maximum(diff, 0.0)), 0.0)[None]
    xi = jnp.exp(log_g[:, None] * (i + 1.0))[None, :, :, None]
    zeta = jnp.exp(log_g[:, None] * (RET_CHUNK - 1.0 - i))[None, :, :, None]
    g_c = jnp.exp(log_g * RET_CHUNK)[None, :, None, None]

    def step(r, blk):
        qb, kb, vb = blk
        s = jnp.einsum('bhqd,bhkd->bhqk', qb, kb) * dmask
        y = jnp.einsum('bhqk,bhke->bhqe', s, vb) + jnp.einsum('bhqd,bhde->bhqe', qb * xi, r)
        r = g_c * r + jnp.einsum('bhkd,bhke->bhde', kb * zeta, vb)
        return r, y

    _, ys = lax.scan(step, r0, (chunk(q), chunk(k), chunk(v)))
    return jnp.moveaxis(ys, 0, 2).reshape(B, H, T, dv)


def context_state(k, v, log_g, reverse):
    L = k.shape[2]
    m = jnp.arange(L, dtype=jnp.float32)
    expo = m if reverse else (L - 1.0) - m
    w = jnp.exp(log_g[:, None] * expo[None, :])
    return jnp.einsum('bhld,bhle->bhde', k * w[None, :, :, None], v)


def retention_bidir(q, k, v, log_f, log_b, r_f, r_b):
    flip = lambda a: a[:, :, ::-1]
    y_f = retention_scan(q, k, v, log_f, r_f)
    y_b = flip(retention_scan(flip(q), flip(k), flip(v), log_b, r_b))
    return y_f + y_b


def ret_output(y, gate):
    B, H, T, dv = y.shape
    mu = y.mean(-1, keepdims=True)
    var = jnp.square(y - mu).mean(-1, keepdims=True)
    yn = jnp.moveaxis((y - mu) * lax.rsqrt(var + NORM_EPS), 1, 2).reshape(B, T, H * dv)
    return (jax.nn.silu(gate.astype(jnp.float32)) * yn).astype(gate.dtype)


def gqa_q(q, p, pos):
    B, T, _ = q.shape
    q = rms_norm(q.reshape(B, T, GQA_HEADS, GQA_DH), p['gqa_q_norm'])
    if pos is not None:
        q = axial_rope(q, *pos)
    return q.reshape(B, T, GQA_KV_HEADS, GQA_GROUP, GQA_DH)


def gqa_kv(k, v, p, pos):
    B, T, _ = k.shape
    k = rms_norm(k.reshape(B, T, GQA_KV_HEADS, GQA_DH), p['gqa_k_norm'])
    if pos is not None:
        k = axial_rope(k, *pos)
    return k, v.reshape(B, T, GQA_KV_HEADS, GQA_DH)


def merge_branches(o_a, o_b, o_c, g_a, g_b, g_c, p):
    m = (jax.nn.sigmoid(g_a) * (o_a @ p['w_br_a'])
         + jax.nn.sigmoid(g_b) * (o_b @ p['w_br_b'])
         + jax.nn.sigmoid(g_c) * (o_c @ p['w_br_c']))
    return m @ p['w_out']


def token_mixer(h, hc, p, pos, with_ctx_out):
    pl = split_in(h @ p['w_in'])
    pc = split_in(hc @ p['w_in'])
    cat = lambda a, b: jnp.concatenate([a, b], axis=1)
    ka_c, va_c = mla_kv(pc[1], pc[2], p, None)
    ka, va = mla_kv(pl[1], pl[2], p, pos)
    o_a = block_attention(mla_q(pl[0], p, pos), cat(ka_c, ka), cat(va_c, va), MLA_SCALE)
    log_f = jax.nn.log_sigmoid(p['ret_decay_logit'][0].astype(jnp.float32))
    log_b = jax.nn.log_sigmoid(p['ret_decay_logit'][1].astype(jnp.float32))
    kb_c = ret_heads(pc[4], RET_DK, None) * RET_K_SCALE
    vb_c = ret_heads(pc[5], RET_DV, None)
    r_f = context_state(kb_c, vb_c, log_f, False)
    r_b = context_state(kb_c, vb_c, log_b, True)
    y_b = retention_bidir(ret_heads(pl[3], RET_DK, pos), ret_heads(pl[4], RET_DK, pos) * RET_K_SCALE,
                          ret_heads(pl[5], RET_DV, None), log_f, log_b, r_f, r_b)
    o_b = ret_output(y_b, pl[6])
    kc_c, vc_c = gqa_kv(pc[8], pc[9], p, None)
    kc, vc = gqa_kv(pl[8], pl[9], p, pos)
    o_c = block_attention(gqa_q(pl[7], p, pos), cat(kc_c, kc), cat(vc_c, vc), GQA_SCALE)
    out = merge_branches(o_a, o_b, o_c, pl[10], pl[11], pl[12], p)
    if not with_ctx_out:
        return out, None
    oa_c = block_attention(mla_q(pc[0], p, None), ka_c, va_c, MLA_SCALE)
    zeros = jnp.zeros_like(r_f)
    ob_c = ret_output(retention_bidir(ret_heads(pc[3], RET_DK, None), kb_c, vb_c,
                                      log_f, log_b, zeros, zeros), pc[6])
    oc_c = block_attention(gqa_q(pc[7], p, None), kc_c, vc_c, GQA_SCALE)
    return out, merge_branches(oa_c, ob_c, oc_c, pc[10], pc[11], pc[12], p)


def peer_ffn(h, p):
    B, T, D = h.shape
    q = (h @ p['peer_w_q']).reshape(B, T, PEER_HEADS, PEER_DQ)
    half = PEER_DQ // 2
    s1 = jnp.einsum('bthd,kd->bthk', q[..., :half], p['peer_k1']).astype(jnp.float32)
    s2 = jnp.einsum('bthd,kd->bthk', q[..., half:], p['peer_k2']).astype(jnp.float32)
    v1, i1 = lax.top_k(s1, PEER_TOPK)
    v2, i2 = lax.top_k(s2, PEER_TOPK)
    cand_s = (v1[..., :, None] + v2[..., None, :]).reshape(B, T, PEER_HEADS, PEER_TOPK * PEER_TOPK)
    cand_e = (i1[..., :, None] * PEER_N_KEYS + i2[..., None, :]).reshape(B, T, PEER_HEADS, PEER_TOPK * PEER_TOPK)
    top_s, top_j = lax.top_k(cand_s, PEER_TOPK)
    experts = jnp.take_along_axis(cand_e, top_j, axis=-1)
    gates = jax.nn.softmax(top_s, axis=-1)
    nb = (B * T) // TOKEN_BLOCK
    kk = PEER_HEADS * PEER_TOPK
    hb = h.reshape(nb, TOKEN_BLOCK, D)
    eb = experts.reshape(nb, TOKEN_BLOCK, kk)
    gb = gates.reshape(nb, TOKEN_BLOCK, kk).astype(h.dtype)
    u_tab, v_tab = p['peer_u'], p['peer_v']

    def one_block(args):
        hx, ex, gx = args
        u = jnp.take(u_tab, ex, axis=0)
        a = jax.nn.gelu(jnp.einsum('nd,nkd->nk', hx, u), approximate=False) * gx
        return jnp.einsum('nk,nkd->nd', a, jnp.take(v_tab, ex, axis=0))

    return lax.map(one_block, (hb, eb, gb)).reshape(B, T, D)


def setup_inputs(seed: int = 0) -> dict:
    key = jax.random.key(seed)
    ks = jax.random.split(key, 32)
    L, D = DEPTH, D_MODEL
    nrm = lambda k, shape, s: jax.random.normal(k, shape, jnp.float32) * s
    gain = lambda k, n: 1.0 + nrm(k, (L, n), 0.01)
    decay_base = jnp.log(2.0 ** (5.0 + jnp.arange(RET_HEADS, dtype=jnp.float32)) - 1.0)
    return {
        'x': nrm(ks[0], (BATCH, SEQ, D), 1.0),
        'c': nrm(ks[1], (BATCH, D), 1.0),
        'ctx': nrm(ks[2], (BATCH, CTX_LEN, D), 1.0),
        'c_ctx': nrm(ks[3], (D,), 1.0),
        'w_mod': nrm(ks[4], (L, D, 6 * D), 0.5 * D ** -0.5),
        'b_mod': nrm(ks[5], (L, 6 * D), 0.01),
        'w_in': nrm(ks[6], (L, D, N_IN), D ** -0.5),
        'mla_q_norm': gain(ks[7], MLA_Q_LORA),
        'mla_w_qup': nrm(ks[8], (L, MLA_Q_LORA, MLA_HEADS * (MLA_NOPE + MLA_ROPE)), MLA_Q_LORA ** -0.5),
        'mla_kv_norm': gain(ks[9], MLA_KV_LORA),
        'mla_w_kvup': nrm(ks[10], (L, MLA_KV_LORA, MLA_HEADS * (MLA_NOPE + MLA_V)), MLA_KV_LORA ** -0.5),
        'ret_decay_logit': decay_base[None, None, :] + nrm(ks[11], (L, 2, RET_HEADS), 0.01),
        'gqa_q_norm': gain(ks[12], GQA_DH),
        'gqa_k_norm': gain(ks[13], GQA_DH),
        'w_br_a': nrm(ks[14], (L, MLA_HEADS * MLA_V, D), (MLA_HEADS * MLA_V) ** -0.5),
        'w_br_b': nrm(ks[15], (L, RET_HEADS * RET_DV, D), (RET_HEADS * RET_DV) ** -0.5),
        'w_br_c': nrm(ks[16], (L, GQA_HEADS * GQA_DH, D), (GQA_HEADS * GQA_DH) ** -0.5),
        'w_out': nrm(ks[17], (L, D, D), DEEPNORM_BETA * D ** -0.5),
        'ln1_g': gain(ks[18], D),
        'ln1_b': nrm(ks[19], (L, D), 0.01),
        'peer_w_q': nrm(ks[20], (L, D, PEER_HEADS * PEER_DQ), D ** -0.5),
        'peer_k1': nrm(ks[21], (L, PEER_N_KEYS, PEER_DQ // 2), (PEER_DQ // 2) ** -0.5),
        'peer_k2': nrm(ks[22], (L, PEER_N_KEYS, PEER_DQ // 2), (PEER_DQ // 2) ** -0.5),
        'peer_u': nrm(ks[23], (L, PEER_N_EXPERTS, D), D ** -0.5),
        'peer_v': nrm(ks[24], (L, PEER_N_EXPERTS, D), DEEPNORM_BETA),
        'ln2_g': gain(ks[25], D),
        'ln2_b': nrm(ks[26], (L, D), 0.01),
    }


def reference(x, c, ctx, c_ctx, w_mod, b_mod, w_in, mla_q_norm, mla_w_qup, mla_kv_norm, mla_w_kvup,
              ret_decay_logit, gqa_q_norm, gqa_k_norm, w_br_a, w_br_b, w_br_c, w_out, ln1_g, ln1_b,
              peer_w_q, peer_k1, peer_k2, peer_u, peer_v, ln2_g, ln2_b):
    S = x.shape[1]
    ROWS = S // GRID_W
    row = jnp.repeat(jnp.arange(ROWS, dtype=jnp.int32), GRID_W)
    col = jnp.tile(jnp.arange(GRID_W, dtype=jnp.int32), ROWS)
    pos = (row, col)
    silu_c = jax.nn.silu(c)
    silu_cc = jax.nn.silu(c_ctx)
    xc = ctx
    for l in range(DEPTH):
        last = l == DEPTH - 1
        p = {
            'w_in': w_in[l], 'mla_q_norm': mla_q_norm[l], 'mla_w_qup': mla_w_qup[l],
            'mla_kv_norm': mla_kv_norm[l], 'mla_w_kvup': mla_w_kvup[l],
            'ret_decay_logit': ret_decay_logit[l], 'gqa_q_norm': gqa_q_norm[l], 'gqa_k_norm': gqa_k_norm[l],
            'w_br_a': w_br_a[l], 'w_br_b': w_br_b[l], 'w_br_c': w_br_c[l], 'w_out': w_out[l],
            'peer_w_q': peer_w_q[l], 'peer_k1': peer_k1[l], 'peer_k2': peer_k2[l],
            'peer_u': peer_u[l], 'peer_v': peer_v[l],
        }
        mod = silu_c @ w_mod[l] + b_mod[l]
        mod_c = silu_cc @ w_mod[l] + b_mod[l]
        sh1, sc1, g1, sh2, sc2, g2 = jnp.split(mod[:, None, :], 6, axis=-1)
        sh1c, sc1c, g1c, sh2c, sc2c, g2c = jnp.split(mod_c, 6, axis=-1)
        h = x * (1.0 + sc1) + sh1
        hc = xc * (1.0 + sc1c) + sh1c
        o, oc = token_mixer(h, hc, p, pos, not last)
        x = layer_norm(DEEPNORM_ALPHA * x + g1 * o, ln1_g[l], ln1_b[l])
        x = layer_norm(DEEPNORM_ALPHA * x + g2 * peer_ffn(x * (1.0 + sc2) + sh2, p), ln2_g[l], ln2_b[l])
        if not last:
            xc = layer_norm(DEEPNORM_ALPHA * xc + g1c * oc, ln1_g[l], ln1_b[l])
            xc = layer_norm(DEEPNORM_ALPHA * xc + g2c * peer_ffn(xc * (1.0 + sc2c) + sh2c, p), ln2_g[l], ln2_b[l])
    return x
```

```python
import os
import numpy as np
import concourse.bass as bass
import concourse.mybir as mybir
from concourse.bass_utils import run_bass_kernel_spmd

F32 = mybir.dt.float32
BF16 = mybir.dt.bfloat16
ALU = mybir.AluOpType
AF = mybir.ActivationFunctionType

D = 2048
SEQ = 4096
CTX = 256
T = SEQ + CTX
DEPTH = 2
GRID_W = 64
NKC = 16
N_IN = 10016
ALPHA = (2.0 * DEPTH) ** 0.25
MLA_SCALE = 96 ** -0.5
RET_K_SCALE = 32 ** -0.5
GQA_SCALE = 128 ** -0.5
NORM_EPS = 1e-5
RMS_EPS = 1e-6
GROUPS = [(0, 256)] + [(256 + 512 * i, 512) for i in range(8)]
NCH = T // 128

ENGINES = ("pe", "dve", "act", "pool", "sp")
EPOCH = int(os.environ.get("KEPOCH", "16000"))
SAME_ENGINE_SYNC = True
NCHAN = int(os.environ.get('KNCHAN', '1000000007'))
ARENA_WORDS = 46 * 1024


class Tk:
    _n = 0

    def __init__(self, ap, name):
        self.t = ap
        self.name = name
        Tk._n += 1
        self.id = Tk._n

    def __getitem__(self, idx):
        return self.t[idx]


class Chan:
    def __init__(self, name):
        self.name = name
        self.count = 0
        self.sem = None
        self.id = name


class Op:
    __slots__ = ("eng", "fn", "deps", "idx", "signal", "seq", "is_dma", "chan", "chan_count", "line")


class Prog:
    def __init__(self, nc):
        self.nc = nc
        self.ops = []
        self.state = {}
        self._ctx = []
        self.chans = {}
        self.last_eng = {}
        self.last_chan = {}
        self.names = {}
        self.pending = {}
        cm = nc.sbuf_tensor("arena", [128, ARENA_WORDS], F32)
        self.arena = cm.__enter__()
        self._ctx.append(cm)
        self.arena_off = 0
        self.arena_base = 0

    def sbuf(self, name, shape, dt, parts=128):
        n = int(np.prod(shape[1:]))
        words = n if dt == F32 else (n + 1) // 2
        off = self.arena_off
        assert off + words <= ARENA_WORDS, f"arena overflow at {name}: {off}+{words}"
        self.arena_off += words
        v = self.arena[0:shape[0], off:off + words]
        if dt != F32:
            v = v.bitcast(dt)[:, 0:n]
        if len(shape) == 3:
            v = v.rearrange("p (a b) -> p a b", a=shape[1])
        elif len(shape) == 4:
            v = v.rearrange("p (a b c) -> p a b c", a=shape[1], b=shape[2])
        elif len(shape) == 5:
            v = v.rearrange("p (a b c d) -> p a b c d", a=shape[1], b=shape[2], c=shape[3])
        return Tk(v, name)

    def persist(self):
        self.arena_base = self.arena_off

    def reset(self):
        self.barrier()
        self.arena_off = self.arena_base

    def reset_to(self, base):
        self.barrier()
        if os.environ.get("KNOALIAS"):
            return
        self.arena_off = self.arena_base = base

    def psum(self, name, shape, dt):
        cm = self.nc.psum_tensor(name, list(shape), dt)
        h = cm.__enter__()
        self._ctx.append(cm)
        return Tk(h, name)

    def dram(self, name, shape, dt, kind="Internal"):
        if kind == "Internal" and name in getattr(self, "dump", ()):
            kind = "ExternalOutput"
        h = self.nc.dram_tensor(name, list(shape), dt, kind=kind)
        return Tk(h.ap(), name)

    @staticmethod
    def _tok(x):
        if isinstance(x, tuple):
            return x[0], x[1]
        return x, None

    def _conf(self, st, key):
        if key is None:
            return list(st.keys())
        ks = []
        if key in st:
            ks.append(key)
        if None in st:
            ks.append(None)
        return ks

    def op(self, eng, fn, R=(), W=(), dma=False, extra=()):
        o = Op()
        o.eng, o.fn, o.idx, o.signal, o.seq, o.is_dma = eng, fn, len(self.ops), False, None, dma
        o.chan, o.chan_count = None, 0
        import sys as _s
        f = _s._getframe(1)
        while f.f_code.co_name in ("op", "mm", "tr", "act", "tt", "ts", "stt", "cp", "memset", "dma", "barrier"):
            f = f.f_back
        o.line = f.f_lineno
        deps = set(extra) | self.pending.pop(eng, set())
        for x in R:
            tile, key = self._tok(x)
            st = self.state.setdefault(tile.id, {})
            for k in self._conf(st, key):
                if st[k][0] is not None:
                    deps.add(st[k][0])
        for x in W:
            tile, key = self._tok(x)
            st = self.state.setdefault(tile.id, {})
            for k in self._conf(st, key):
                w, rs = st[k]
                if w is not None:
                    deps.add(w)
                deps.update(rs)
        for x in R:
            tile, key = self._tok(x)
            st = self.state.setdefault(tile.id, {})
            if key not in st:
                st[key] = [st[None][0] if None in st else None, []]
            st[key][1].append(o.idx)
        for x in W:
            tile, key = self._tok(x)
            st = self.state.setdefault(tile.id, {})
            if key is None:
                st.clear()
            st[key] = [o.idx, []]
        deps.discard(o.idx)
        o.deps = deps
        self.ops.append(o)
        self.last_eng[eng] = o.idx
        return o

    def dma(self, eng, out_ap, in_ap, R, W, chan):
        import zlib as _z
        chan = f"q{_z.crc32(chan.encode()) % NCHAN}_{eng}"
        if chan not in self.chans:
            self.chans[chan] = Chan(chan)
        c = self.chans[chan]
        o = self.op(eng, lambda e: e.dma_start(out=out_ap, in_=in_ap), R, W, dma=True)
        o.chan = c
        c.count += 1
        o.chan_count = c.count
        self.last_chan[chan] = o.idx
        return o

    def barrier(self, final=False):
        deps = set(self.last_eng.values()) | set(self.last_chan.values())
        for pd in self.pending.values():
            deps |= pd
        if final:
            for e in ENGINES:
                self.op(e, lambda en: en.nop(nofuse=True), extra=deps)
        else:
            self.pending = {e: set(deps) for e in ENGINES}

    def mm(self, out, lhsT, rhs, start, stop, R, W):
        return self.op("pe", lambda e: e.matmul(out, lhsT=lhsT, rhs=rhs, start=start, stop=stop), R, W)

    def tr(self, out, in_, ident, R, W):
        return self.op("pe", lambda e: e.transpose(out, in_, ident), R, W)

    def act(self, out, in_, func, R, W, bias=None, scale=None, accum=None):
        kw = {}
        if bias is not None:
            kw["bias"] = bias
        if scale is not None:
            kw["scale"] = scale
        if accum is not None:
            kw["accum_out"] = accum
        return self.op("act", lambda e: e.activation(out=out, in_=in_, func=func, **kw), R, W)

    def tt(self, eng, out, in0, in1, op, R, W):
        return self.op(eng, lambda e: e.tensor_tensor(out=out, in0=in0, in1=in1, op=op), R, W)

    def ts(self, eng, out, in0, s1, s2, op0, op1, R, W):
        if op1 is None:
            return self.op(eng, lambda e: e.tensor_scalar(out=out, in0=in0, scalar1=s1, scalar2=None, op0=op0), R, W)
        return self.op(eng, lambda e: e.tensor_scalar(out=out, in0=in0, scalar1=s1, scalar2=s2, op0=op0, op1=op1), R, W)

    def stt(self, out, in0, scalar, in1, op0, op1, R, W):
        return self.op("dve", lambda e: e.scalar_tensor_tensor(out=out, in0=in0, scalar=scalar, in1=in1, op0=op0, op1=op1), R, W)

    def cp(self, eng, out, in_, R, W):
        if eng == "act":
            return self.op("act", lambda e: e.copy(out=out, in_=in_), R, W)
        return self.op(eng, lambda e: e.tensor_copy(out=out, in_=in_), R, W)

    def recip(self, out, in_, R, W):
        return self.op("dve", lambda e: e.reciprocal(out=out, in_=in_), R, W)

    def max8(self, out, in_, R, W):
        return self.op("dve", lambda e: e.max(out=out, in_=in_), R, W)

    def mrep(self, out, rep, vals, R, W):
        return self.op("dve", lambda e: e.match_replace(out=out, in_to_replace=rep, in_values=vals, imm_value=-1e30), R, W)

    def rsum(self, out, in_, R, W):
        return self.op("dve", lambda e: e.reduce_sum(out=out, in_=in_, axis=mybir.AxisListType.X), R, W)

    def memset(self, eng, ap, val, W):
        return self.op(eng, lambda e: e.memset(ap, val), (), W)

    def emit(self):
        nc, ops = self.nc, self.ops

        def skip(y, o):
            return y.eng == o.eng and (y.eng == "pe" or not SAME_ENGINE_SYNC)

        for o in ops:
            for d in o.deps:
                y = ops[d]
                if y.is_dma or skip(y, o):
                    continue
                y.signal = True
        cnt = {e: 0 for e in ENGINES}
        for o in ops:
            if o.signal and not o.is_dma:
                cnt[o.eng] += 1
                o.seq = cnt[o.eng]
        sems, sem_ctx, nsem = {}, [], 0
        for e in ENGINES:
            sems[e] = []
            for k in range(max(1, (cnt[e] + EPOCH - 1) // EPOCH)):
                cm = nc.semaphore(f"s_{e}_{k}")
                sems[e].append(cm.__enter__())
                sem_ctx.append(cm)
                nsem += 1
        for c in self.chans.values():
            cm = nc.semaphore(f"d_{c.name}")
            c.sem = cm.__enter__()
            sem_ctx.append(cm)
            nsem += 1
        for k in range(int(os.environ.get("KXSEM", "0"))):
            cm = nc.semaphore(f"dummy{k}")
            cm.__enter__()
            sem_ctx.append(cm)
            nsem += 1
        self.nsem = nsem
        assert nsem <= 140, f"too many semaphores: {nsem}"
        known = {e: {} for e in ENGINES}
        run = {}
        waits = []
        for o in ops:
            w = {}
            for d in o.deps:
                y = ops[d]
                if y.is_dma:
                    key = ("d", y.chan.id)
                    val = 16 * run[y.chan.id]
                    semh = y.chan.sem
                else:
                    if skip(y, o):
                        continue
                    ep = (y.seq - 1) // EPOCH
                    key = ("e", y.eng, ep)
                    val = (y.seq - 1) % EPOCH + 1
                    semh = sems[y.eng][ep]
                if known[o.eng].get(key, 0) >= val:
                    continue
                if key not in w or w[key][1] < val:
                    w[key] = (semh, val)
            for key, (semh, val) in w.items():
                known[o.eng][key] = val
            waits.append(list(w.values()))
            if o.is_dma:
                run[o.chan.id] = o.chan_count
        self.waits_dbg = waits
        engmap = {"pe": "tensor", "dve": "vector", "act": "scalar", "pool": "gpsimd", "sp": "sync"}
        by_eng = {e: [o for o in ops if o.eng == e] for e in ENGINES}
        self.stats = {e: len(by_eng[e]) for e in ENGINES}
        self.stats["waits"] = sum(len(w) for w in waits)
        with nc.Block() as block:
            for e in ENGINES:
                lst = by_eng[e]
                if not lst:
                    continue

                def body(engine, lst=lst, e=e):
                    for o in lst:
                        for semh, val in waits[o.idx]:
                            engine.wait_ge(semh, val)
                        ins = o.fn(engine)
                        if os.environ.get("KDBG"):
                            try:
                                self.names[ins.ins.name] = o.line
                            except Exception:
                                self.names[str(ins)[:40]] = o.line
                        if o.is_dma:
                            ins.then_inc(o.chan.sem, 16)
                        elif o.signal:
                            ins.then_inc(sems[e][(o.seq - 1) // EPOCH], 1)
                getattr(block, engmap[e])(body)
        for cm in reversed(sem_ctx):
            cm.__exit__(None, None, None)
        for cm in reversed(self._ctx):
            cm.__exit__(None, None, None)


OFF = dict(cq=0, ckv=512, kr=768, rq=800, rk=1056, rv=1312, rg=1824, gq=2336, gk=3360, gv=3616,
           ga=3872, gb=5920, gc=7968)
FM_TILES = []
FM_TILES.append(("kr", 0, OFF["kr"], 32))
for i in range(4):
    FM_TILES.append(("cq", i, OFF["cq"] + 128 * i, 128))
for i in range(2):
    FM_TILES.append(("ckv", i, OFF["ckv"] + 128 * i, 128))
for i in range(4):
    FM_TILES.append(("rq", i, OFF["rq"] + 64 * i, 64))
for i in range(4):
    FM_TILES.append(("rk", i, OFF["rk"] + 64 * i, 64))
for i in range(8):
    FM_TILES.append(("rg", i, OFF["rg"] + 64 * i, 64))
for i in range(8):
    FM_TILES.append(("gq", i, OFF["gq"] + 128 * i, 128))
for i in range(2):
    FM_TILES.append(("gk", i, OFF["gk"] + 128 * i, 128))
for i in range(48):
    FM_TILES.append(("gate", i, OFF["ga"] + 128 * i, 128))
NFM = len(FM_TILES)


def rope_tables(r, nblk, scale, nrows_pad=None, row0=0):
    ra, nf = r // 2, r // 4
    inv = 10000.0 ** (-np.arange(nf, dtype=np.float64) / nf)
    tl = np.arange(SEQ)
    row, col = tl // GRID_W, tl % GRID_W
    rows = nblk * r
    tot = nrows_pad or rows
    C = np.full((tot, T), scale, np.float64)
    S = np.zeros((tot, T), np.float64)
    P = np.zeros((tot, tot), np.float32)
    for b in range(nblk):
        for d in range(r):
            g = row0 + b * r + d
            half = d // ra
            dd = d % ra
            pos = row if half == 0 else col
            f = inv[dd % nf]
            C[g, CTX:] = scale * np.cos(pos * f)
            if dd < nf:
                S[g, CTX:] = -scale * np.sin(pos * f)
                P[g + nf, g] = 1.0
            else:
                S[g, CTX:] = scale * np.sin(pos * f)
                P[g - nf, g] = 1.0
    return C.astype(np.float32), S.astype(np.float32), P


_CONST = None


def host_consts():
    global _CONST
    if _CONST is not None:
        return _CONST
    c = {}
    Cq, Sq, Pq = rope_tables(32, 1, MLA_SCALE, nrows_pad=96, row0=64)
    Ckr, Skr, Pkr = rope_tables(32, 1, 1.0)
    Crq, Srq, Prr = rope_tables(32, 4, 1.0)
    Crk, Srk, _ = rope_tables(32, 4, RET_K_SCALE)
    Cgq, Sgq, Pg = rope_tables(128, 1, GQA_SCALE)
    Cgk, Sgk, _ = rope_tables(128, 1, 1.0)
    tab = np.zeros((12, 128, T), np.float32)
    for i, a in enumerate([Cq, Sq, Ckr, Skr, Crq, Srq, Crk, Srk, Cgq, Sgq, Cgk, Sgk]):
        tab[i, :a.shape[0]] = a
    c["ropetab"] = tab
    pm = np.zeros((4, 128, 128), np.float32)
    pm[0, :96, :96] = Pq
    pm[1, :32, :32] = Pkr
    pm[2] = Prr
    pm[3] = Pg
    c["permm"] = pm
    misc = np.zeros((128, 8, 128), np.float32)
    misc[:, 0, :] = np.eye(128)
    sh = np.zeros((128, 128), np.float32)
    for k in range(32):
        sh[k, 64 + k] = 1.0
    misc[:, 1, :] = sh
    i = np.arange(128)
    misc[:, 2, :] = np.maximum(i[None, :] - i[:, None], 0)
    misc[:, 3, :] = (i[None, :] >= i[:, None])
    misc[:, 4, :] = np.maximum(i[:, None] - i[None, :], 0)
    misc[:, 5, :] = (i[:, None] >= i[None, :])
    misc[:, 6, :] = (i[None, :] + 1.0)
    misc[:, 7, :] = (128.0 - i[None, :])
    c["misc"] = misc
    col = np.zeros((128, 4), np.float32)
    col[:, 0] = 127.0 - i
    col[:, 1] = i
    c["colc"] = col
    sel = np.zeros((128, 4, 8), np.float32)
    for p in range(64):
        for j in range(4):
            sel[p, j, j * 2 + p // 32] = 1.0
    c["hsel"] = sel
    _CONST = c
    return c


def fm(v):
    v = np.asarray(v, np.float32)
    return np.ascontiguousarray(v.reshape(-1, 128).T)


def prep_layer_weights(inp, l):
    w = {}
    w_in = inp["w_in"][l]
    wf = np.zeros((NFM, 128, NKC, 128), np.float32)
    for ti, (_, _, c0, nc_) in enumerate(FM_TILES):
        wf[ti, :, :, :nc_] = w_in[:, c0:c0 + nc_].reshape(NKC, 128, nc_).transpose(1, 0, 2)
    w["wf"] = wf
    w["wt_rv"] = np.ascontiguousarray(w_in[:, OFF["rv"]:OFF["rv"] + 512].reshape(NKC, 128, 512).transpose(1, 0, 2))
    w["wt_gv"] = np.ascontiguousarray(w_in[:, OFF["gv"]:OFF["gv"] + 256].reshape(NKC, 128, 256).transpose(1, 0, 2))
    w["wmod"] = np.ascontiguousarray(inp["w_mod"][l].reshape(NKC, 128, 96, 128).transpose(2, 1, 0, 3))
    w["bmod"] = fm(inp["b_mod"][l])
    w["wqup"] = np.ascontiguousarray(inp["mla_w_qup"][l].reshape(4, 128, 768).transpose(1, 0, 2))
    kv = inp["mla_w_kvup"][l].reshape(2, 128, 8, 128)
    wk = np.zeros((128, 2, 8, 96), np.float32)
    wk[:, :, :, :64] = kv[:, :, :, :64].transpose(1, 0, 2, 3)
    w["wkpad"] = wk
    w["wv"] = np.ascontiguousarray(kv[:, :, :, 64:].transpose(1, 0, 2, 3).reshape(128, 2, 512))
    nrm = np.zeros((128, 8), np.float32)
    nrm[:, 0:4] = fm(inp["mla_q_norm"][l])
    nrm[:, 4:6] = fm(inp["mla_kv_norm"][l])
    nrm[:, 6:7] = fm(inp["gqa_q_norm"][l])
    nrm[:, 7:8] = fm(inp["gqa_k_norm"][l])
    w["nrm"] = nrm
    w["decay"] = np.ascontiguousarray(inp["ret_decay_logit"][l].reshape(16))
    w["wbra"] = np.ascontiguousarray(inp["w_br_a"][l].reshape(8, 64, 16, 128).transpose(2, 1, 0, 3))
    w["wbrb"] = np.ascontiguousarray(inp["w_br_b"][l].reshape(8, 64, 16, 128).transpose(2, 1, 0, 3))
    w["wbrc"] = np.ascontiguousarray(inp["w_br_c"][l].reshape(8, 128, 16, 128).transpose(2, 1, 0, 3))
    w["wout"] = np.ascontiguousarray(inp["w_out"][l].reshape(16, 128, 16, 128).transpose(2, 1, 0, 3))
    ln = np.zeros((128, 4, 16), np.float32)
    ln[:, 0] = fm(inp["ln1_g"][l]); ln[:, 1] = fm(inp["ln1_b"][l])
    ln[:, 2] = fm(inp["ln2_g"][l]); ln[:, 3] = fm(inp["ln2_b"][l])
    w["ln"] = ln
    w["pwq"] = np.ascontiguousarray(inp["peer_w_q"][l].reshape(16, 128, 8, 128).transpose(2, 1, 0, 3))
    kk = np.zeros((128, 256), np.float32)
    kk[0:64, 0:128] = inp["peer_k1"][l].T
    kk[64:128, 128:256] = inp["peer_k2"][l].T
    w["pk"] = kk
    w["put"] = np.ascontiguousarray(inp["peer_u"][l].reshape(128, 128, 16, 128).transpose(0, 3, 2, 1))
    w["pvt"] = np.ascontiguousarray(inp["peer_v"][l].reshape(16, 8, 128, 16, 128).transpose(0, 3, 2, 1, 4))
    return w


WKEYS = ["wf", "wt_rv", "wt_gv", "wmod", "bmod", "wqup", "wkpad", "wv", "nrm", "decay", "wbra", "wbrb", "wbrc",
         "wout", "ln", "pwq", "pk", "put", "pvt"]
WSHAPES = dict(wf=[NFM, 128, NKC, 128], wt_rv=[128, NKC, 512], wt_gv=[128, NKC, 256], wmod=[96, 128, NKC, 128],
               bmod=[128, 96], wqup=[128, 4, 768], wkpad=[128, 2, 8, 96], wv=[128, 2, 512], nrm=[128, 8],
               decay=[16], wbra=[16, 64, 8, 128], wbrb=[16, 64, 8, 128], wbrc=[16, 128, 8, 128],
               wout=[16, 128, 16, 128], ln=[128, 4, 16], pwq=[8, 128, 16, 128], pk=[128, 256],
               put=[128, 128, 16, 128], pvt=[16, 16, 128, 8, 128])


def build_program(n_layers=DEPTH, debug=None):
    debug = debug or {}
    nc = bass.Bass("TRN2", target_bir_lowering=False)
    p = Prog(nc)
    p.dump = debug.get("dump", ()) if debug else ()
    EI = "ExternalInput"
    xT_in = p.dram("xT", [128, NKC, T], F32, EI)
    cc_in = p.dram("cc", [128, NKC, 2], F32, EI)
    ropetab = p.dram("ropetab", [12, 128, T], F32, EI)
    permm_in = p.dram("permm", [4, 128, 128], F32, EI)
    misc_in = p.dram("misc", [128, 8, 128], F32, EI)
    colc_in = p.dram("colc", [128, 4], F32, EI)
    hsel_in = p.dram("hsel", [128, 4, 8], F32, EI)
    Wd = [{k: p.dram(f"{k}{l}", WSHAPES[k], F32, EI) for k in WKEYS} for l in range(n_layers)]
    out = p.dram("out", [128, NKC, SEQ], F32, "ExternalOutput")

    XS = [p.dram(f"xs{i}", [128, NKC, T], F32) for i in range(2)]
    X1 = p.dram("x1", [128, NKC, T], F32)
    H2 = p.dram("h2", [128, NKC, T], BF16)
    QA = p.dram("qa", [8, 96, T], BF16)
    KA = p.dram("ka", [8, 96, T], BF16)
    VA = p.dram("va", [NCH, 128, 8 * 65], BF16)
    QB = p.dram("qb", [4, 64, T], BF16)
    KB = p.dram("kb", [4, 64, T], BF16)
    VB = p.dram("vb", [NCH, 128, 512], BF16)
    GB = p.dram("gb", [8, 64, T], BF16)
    QC = p.dram("qc", [8, 128, T], BF16)
    KC = p.dram("kc", [2, 128, T], BF16)
    VC = p.dram("vc", [NCH, 128, 256], BF16)
    GT = p.dram("gt", [48, 128, T], BF16)
    OA = p.dram("oa", [8, 64, T], BF16)
    OC = p.dram("oc", [8, 128, T], BF16)
    DR = p.dram("dr", [2, NCH, 32, 512], F32)
    RS = p.dram("rs", [2, NCH, 32, 512], BF16)
    dbg = {}

    ps = [p.psum(f"ps{i}", [128, 512], F32) for i in range(8)]

    def psb(i):
        return ps[i][:, 0:256].bitcast(BF16)

    ident = p.sbuf("ident", [128, 128], BF16)
    identf = p.sbuf("identf", [128, 128], F32)
    shiftm = p.sbuf("shiftm", [128, 128], BF16)
    permm = p.sbuf("permm_sb", [128, 4, 128], BF16)
    ones_bf = p.sbuf("ones_bf", [128, 128], BF16)
    ones_f = p.sbuf("ones_f", [128, 128], F32)
    misc = p.sbuf("misc_sb", [128, 8, 128], F32)
    colc = p.sbuf("colc_sb", [128, 4], F32)
    hsel = p.sbuf("hsel_sb", [128, 4, 8], F32)
    silu_c = p.sbuf("silu_c", [128, NKC, 2], F32)
    epsc = p.sbuf("epsc", [128, 4], F32)
    p.dma("sp", misc[:], misc_in[:], [misc_in], [misc], "c_misc")
    p.dma("sp", colc[:], colc_in[:], [colc_in], [colc], "c_misc")
    p.dma("sp", hsel[:], hsel_in[:], [hsel_in], [hsel], "c_misc")
    p.dma("sp", silu_c[:], cc_in[:], [cc_in], [silu_c], "c_misc")
    p.dma("pool", permm[:], permm_in[:].rearrange("a p c -> p a c"), [permm_in], [permm], "c_perm")
    p.cp("dve", ident[:], misc[:, 0, :], [misc], [ident])
    p.cp("dve", identf[:], misc[:, 0, :], [misc], [identf])
    p.cp("dve", shiftm[:], misc[:, 1, :], [misc], [shiftm])
    p.memset("dve", ones_bf[:], 1.0, [ones_bf])
    p.memset("dve", epsc[:, 0:1], RMS_EPS, [epsc])
    p.memset("dve", epsc[:, 1:2], NORM_EPS, [epsc])
    p.memset("dve", epsc[:, 2:3], 1.0, [epsc])
    p.memset("dve", epsc[:, 3:4], 0.0, [epsc])
    p.memset("dve", ones_f[:], 1.0, [ones_f])
    p.act(silu_c[:], silu_c[:], AF.Silu, [silu_c], [silu_c])
    p.persist()
    base0 = p.arena_off

    for l in range(n_layers):
        W = Wd[l]
        XT = xT_in if l == 0 else XS[(l - 1) % 2]
        XO = XS[l % 2]
        last = l == n_layers - 1
        p.reset_to(base0)
        modv = p.sbuf("modv", [128, 96, 2], F32)
        bmod = p.sbuf("bmod", [128, 96], F32)
        nrm = p.sbuf("nrm", [128, 8], F32)
        lnp = p.sbuf("lnp", [128, 4, 16], F32)
        lg = p.sbuf("lg", [128, 16], F32)
        p.dma("sp", bmod[:], W["bmod"][:], [W["bmod"]], [bmod], "c_misc")
        p.dma("sp", nrm[:], W["nrm"][:], [W["nrm"]], [nrm], "c_misc")
        p.dma("sp", lnp[:], W["ln"][:], [W["ln"]], [lnp], "c_misc")
        p.dma("sp", lg[:], W["decay"][:].partition_broadcast(128), [W["decay"]], [lg], "c_misc")
        p.act(lg[:], lg[:], AF.Exp, [lg], [lg], scale=-1.0)
        p.act(lg[:], lg[:], AF.Ln, [lg, epsc], [lg], bias=epsc[:, 2:3])
        p.ts("dve", lg[:], lg[:], -1.0, None, ALU.mult, None, [lg], [lg])
        layer_base = p.arena_off
        p.arena_base = layer_base
        wmt = [p.sbuf(f"wmt{i}", [128, NKC, 128], F32) for i in range(3)]
        for j in range(96):
            wt = wmt[j % 3]
            p.dma("sp" if j % 2 == 0 else "act", wt[:], W["wmod"][j], [W["wmod"]], [wt], f"wmt{j % 3}")
            bank = ps[j % 2]
            for kc in range(NKC):
                p.mm(bank[:, 0:2], wt[:, kc, :], silu_c[:, kc, :], kc == 0, kc == NKC - 1, [wt, silu_c], [bank])
            p.tt("dve", modv[:, j, :], bank[:, 0:2], bmod[:, j:j + 1].to_broadcast([128, 2]), ALU.add,
                 [bank, bmod], [(modv, j)])
        for s in (1, 4):
            p.ts("dve", modv[:, 16 * s:16 * s + 16, :], modv[:, 16 * s:16 * s + 16, :], 1.0, None, ALU.add, None,
                 [modv], [modv])
        p.reset()

        def mod(kind, c, isctx):
            j = dict(sh1=0, sc1=1, g1=2, sh2=3, sc2=4, g2=5)[kind] * 16 + c
            return modv[:, j, (1 if isctx else 0):(2 if isctx else 1)]

        wq_sb = p.sbuf("wqup_sb", [128, 4, 768], BF16)
        wk_sb = p.sbuf("wkpad_sb", [128, 2, 8, 96], BF16)
        wv_sb = p.sbuf("wv_sb", [128, 2, 512], BF16)
        wrv_sb = p.sbuf("wrv_sb", [128, NKC, 512], BF16)
        wgv_sb = p.sbuf("wgv_sb", [128, NKC, 256], BF16)
        zf = p.sbuf("zf", [128, 8], F32)
        zb = p.sbuf("zb", [128, 8], F32)
        p.dma("pool", wq_sb[:], W["wqup"][:], [W["wqup"]], [wq_sb], "a_w0")
        p.dma("pool", wk_sb[:], W["wkpad"][:], [W["wkpad"]], [wk_sb], "a_w0")
        p.dma("pool", wv_sb[:], W["wv"][:], [W["wv"]], [wv_sb], "a_w0")
        p.dma("pool", wrv_sb[:], W["wt_rv"][:], [W["wt_rv"]], [wrv_sb], "a_w0")
        p.dma("pool", wgv_sb[:], W["wt_gv"][:], [W["wt_gv"]], [wgv_sb], "a_w0")
        p.ts("dve", zf[:], lg[:, 0:8], colc[:, 0:1], None, ALU.mult, None, [lg, colc], [zf])
        p.ts("dve", zb[:], lg[:, 8:16], colc[:, 1:2], None, ALU.mult, None, [lg, colc], [zb])
        p.act(zf[:], zf[:], AF.Exp, [zf], [zf])
        p.act(zb[:], zb[:], AF.Exp, [zb], [zb])
        xg = p.sbuf("xg", [128, NKC, 512], F32)
        hT = p.sbuf("hT", [128, NKC, 512], BF16)
        wfb = [p.sbuf(f"wfb{i}", [128, NKC, 128], BF16) for i in range(3)]
        tabs = [p.sbuf(f"tab{i}", [128, 2, 512], F32) for i in range(2)]
        raw = [p.sbuf(f"raw{i}", [128, 512], F32) for i in range(2)]
        rawb = [p.sbuf(f"rawb{i}", [128, 512], BF16) for i in range(2)]
        sqb = [p.sbuf(f"sqb{i}", [128, 512], BF16) for i in range(2)]
        cqT = p.sbuf("cqT", [128, 4, 512], F32)
        cqn = p.sbuf("cqn", [128, 4, 512], BF16)
        ckvn = p.sbuf("ckvn", [128, 2, 512], BF16)
        krr = p.sbuf("krr", [32, 512], BF16)
        rstd = p.sbuf("rstd", [128, 512], F32)
        t1 = [p.sbuf(f"t1_{i}", [128, 512], F32) for i in range(2)]
        t2 = [p.sbuf(f"t2_{i}", [128, 512], F32) for i in range(2)]
        outb = [p.sbuf(f"outb{i}", [128, 512], BF16) for i in range(3)]
        vaext = [p.sbuf(f"vaext{i}", [128, 8, 65], BF16) for i in range(2)]
        vtm = [p.sbuf(f"vtm{i}", [128, 512], BF16) for i in range(2)]
        kz = [p.sbuf(f"kz{i}", [128, 2, 256], BF16) for i in range(2)]
        kbT = p.sbuf("kbT", [64, 4, 512], BF16)
        drs = [p.sbuf(f"drs{i}", [32, 2, 512], F32) for i in range(2)]
        for i in range(2):
            p.memset("pool", vaext[i][:], 1.0, [vaext[i]])
        cnt = {"o": 0, "r": 0, "t": 0}

        def rope(xT_, xap, nrow, tabidx, permidx, g0, N, bank_r):
            i = cnt["r"] % 2
            cnt["r"] += 1
            tb = tabs[i]
            p.dma("sp", tb[0:nrow, :, 0:N], ropetab[tabidx:tabidx + 2, 0:nrow, g0:g0 + N].rearrange("a p n -> p a n"),
                  [ropetab], [tb], f"tab{i}")
            br = bank_r
            p.mm(br[0:nrow, 0:N], permm[0:nrow, permidx, 0:nrow], xap, True, True, [permm, xT_], [br])
            a, b = t1[i], t2[i]
            p.tt("dve", a[0:nrow, 0:N], xap, tb[0:nrow, 0, 0:N], ALU.mult, [xT_, tb], [a])
            p.tt("dve", b[0:nrow, 0:N], br[0:nrow, 0:N], tb[0:nrow, 1, 0:N], ALU.mult, [br, tb], [b])
            j = cnt["o"] % 3
            cnt["o"] += 1
            ob = outb[j]
            p.tt("pool", ob[0:nrow, 0:N], a[0:nrow, 0:N], b[0:nrow, 0:N], ALU.add, [a, b], [ob])
            return ob, j

        def to_bf(bank, nrow, N):
            i = cnt["t"] % 2
            cnt["t"] += 1
            xb = rawb[i]
            p.cp("act", xb[0:nrow, 0:N], bank[0:nrow, 0:N], [bank], [xb])
            return xb

        def rms_bcast(src_list, nfeat, N, bank):
            for k, (sT, sap) in enumerate(src_list):
                sq = sqb[k % 2]
                p.act(sq[:, 0:N], sap, AF.Square, [sT], [sq])
                p.mm(bank[:, 0:N], ones_bf[:], sq[:, 0:N], k == 0, k == len(src_list) - 1, [ones_bf, sq], [bank])
            p.act(rstd[:, 0:N], bank[:, 0:N], AF.Sqrt, [bank, epsc], [rstd], bias=epsc[:, 0:1], scale=1.0 / nfeat)
            p.recip(rstd[:, 0:N], rstd[:, 0:N], [rstd], [rstd])

        for gi, (g0, N) in enumerate(GROUPS):
            isctx = gi == 0
            nsub = N // 128
            for c in range(NKC):
                p.dma("sp" if c % 2 == 0 else "act", xg[:, c, 0:N], XT[:, c, g0:g0 + N], [XT], [(xg, c)], "xg")
            for c in range(NKC):
                p.ts("dve", hT[:, c, 0:N], xg[:, c, 0:N], mod("sc1", c, isctx), mod("sh1", c, isctx),
                     ALU.mult, ALU.add, [(xg, c), modv], [(hT, c)])
            for ti, (kind, idx, c0, ncol) in enumerate(FM_TILES):
                if last and isctx and kind in ("cq", "rq", "rg", "gq", "gate"):
                    continue
                wt = wfb[ti % 3]
                p.dma("pool", wt[:], W["wf"][ti], [W["wf"]], [wt], f"wfb{ti % 3}")
                bank = ps[ti % 2]
                for kc in range(NKC):
                    p.mm(bank[0:ncol, 0:N], wt[:, kc, 0:ncol], hT[:, kc, 0:N], kc == 0, kc == NKC - 1, [wt, (hT, kc)], [bank])
                if kind == "kr":
                    xb = to_bf(bank, 32, N)
                    ob, j = rope(xb, xb[0:32, 0:N], 32, 2, 1, g0, N, ps[2])
                    p.cp("dve", krr[:, 0:N], ob[0:32, 0:N], [ob], [krr])
                elif kind == "cq":
                    p.cp("act", cqT[:, idx, 0:N], bank[:, 0:N], [bank], [(cqT, idx)])
                    if idx == 3:
                        rms_bcast([((cqT, k), cqT[:, k, 0:N]) for k in range(4)], 512, N, ps[2])
                        for k in range(4):
                            p.stt(cqn[:, k, 0:N], cqT[:, k, 0:N], nrm[:, k:k + 1], rstd[:, 0:N], ALU.mult, ALU.mult,
                                  [(cqT, k), nrm, rstd], [(cqn, k)])
                        for h in range(8):
                            b2 = ps[3 + h % 2]
                            for k in range(4):
                                p.mm(b2[0:96, 0:N], wq_sb[:, k, h * 96:(h + 1) * 96], cqn[:, k, 0:N], k == 0, k == 3,
                                     [wq_sb, (cqn, k)], [b2])
                            xb = to_bf(b2, 96, N)
                            ob, j = rope(xb, xb[0:96, 0:N], 96, 0, 0, g0, N, ps[5 + h % 2])
                            p.dma("sp", QA[h, :, g0:g0 + N], ob[0:96, 0:N], [ob], [QA], f"outb{j}")
                elif kind == "ckv":
                    p.cp("act", cqT[:, idx, 0:N], bank[:, 0:N], [bank], [(cqT, idx)])
                    if idx == 1:
                        rms_bcast([((cqT, k), cqT[:, k, 0:N]) for k in range(2)], 256, N, ps[2])
                        for k in range(2):
                            p.stt(ckvn[:, k, 0:N], cqT[:, k, 0:N], nrm[:, 4 + k:5 + k], rstd[:, 0:N], ALU.mult, ALU.mult,
                                  [(cqT, k), nrm, rstd], [(ckvn, k)])
                        for h in range(8):
                            b2 = ps[3 + h % 2]
                            for k in range(2):
                                p.mm(b2[0:96, 0:N], wk_sb[:, k, h, :], ckvn[:, k, 0:N], k == 0, False, [wk_sb, (ckvn, k)], [b2])
                            p.mm(b2[0:96, 0:N], shiftm[0:32, 0:96], krr[:, 0:N], False, True, [shiftm, krr], [b2])
                            j = cnt["o"] % 3
                            cnt["o"] += 1
                            ob = outb[j]
                            p.cp("act", ob[0:96, 0:N], b2[0:96, 0:N], [b2], [ob])
                            p.dma("sp", KA[h, :, g0:g0 + N], ob[0:96, 0:N], [ob], [KA], f"outb{j}")
                        for s in range(nsub):
                            b2 = ps[5 + s % 2]
                            for k in range(2):
                                p.mm(b2[:, :], ckvn[:, k, s * 128:(s + 1) * 128], wv_sb[:, k, :], k == 0, k == 1,
                                     [(ckvn, k), wv_sb], [b2])
                            ve = vaext[s % 2]
                            p.cp("act", ve[:, :, 0:64], b2[:, :].rearrange("p (h v) -> p h v", h=8), [b2], [ve])
                            p.dma("sp", VA[g0 // 128 + s], ve[:].rearrange("p h v -> p (h v)"), [ve], [VA], f"vaext{s % 2}")
                elif kind == "rq":
                    xb = to_bf(bank, 64, N)
                    ob, j = rope(xb, xb[0:64, 0:N], 64, 4, 2, g0, N, ps[2])
                    p.dma("sp", QB[idx, :, g0:g0 + N], ob[0:64, 0:N], [ob], [QB], f"outb{j}")
                elif kind == "rk":
                    xb = to_bf(bank, 64, N)
                    ob, j = rope(xb, xb[0:64, 0:N], 64, 6, 2, g0, N, ps[2])
                    p.dma("sp", KB[idx, :, g0:g0 + N], ob[0:64, 0:N], [ob], [KB], f"outb{j}")
                    p.cp("pool", kbT[:, idx, 0:N], ob[0:64, 0:N], [ob], [(kbT, idx)])
                elif kind == "rg":
                    j = cnt["o"] % 3
                    cnt["o"] += 1
                    ob = outb[j]
                    p.act(ob[0:64, 0:N], bank[0:64, 0:N], AF.Silu, [bank], [ob])
                    p.dma("sp", GB[idx, :, g0:g0 + N], ob[0:64, 0:N], [ob], [GB], f"outb{j}")
                elif kind in ("gq", "gk"):
                    i = cnt["t"] % 2
                    cnt["t"] += 1
                    rw = raw[i]
                    p.cp("act", rw[:, 0:N], bank[:, 0:N], [bank], [rw])
                    rms_bcast([(rw, rw[:, 0:N])], 128, N, ps[2])
                    gcol = 6 if kind == "gq" else 7
                    qn = rawb[i]
                    p.stt(qn[:, 0:N], rw[:, 0:N], nrm[:, gcol:gcol + 1], rstd[:, 0:N], ALU.mult, ALU.mult, [rw, nrm, rstd], [qn])
                    dstT = QC if kind == "gq" else KC
                    ob, j = rope(qn, qn[:, 0:N], 128, 8 if kind == "gq" else 10, 3, g0, N, ps[3])
                    p.dma("sp", dstT[idx, :, g0:g0 + N], ob[:, 0:N], [ob], [dstT], f"outb{j}")
                elif kind == "gate":
                    j = cnt["o"] % 3
                    cnt["o"] += 1
                    ob = outb[j]
                    p.act(ob[:, 0:N], bank[:, 0:N], AF.Sigmoid, [bank], [ob])
                    p.dma("sp", GT[idx, :, g0:g0 + N], ob[:, 0:N], [ob], [GT], f"outb{j}")
            for s in range(nsub):
                ch = g0 // 128 + s
                b2 = ps[4 + s % 2]
                for kc in range(NKC):
                    p.mm(b2[:, :], hT[:, kc, s * 128:(s + 1) * 128], wrv_sb[:, kc, :], kc == 0, kc == NKC - 1,
                         [(hT, kc), wrv_sb], [b2])
                vt = vtm[s % 2]
                p.cp("act", vt[:], b2[:, :], [b2], [vt])
                p.dma("sp", VB[ch], vt[:], [vt], [VB], f"vtm{s % 2}")
                b3 = ps[6]
                for kc in range(NKC):
                    p.mm(b3[:, 0:256], hT[:, kc, s * 128:(s + 1) * 128], wgv_sb[:, kc, :], kc == 0, kc == NKC - 1,
                         [(hT, kc), wgv_sb], [b3])
                j = cnt["o"] % 3
                cnt["o"] += 1
                ob = outb[j]
                p.cp("act", ob[:, 0:256], b3[:, 0:256], [b3], [ob])
                p.dma("sp", VC[ch], ob[:, 0:256], [ob], [VC], f"outb{j}")
                b4 = ps[7]
                b4b = psb(7)
                for j2 in range(4):
                    p.tr(b4b[:, j2 * 64:(j2 + 1) * 64], kbT[:, j2, s * 128:(s + 1) * 128], ident[0:64, 0:64], [(kbT, j2), ident], [b4])
                kzt = kz[s % 2]
                p.tt("dve", kzt[:, 0, :].rearrange("p (h d) -> p h d", h=8), b4b[:, 0:256].rearrange("p (h d) -> p h d", h=8),
                     zf[:].unsqueeze(2).to_broadcast([128, 8, 32]), ALU.mult, [b4, zf], [(kzt, 0)])
                p.tt("dve", kzt[:, 1, :].rearrange("p (h d) -> p h d", h=8), b4b[:, 0:256].rearrange("p (h d) -> p h d", h=8),
                     zb[:].unsqueeze(2).to_broadcast([128, 8, 32]), ALU.mult, [b4, zb], [(kzt, 1)])
                dd = drs[s % 2]
                for dr_ in range(2):
                    b5 = ps[2 + dr_]
                    for h in range(8):
                        p.mm(b5[0:32, h * 64:(h + 1) * 64], kzt[:, dr_, h * 32:(h + 1) * 32], vt[:, h * 64:(h + 1) * 64],
                             True, True, [(kzt, dr_), vt], [b5])
                    p.cp("act" if dr_ == 0 else "dve", dd[:, dr_, :], b5[0:32, :], [b5], [(dd, dr_)])
                    p.dma("sp", DR[dr_, ch], dd[:, dr_, :], [(dd, dr_)], [DR], f"drs{s % 2}")
        p.reset()
        if "A" in debug and l == debug["A"]:
            break

        gtab = p.sbuf("gtab", [32, 2, 8], F32)
        p.act(gtab[:].rearrange("p a h -> p (a h)"), lg[0:32, :], AF.Exp, [lg], [gtab], scale=128.0)
        st = p.sbuf("st", [32, 512], F32)
        stb = [p.sbuf(f"stb{i}", [32, 512], BF16) for i in range(2)]
        dl = [p.sbuf(f"dl{i}", [32, 512], F32) for i in range(2)]
        zero = p.sbuf("zero32", [32, 512], BF16)
        p.memset("dve", zero[:], 0.0, [zero])
        sc = {"n": 0}

        def st_store(dr_, ch, src=None):
            i = sc["n"] % 2
            sc["n"] += 1
            if src is None:
                p.cp("act", stb[i][:], st[:], [st], [stb[i]])
                src = stb[i]
                p.dma("sp", RS[dr_, ch], src[:], [src], [RS], f"stb{i}")
            else:
                p.dma("sp", RS[dr_, ch], src[:], [src], [RS], "zero32")

        def st_load(dr_, ch, into_state):
            i = sc["n"] % 2
            d_ = dl[i]
            p.dma("act", d_[:], DR[dr_, ch], [DR], [d_], f"dl{i}")
            return d_

        def st_step(dr_, ch, first=False):
            d_ = st_load(dr_, ch, False)
            if first:
                p.cp("dve", st[:], d_[:], [d_], [st])
            else:
                g3 = gtab[:, dr_, :].unsqueeze(2).to_broadcast([32, 8, 64])
                p.tt("dve", st[:].rearrange("p (h v) -> p h v", h=8), st[:].rearrange("p (h v) -> p h v", h=8), g3, ALU.mult,
                     [st, gtab], [st])
                p.tt("dve", st[:], st[:], d_[:], ALU.add, [st, d_], [st])

        st_store(0, 0, zero)
        st_step(0, 0, first=True)
        st_store(0, 1)
        st_step(0, 1)
        for ch in range(2, NCH):
            st_store(0, ch)
            if ch < NCH - 1:
                st_step(0, ch)
        st_store(1, 1, zero)
        st_step(1, 1, first=True)
        st_store(1, 0)
        st_step(1, 0)
        for ch in range(NCH - 1, 1, -1):
            st_store(1, ch)
            if ch > 2:
                st_step(1, ch)
        p.reset()

        ka = p.sbuf("ka_sb", [96, 8, T], BF16)
        va = p.sbuf("va_sb", [128, NCH, 520], BF16)
        for h in range(8):
            p.dma("sp" if h % 2 == 0 else "act", ka[:, h, :], KA[h], [KA], [(ka, h)], "b_kv")
        for c4 in range(0, NCH, 2):
            p.dma("sp" if c4 % 4 == 0 else "act", va[:, c4:c4 + 2, :], VA[c4:c4 + 2].rearrange("c p f -> p c f"), [VA], [(va, c4)], "b_kv")
        qsb = [p.sbuf(f"qsb{i}", [128, 512], BF16) for i in range(2)]
        pT = [p.sbuf(f"pT{i}", [128, 512], BF16) for i in range(3)]
        rden = p.sbuf("rden", [128, 512], F32)
        accs = p.sbuf("accs", [128, 512], F32)
        oo = [p.sbuf(f"oo{i}", [128, 512], BF16) for i in range(2)]
        for gi, (g0, N) in enumerate(GROUPS):
            isctx = gi == 0
            if last and isctx:
                continue
            kt_list = list(range(2)) if isctx else list(range(NCH))
            for h in range(8):
                q_ = qsb[h % 2]
                p.dma("sp", q_[0:96, 0:N], QA[h, :, g0:g0 + N], [QA], [q_], f"qsb{h % 2}")
                acc = ps[4 + h % 2]
                for ki, kt in enumerate(kt_list):
                    sb_ = ps[ki % 3]
                    p.mm(sb_[:, 0:N], ka[:, h, kt * 128:(kt + 1) * 128], q_[0:96, 0:N], True, True, [ka, q_], [sb_])
                    if "B1dbg" in debug and ki == 2:
                        DBG = p.dram("dbgt", [4, 128, 512], F32, "ExternalOutput")
                        p.cp("act", accs[:, 0:N], sb_[:, 0:N], [sb_], [accs])
                        p.dma("sp", DBG[0], accs[:, 0:N], [accs], [DBG], "dbg")
                        p.cp("act", rden[0:96, 0:N], q_[0:96, 0:N], [q_], [rden])
                        p.dma("sp", DBG[1][0:96], rden[0:96, 0:N], [rden], [DBG], "dbg1")
                        o_ = p.sbuf("dbgk", [128, 512], F32)
                        p.cp("act", o_[0:96, 0:128], ka[:, h, kt * 128:(kt + 1) * 128], [ka], [o_])
                        p.dma("sp", DBG[2][0:96, 0:128], o_[0:96, 0:128], [o_], [DBG], "dbg2")
                        o2_ = p.sbuf("dbgv", [128, 520], F32)
                        p.cp("act", o2_[:, :], va[:, kt, :], [va], [o2_])
                        p.dma("sp", DBG[3][:, 0:512], o2_[:, 0:512], [o2_], [DBG], "dbg3")
                        p.barrier(final=True)
                        p.emit()
                        return nc, p
                    pt = pT[ki % 3]
                    p.act(pt[:, 0:N], sb_[:, 0:N], AF.Exp, [sb_], [pt])
                    fl = (ki == 0, ki == len(kt_list) - 1)
                    p.mm(acc[0:64, 0:N], va[:, kt, h * 65:h * 65 + 64], pt[:, 0:N], fl[0], fl[1], [va, pt], [acc])
                    p.mm(ps[7][0:1, 0:N], ones_bf[:, 0:1], pt[:, 0:N], fl[0], fl[1], [ones_bf, pt], [ps[7]])
                p.recip(rden[0:1, 0:N], ps[7][0:1, 0:N], [ps[7]], [rden])
                p.cp("act", accs[0:64, 0:N], acc[0:64, 0:N], [acc], [accs])
                p.mm(ps[6][0:64, 0:N], ones_f[0:1, 0:64], rden[0:1, 0:N], True, True, [ones_f, rden], [ps[6]])
                o_ = oo[h % 2]
                p.tt("dve", o_[0:64, 0:N], accs[0:64, 0:N], ps[6][0:64, 0:N], ALU.mult, [accs, ps[6]], [o_])
                p.dma("sp", OA[h, :, g0:g0 + N], o_[0:64, 0:N], [o_], [OA], f"oo{h % 2}")
        p.reset_to(layer_base)
        if "B1" in debug and l == debug["B1"]:
            break
        kcs = p.sbuf("kc_sb", [128, 2, T], BF16)
        vcs = p.sbuf("vc_sb", [128, NCH, 256], BF16)
        for c4 in range(0, NCH, 2):
            p.dma("act", vcs[:, c4:c4 + 2, :], VC[c4:c4 + 2].rearrange("c p f -> p c f"), [VC], [(vcs, c4)], "b_kv")
        for g in range(2):
            p.dma("sp", kcs[:, g, :], KC[g], [KC], [(kcs, g)], "b_kv")
        qsb = [p.sbuf(f"qsb{i}", [128, 512], BF16) for i in range(2)]
        pT = [p.sbuf(f"pT{i}", [128, 512], BF16) for i in range(3)]
        rden = p.sbuf("rden", [128, 512], F32)
        accs = p.sbuf("accs", [128, 512], F32)
        oo = [p.sbuf(f"oo{i}", [128, 512], BF16) for i in range(2)]
        for gi, (g0, N) in enumerate(GROUPS):
            isctx = gi == 0
            if last and isctx:
                continue
            kt_list = list(range(2)) if isctx else list(range(NCH))
            for h in range(8):
                g = h // 4
                q_ = qsb[h % 2]
                p.dma("sp", q_[:, 0:N], QC[h, :, g0:g0 + N], [QC], [q_], f"qsb{h % 2}")
                acc = ps[4 + h % 2]
                den = ps[7]
                for ki, kt in enumerate(kt_list):
                    sb_ = ps[ki % 3]
                    p.mm(sb_[:, 0:N], kcs[:, g, kt * 128:(kt + 1) * 128], q_[:, 0:N], True, True, [kcs, q_], [sb_])
                    if "B2dbg" in debug and ki == 2:
                        DBG = p.dram("dbgt", [3, 128, 512], F32, "ExternalOutput")
                        p.cp("act", accs[:, 0:N], sb_[:, 0:N], [sb_], [accs])
                        p.dma("sp", DBG[0], accs[:, 0:N], [accs], [DBG], "dbg")
                        p.cp("act", rden[:, 0:N], q_[:, 0:N], [q_], [rden])
                        p.dma("sp", DBG[1], rden[:, 0:N], [rden], [DBG], "dbg1")
                        o_ = p.sbuf("dbgk", [128, 512], F32)
                        p.cp("act", o_[:, 0:128], kcs[:, g, kt * 128:(kt + 1) * 128], [kcs], [o_])
                        p.dma("sp", DBG[2][:, 0:128], o_[:, 0:128], [o_], [DBG], "dbg2")
                        p.barrier(final=True)
                        p.emit()
                        return nc, p
                    pt = pT[ki % 3]
                    p.act(pt[:, 0:N], sb_[:, 0:N], AF.Exp, [sb_], [pt])
                    fl = (ki == 0, ki == len(kt_list) - 1)
                    p.mm(acc[:, 0:N], vcs[:, kt, g * 128:(g + 1) * 128], pt[:, 0:N], fl[0], fl[1], [vcs, pt], [acc])
                    p.mm(den[0:1, 0:N], ones_bf[:, 0:1], pt[:, 0:N], fl[0], fl[1], [ones_bf, pt], [den])
                p.recip(rden[0:1, 0:N], den[0:1, 0:N], [den], [rden])
                p.cp("act", accs[:, 0:N], acc[:, 0:N], [acc], [accs])
                p.mm(ps[6][:, 0:N], ones_f[0:1, :], rden[0:1, 0:N], True, True, [ones_f, rden], [ps[6]])
                o_ = oo[h % 2]
                p.tt("dve", o_[:, 0:N], accs[:, 0:N], ps[6][:, 0:N], ALU.mult, [accs, ps[6]], [o_])
                p.dma("sp", OC[h, :, g0:g0 + N], o_[:, 0:N], [o_], [OC], f"oo{h % 2}")
        p.reset_to(layer_base)
        if "B2" in debug and l == debug["B2"]:
            break
        lgs = p.sbuf("lgs", [128, 2, 4], F32)
        tmp8 = p.sbuf("tmp8", [128, 4, 8], F32)
        for dr_ in range(2):
            p.tt("dve", tmp8[:], hsel[:], lg[:, dr_ * 8:dr_ * 8 + 8].unsqueeze(1).to_broadcast([128, 4, 8]), ALU.mult,
                 [hsel, lg], [tmp8])
            p.rsum(lgs[:, dr_, :], tmp8[:], [tmp8], [lgs])
        xi = p.sbuf("xi", [128, 2, 4, 128], BF16)
        xif = p.sbuf("xif", [128, 128], F32)
        for dr_ in range(2):
            for j in range(4):
                p.act(xif[:], misc[:, 6 + dr_, :], AF.Exp, [misc, lgs], [xif], scale=lgs[:, dr_, j:j + 1])
                p.cp("dve", xi[:, dr_, j, :], xif[:], [xif], [xi])
        mcomb = p.sbuf("mcomb", [128, 8, 128], F32)
        mtmp = p.sbuf("mtmp", [128, 128], F32)
        for h in range(8):
            p.act(mcomb[:, h, :], misc[:, 2, :], AF.Exp, [misc, lg], [(mcomb, h)], scale=lg[:, h:h + 1])
            p.tt("dve", mcomb[:, h, :], mcomb[:, h, :], misc[:, 3, :], ALU.mult, [(mcomb, h), misc], [(mcomb, h)])
            p.act(mtmp[:], misc[:, 4, :], AF.Exp, [misc, lg], [mtmp], scale=lg[:, 8 + h:9 + h])
            p.tt("dve", mtmp[:], mtmp[:], misc[:, 5, :], ALU.mult, [mtmp, misc], [mtmp])
            p.tt("dve", mcomb[:, h, :], mcomb[:, h, :], mtmp[:], ALU.add, [(mcomb, h), mtmp], [(mcomb, h)])
        ones64 = p.sbuf("ones64", [64, 64], F32)
        p.memset("dve", ones64[:], 1.0 / 64.0, [ones64])
        onesD = p.sbuf("onesD", [128, 128], F32)
        p.memset("dve", onesD[:], 1.0 / D, [onesD])
        oA = p.sbuf("oA", [64, 8, 512], BF16)
        oB = p.sbuf("oB", [64, 8, 512], BF16)
        oC = p.sbuf("oC", [128, 8, 512], BF16)
        qb_sb = p.sbuf("qb_sb", [64, 4, 512], BF16)
        kb_sb = p.sbuf("kb_sb", [64, 4, 512], BF16)
        vb_sb = p.sbuf("vb_sb", [128, 4, 512], BF16)
        qxi = p.sbuf("qxi", [64, 2, 4, 512], BF16)
        rsb = p.sbuf("rsb", [64, 2, 4, 4, 64], BF16)
        pr = [p.sbuf(f"pr{i}", [128, 128], BF16) for i in range(2)]
        ysb = p.sbuf("ysb", [64, 512], F32)
        ysq = p.sbuf("ysq", [64, 512], F32)
        mean = p.sbuf("mean", [128, 512], F32)
        var = p.sbuf("var", [128, 512], F32)
        gbs = [p.sbuf(f"gbs{i}", [64, 512], BF16) for i in range(2)]
        wa_sb = [p.sbuf(f"wa_sb{i}", [64, 8, 128], BF16) for i in range(2)]
        wb_sb = [p.sbuf(f"wb_sb{i}", [64, 8, 128], BF16) for i in range(2)]
        wc_sb = [p.sbuf(f"wc_sb{i}", [128, 8, 128], BF16) for i in range(2)]
        wo_sb = [p.sbuf(f"wo_sb{i}", [128, 16, 128], BF16) for i in range(2)]
        gts = [p.sbuf(f"gts{i}", [128, 3, 512], BF16) for i in range(2)]
        mT = p.sbuf("mT", [128, 16, 512], BF16)
        tT = p.sbuf("tT", [128, 16, 512], F32)
        xa = [p.sbuf(f"xa{i}", [128, 512], F32) for i in range(2)]
        u1 = [p.sbuf(f"u1_{i}", [128, 512], F32) for i in range(2)]
        u2 = [p.sbuf(f"u2_{i}", [128, 512], F32) for i in range(2)]
        sq32 = [p.sbuf(f"sq32_{i}", [128, 512], F32) for i in range(2)]
        x1o = [p.sbuf(f"x1o{i}", [128, 512], F32) for i in range(2)]
        h2o = [p.sbuf(f"h2o{i}", [128, 512], BF16) for i in range(2)]

        for gi, (g0, N) in enumerate(GROUPS):
            isctx = gi == 0
            if last and isctx:
                continue
            nsub = N // 128
            for h in range(8):
                p.dma("sp" if h % 2 == 0 else "act", oA[:, h, 0:N], OA[h, :, g0:g0 + N], [OA], [(oA, h)], "oA")
                p.dma("act" if h % 2 == 0 else "sp", oC[:, h, 0:N], OC[h, :, g0:g0 + N], [OC], [(oC, h)], "oC")
            for j in range(4):
                p.dma("sp", qb_sb[:, j, 0:N], QB[j, :, g0:g0 + N], [QB], [(qb_sb, j)], "qb_sb")
                p.dma("act", kb_sb[:, j, 0:N], KB[j, :, g0:g0 + N], [KB], [(kb_sb, j)], "kb_sb")
            p.dma("sp", vb_sb[:, 0:nsub, :], VB[g0 // 128:g0 // 128 + nsub].rearrange("c p f -> p c f"), [VB], [vb_sb], "vb_sb")
            for dr_ in range(2):
                for s in range(nsub):
                    for hh in range(2):
                        p.dma("act", rsb[hh * 32:(hh + 1) * 32, dr_, s, :, :],
                              RS[dr_, g0 // 128 + s].rearrange("d (j h v) -> h d j v", j=4, h=2)[hh], [RS], [(rsb, (dr_, s, hh))], "rsb")
                for j in range(4):
                    p.tt("pool", qxi[:, dr_, j, 0:N].rearrange("p (s i) -> p s i", i=128),
                         qb_sb[:, j, 0:N].rearrange("p (s i) -> p s i", i=128),
                         xi[0:64, dr_, j, :].unsqueeze(1).to_broadcast([64, nsub, 128]), ALU.mult, [qb_sb, xi], [qxi])
            for h in range(8):
                j, hh = h // 2, h % 2
                pb = slice(hh * 32, hh * 32 + 32)
                yb = ps[4 + h % 2]
                for s in range(nsub):
                    cs = slice(s * 128, (s + 1) * 128)
                    sb_ = ps[(h * 4 + s) % 3]
                    p.mm(sb_[:, 0:128], kb_sb[pb, j, cs], qb_sb[pb, j, cs], True, True, [kb_sb, qb_sb], [sb_])
                    pr_ = pr[(h * 4 + s) % 2]
                    p.tt("dve", pr_[:], sb_[:, 0:128], mcomb[:, h, :], ALU.mult, [sb_, (mcomb, h)], [pr_])
                    p.mm(yb[0:64, cs], vb_sb[:, s, h * 64:(h + 1) * 64], pr_[:], True, False, [vb_sb, pr_], [yb])
                    p.mm(yb[0:64, cs], rsb[pb, 0, s, j, :], qxi[pb, 0, j, cs], False, False, [rsb, qxi], [yb])
                    p.mm(yb[0:64, cs], rsb[pb, 1, s, j, :], qxi[pb, 1, j, cs], False, True, [rsb, qxi], [yb])
                p.cp("act", ysb[:, 0:N], yb[0:64, 0:N], [yb], [ysb])
                p.act(ysq[:, 0:N], yb[0:64, 0:N], AF.Square, [yb], [ysq])
                p.mm(ps[6][0:64, 0:N], ones64[:], ysb[:, 0:N], True, True, [ones64, ysb], [ps[6]])
                p.mm(ps[7][0:64, 0:N], ones64[:], ysq[:, 0:N], True, True, [ones64, ysq], [ps[7]])
                gb_ = gbs[h % 2]
                p.dma("sp", gb_[:, 0:N], GB[h, :, g0:g0 + N], [GB], [gb_], f"gbs{h % 2}")
                p.cp("act", mean[0:64, 0:N], ps[6][0:64, 0:N], [ps[6]], [mean])
                p.tt("dve", var[0:64, 0:N], mean[0:64, 0:N], mean[0:64, 0:N], ALU.mult, [mean], [var])
                p.tt("dve", var[0:64, 0:N], ps[7][0:64, 0:N], var[0:64, 0:N], ALU.subtract, [ps[7], var], [var])
                p.act(var[0:64, 0:N], var[0:64, 0:N], AF.Sqrt, [var, epsc], [var], bias=epsc[0:64, 1:2], scale=1.0)
                p.recip(var[0:64, 0:N], var[0:64, 0:N], [var], [var])
                p.tt("pool", ysb[:, 0:N], ysb[:, 0:N], mean[0:64, 0:N], ALU.subtract, [ysb, mean], [ysb])
                p.tt("dve", ysb[:, 0:N], ysb[:, 0:N], var[0:64, 0:N], ALU.mult, [ysb, var], [ysb])
                p.tt("dve", oB[:, h, 0:N], ysb[:, 0:N], gb_[:, 0:N], ALU.mult, [ysb, gb_], [(oB, h)])
            for ct in range(16):
                i = ct % 2
                p.dma("pool", wa_sb[i][:], W["wbra"][ct], [W["wbra"]], [wa_sb[i]], f"wa_sb{i}")
                p.dma("pool", wb_sb[i][:], W["wbrb"][ct], [W["wbrb"]], [wb_sb[i]], f"wb_sb{i}")
                p.dma("pool", wc_sb[i][:], W["wbrc"][ct], [W["wbrc"]], [wc_sb[i]], f"wc_sb{i}")
                for b3_ in range(3):
                    p.dma("act", gts[i][:, b3_, 0:N], GT[b3_ * 16 + ct, :, g0:g0 + N], [GT], [(gts[i], b3_)], f"gts{i}")
                for h in range(8):
                    p.mm(ps[0][:, 0:N], wa_sb[i][:, h, :], oA[:, h, 0:N], h == 0, h == 7, [wa_sb[i], (oA, h)], [ps[0]])
                for h in range(8):
                    p.mm(ps[1][:, 0:N], wb_sb[i][:, h, :], oB[:, h, 0:N], h == 0, h == 7, [wb_sb[i], (oB, h)], [ps[1]])
                for h in range(8):
                    p.mm(ps[2][:, 0:N], wc_sb[i][:, h, :], oC[:, h, 0:N], h == 0, h == 7, [wc_sb[i], (oC, h)], [ps[2]])
                a, b = u1[i], u2[i]
                p.tt("dve", a[:, 0:N], ps[0][:, 0:N], gts[i][:, 0, 0:N], ALU.mult, [ps[0], gts[i]], [a])
                p.tt("dve", b[:, 0:N], ps[1][:, 0:N], gts[i][:, 1, 0:N], ALU.mult, [ps[1], gts[i]], [b])
                p.tt("pool", a[:, 0:N], a[:, 0:N], b[:, 0:N], ALU.add, [a, b], [a])
                p.tt("dve", b[:, 0:N], ps[2][:, 0:N], gts[i][:, 2, 0:N], ALU.mult, [ps[2], gts[i]], [b])
                p.tt("pool", mT[:, ct, 0:N], a[:, 0:N], b[:, 0:N], ALU.add, [a, b], [(mT, ct)])
            for ct in range(16):
                i = ct % 2
                p.dma("pool", wo_sb[i][:], W["wout"][ct], [W["wout"]], [wo_sb[i]], f"wo_sb{i}")
                p.dma("sp", xa[i][:, 0:N], XT[:, ct, g0:g0 + N], [XT], [xa[i]], f"xa{i}")
                p.act(xa[i][:, 0:N], xa[i][:, 0:N], AF.Copy, [xa[i]], [xa[i]], scale=float(ALPHA))
                bk = ps[3 + i]
                for kc in range(16):
                    p.mm(bk[:, 0:N], wo_sb[i][:, kc, :], mT[:, kc, 0:N], kc == 0, kc == 15, [wo_sb[i], (mT, kc)], [bk])
                p.stt(tT[:, ct, 0:N], bk[:, 0:N], mod("g1", ct, isctx), xa[i][:, 0:N], ALU.mult, ALU.add, [bk, modv, xa[i]], [(tT, ct)])
            for c in range(NKC):
                p.mm(ps[6][:, 0:N], onesD[:], tT[:, c, 0:N], c == 0, c == NKC - 1, [onesD, (tT, c)], [ps[6]])
            for c in range(NKC):
                s_ = sq32[c % 2]
                p.act(s_[:, 0:N], tT[:, c, 0:N], AF.Square, [(tT, c)], [s_])
                p.mm(ps[7][:, 0:N], onesD[:], s_[:, 0:N], c == 0, c == NKC - 1, [onesD, s_], [ps[7]])
            p.cp("act", mean[:, 0:N], ps[6][:, 0:N], [ps[6]], [mean])
            p.tt("dve", var[:, 0:N], mean[:, 0:N], mean[:, 0:N], ALU.mult, [mean], [var])
            p.tt("dve", var[:, 0:N], ps[7][:, 0:N], var[:, 0:N], ALU.subtract, [ps[7], var], [var])
            p.act(var[:, 0:N], var[:, 0:N], AF.Sqrt, [var, epsc], [var], bias=epsc[:, 1:2], scale=1.0)
            p.recip(var[:, 0:N], var[:, 0:N], [var], [var])
            for c in range(NKC):
                a = u1[c % 2]
                p.tt("pool", a[:, 0:N], tT[:, c, 0:N], mean[:, 0:N], ALU.subtract, [(tT, c), mean], [a])
                p.tt("dve", a[:, 0:N], a[:, 0:N], var[:, 0:N], ALU.mult, [a, var], [a])
                xo = x1o[c % 2]
                p.act(xo[:, 0:N], a[:, 0:N], AF.Identity, [a, lnp], [xo], scale=lnp[:, 0, c:c + 1], bias=lnp[:, 1, c:c + 1])
                p.dma("sp", X1[:, c, g0:g0 + N], xo[:, 0:N], [xo], [X1], f"x1o{c % 2}")
                ho = h2o[c % 2]
                p.ts("dve", ho[:, 0:N], xo[:, 0:N], mod("sc2", c, isctx), mod("sh2", c, isctx), ALU.mult, ALU.add, [xo, modv], [ho])
                p.dma("act", H2[:, c, g0:g0 + N], ho[:, 0:N], [ho], [H2], f"h2o{c % 2}")
        p.reset_to(layer_base)
        if "B" in debug and l == debug["B"]:
            break


        onesD = p.sbuf("onesD", [128, 128], F32)
        p.memset("dve", onesD[:], 1.0 / D, [onesD])
        pk = p.sbuf("pk", [128, 256], F32)
        p.dma("sp", pk[:], W["pk"][:], [W["pk"]], [pk], "c_misc")
        h2 = p.sbuf("h2_sb", [128, NKC, 512], BF16)
        acc = p.sbuf("acc_sb", [128, NKC, 512], F32)
        wqs = [p.sbuf(f"wqs{i}", [128, 16, 128], BF16) for i in range(1)] * 2
        qf = [p.sbuf(f"qf{i}", [128, 512], F32) for i in range(1)] * 2
        aS = p.sbuf("aS", [128, 4, 8, 128], F32)
        s2S = p.sbuf("s2S", [128, 4, 8, 128], F32)
        b2S = p.sbuf("b2S", [128, 4, 8], F32)
        s1t = p.sbuf("s1t", [128, 128], F32)
        wk1 = p.sbuf("wk1", [128, 256], F32)
        wk2 = p.sbuf("wk2", [128, 256], F32)
        v16 = p.sbuf("v16", [128, 2, 16], F32)
        cand = p.sbuf("cand", [128, 16, 16], F32)
        c24 = p.sbuf("c24", [128, 24], F32)
        e16 = p.sbuf("e16", [128, 16], F32)
        sm = p.sbuf("sm", [128, 8], F32)
        NI = 8
        Db = [p.sbuf(f"Db{i}", [128, NI, 128], F32) for i in range(2)]
        Eb = [p.sbuf(f"Eb{i}", [128, NI * 128], F32) for i in range(2)]
        Gacc = [p.sbuf(f"Gacc{i}", [128, NI * 128], F32) for i in range(4)]
        Gbf = [[p.sbuf(f"Gbf{b}_{i}", [128, NI * 128], BF16) for i in range(4)] for b in range(2)]
        ut = [p.sbuf(f"ut{i}", [128, 16, 128], BF16) for i in range(2)]
        vt_ = [p.sbuf(f"vts{i}", [128, NI, 128], BF16) for i in range(2)]
        gl = [p.sbuf(f"gl{i}", [128, 512], BF16) for i in range(2)]
        AT = [p.sbuf(f"AT{b}", [128, NI, 512], BF16) for b in range(2)]
        xa = [Eb[0], Eb[1]]
        tT = acc
        mean = Gacc[0]
        var = Gacc[1]
        u1 = [Gacc[2], Gacc[3]]
        sq32 = [Tk(Db[0][:].rearrange("p a b -> p (a b)"), "Db0v"), Tk(Db[1][:].rearrange("p a b -> p (a b)"), "Db1v")]
        sq32[0].id, sq32[1].id = Db[0].id, Db[1].id
        x1o = [Tk(Gbf[0][0].t.bitcast(F32), "g0v"), Tk(Gbf[0][1].t.bitcast(F32), "g1v")]
        x1o[0].id, x1o[1].id = Gbf[0][0].id, Gbf[0][1].id

        for gi, (g0, N) in enumerate(GROUPS):
            isctx = gi == 0
            if last and isctx:
                continue
            nsub = N // 128
            for c in range(NKC):
                p.dma("sp" if c % 2 == 0 else "act", h2[:, c, 0:N], H2[:, c, g0:g0 + N], [H2], [(h2, c)], "h2_sb")
            for h in range(8):
                w_ = wqs[h % 2]
                p.dma("pool", w_[:], W["pwq"][h], [W["pwq"]], [w_], f"wqs{h % 2}")
                bk = ps[h % 2]
                for kc in range(NKC):
                    p.mm(bk[:, 0:N], w_[:, kc, :], h2[:, kc, 0:N], kc == 0, kc == NKC - 1, [w_, (h2, kc)], [bk])
                q_ = qf[h % 2]
                p.cp("act", q_[:, 0:N], bk[:, 0:N], [bk], [q_])
                for tt_ in range(nsub):
                    cs = slice(tt_ * 128, (tt_ + 1) * 128)
                    sb_ = ps[2 + tt_ % 2]
                    p.mm(sb_[:, 0:256], q_[:, cs], pk[:, :], True, True, [q_, pk], [sb_])
                    p.cp("act", s1t[:], sb_[:, 0:128], [sb_], [s1t])
                    p.cp("act", s2S[:, tt_, h, :], sb_[:, 128:256], [sb_], [(s2S, (tt_, h))])
                    for w2, src in ((0, s1t[:]), (1, s2S[:, tt_, h, :])):
                        p.max8(v16[:, w2, 0:8], src, [s1t, (s2S, (tt_, h))], [v16])
                        p.mrep(wk1[:, 0:128], v16[:, w2, 0:8], src, [s1t, (s2S, (tt_, h)), v16], [wk1])
                        p.max8(v16[:, w2, 8:16], wk1[:, 0:128], [wk1], [v16])
                    p.tt("dve", cand[:], v16[:, 0, :].unsqueeze(2).to_broadcast([128, 16, 16]),
                         v16[:, 1, :].unsqueeze(1).to_broadcast([128, 16, 16]), ALU.add, [v16], [cand])
                    cf = cand[:].rearrange("p a b -> p (a b)")
                    p.max8(c24[:, 0:8], cf, [cand], [c24])
                    p.mrep(wk1[:], c24[:, 0:8], cf, [cand, c24], [wk1])
                    p.max8(c24[:, 8:16], wk1[:], [wk1], [c24])
                    p.ts("dve", sm[:, 0:1], c24[:, 0:1], -1.0, None, ALU.mult, None, [c24], [sm])
                    p.act(e16[:], c24[:, 0:16], AF.Exp, [c24, sm], [e16, sm], bias=sm[:, 0:1], accum=sm[:, 1:2])
                    p.ts("dve", sm[:, 2:3], c24[:, 15:16], -2e-5, None, ALU.add, None, [c24], [sm])
                    p.act(sm[:, 3:4], sm[:, 1:2], AF.Ln, [sm], [sm])
                    p.tt("dve", sm[:, 4:5], sm[:, 2:3], sm[:, 0:1], ALU.add, [sm], [sm])
                    p.tt("dve", b2S[:, tt_, h:h + 1], sm[:, 4:5], sm[:, 3:4], ALU.subtract, [sm], [(b2S, (tt_, h))])
                    p.ts("dve", aS[:, tt_, h, :], s1t[:], sm[:, 2:3], None, ALU.subtract, None, [s1t, sm], [(aS, (tt_, h))])
            if debug.get("Cstop") == 1:
                p.barrier(final=True)
                p.emit()
                return nc, p
            nchunk = 128 // NI
            for ic in range(nchunk):
                gb = ic % 2
                for tt_ in range(nsub):
                    for h in range(8):
                        k = (tt_ * 8 + h) % 2
                        p.tt("pool", Db[k][:], aS[:, tt_, h, ic * NI:(ic + 1) * NI].unsqueeze(2).to_broadcast([128, NI, 128]),
                             s2S[:, tt_, h, :].unsqueeze(1).to_broadcast([128, NI, 128]), ALU.add,
                             [(aS, (tt_, h)), (s2S, (tt_, h))], [Db[k]])
                        df = Db[k][:].rearrange("p a b -> p (a b)")
                        p.stt(df, df, 1e30, df, ALU.mult, ALU.min, [Db[k]], [Db[k]])
                        if h == 0:
                            p.act(Gacc[tt_][:], df, AF.Exp, [Db[k], (b2S, (tt_, h))], [Gacc[tt_]], bias=b2S[:, tt_, h:h + 1])
                        else:
                            p.act(Eb[k][:], df, AF.Exp, [Db[k], (b2S, (tt_, h))], [Eb[k]], bias=b2S[:, tt_, h:h + 1])
                            dst = Gbf[gb][tt_] if h == 7 else Gacc[tt_]
                            p.tt("dve", dst[:], Gacc[tt_][:], Eb[k][:], ALU.add, [Gacc[tt_], Eb[k]], [dst])
                if debug.get("Cstop") == 2:
                    p.barrier(final=True)
                    p.emit()
                    return nc, p
                for e in range(NI):
                    eg = ic * NI + e
                    u_ = ut[eg % 2]
                    p.dma("pool", u_[:], W["put"][eg], [W["put"]], [u_], f"ut{eg % 2}")
                    hb = ps[eg % 2]
                    for kc in range(NKC):
                        p.mm(hb[:, 0:N], u_[:, kc, :], h2[:, kc, 0:N], kc == 0, kc == NKC - 1, [u_, (h2, kc)], [hb])
                    g_ = gl[eg % 2]
                    p.act(g_[:, 0:N], hb[:, 0:N], AF.Gelu, [hb], [g_])
                    gtb = ps[2 + eg % 2]
                    gtv = psb(2 + eg % 2)
                    for tt_ in range(nsub):
                        p.tr(gtv[:, tt_ * 128:(tt_ + 1) * 128], Gbf[gb][tt_][:, e * 128:(e + 1) * 128], ident[:],
                             [Gbf[gb][tt_], ident], [gtb])
                    p.tt("dve", AT[gb][:, e, 0:N], g_[:, 0:N], gtv[:, 0:N], ALU.mult, [g_, gtb], [(AT[gb], e)])
                if debug.get("Cstop") == 3:
                    p.barrier(final=True)
                    p.emit()
                    return nc, p
                for dt in range(16):
                    k = (ic * 16 + dt) % 2
                    v_ = vt_[k]
                    p.dma("pool", v_[:], W["pvt"][ic, dt], [W["pvt"]], [v_], f"vts{k}")
                    ab = ps[4 + dt % 2]
                    for e in range(NI):
                        p.mm(ab[:, 0:N], v_[:, e, :], AT[gb][:, e, 0:N], e == 0, e == NI - 1, [v_, (AT[gb], e)], [ab])
                    if ic == 0:
                        p.cp("act", acc[:, dt, 0:N], ab[:, 0:N], [ab], [(acc, dt)])
                    else:
                        p.tt("dve", acc[:, dt, 0:N], acc[:, dt, 0:N], ab[:, 0:N], ALU.add, [(acc, dt), ab], [(acc, dt)])
            if debug.get("Cstop") == 5:
                p.barrier(final=True)
                p.emit()
                return nc, p
            for ct in range(16):
                i = ct % 2
                p.dma("sp", xa[i][:, 0:N], X1[:, ct, g0:g0 + N], [X1], [xa[i]], f"xa{i}")
                p.act(xa[i][:, 0:N], xa[i][:, 0:N], AF.Copy, [xa[i]], [xa[i]], scale=float(ALPHA))
                p.stt(tT[:, ct, 0:N], acc[:, ct, 0:N], mod("g2", ct, isctx), xa[i][:, 0:N], ALU.mult, ALU.add,
                      [(acc, ct), modv, xa[i]], [(tT, ct)])
            for c in range(NKC):
                p.mm(ps[6][:, 0:N], onesD[:], tT[:, c, 0:N], c == 0, c == NKC - 1, [onesD, (tT, c)], [ps[6]])
            for c in range(NKC):
                s = sq32[c % 2]
                p.act(s[:, 0:N], tT[:, c, 0:N], AF.Square, [(tT, c)], [s])
                p.mm(ps[7][:, 0:N], onesD[:], s[:, 0:N], c == 0, c == NKC - 1, [onesD, s], [ps[7]])
            p.cp("act", mean[:, 0:N], ps[6][:, 0:N], [ps[6]], [mean])
            p.tt("dve", var[:, 0:N], mean[:, 0:N], mean[:, 0:N], ALU.mult, [mean], [var])
            p.tt("dve", var[:, 0:N], ps[7][:, 0:N], var[:, 0:N], ALU.subtract, [ps[7], var], [var])
            p.act(var[:, 0:N], var[:, 0:N], AF.Sqrt, [var, epsc], [var], bias=epsc[:, 1:2], scale=1.0)
            p.recip(var[:, 0:N], var[:, 0:N], [var], [var])
            for c in range(NKC):
                a = u1[c % 2]
                p.tt("pool", a[:, 0:N], tT[:, c, 0:N], mean[:, 0:N], ALU.subtract, [(tT, c), mean], [a])
                p.tt("dve", a[:, 0:N], a[:, 0:N], var[:, 0:N], ALU.mult, [a, var], [a])
                xo = x1o[c % 2]
                p.act(xo[:, 0:N], a[:, 0:N], AF.Identity, [a, lnp], [xo], scale=lnp[:, 2, c:c + 1], bias=lnp[:, 3, c:c + 1])
                if last:
                    p.dma("sp", out[:, c, g0 - CTX:g0 - CTX + N], xo[:, 0:N], [xo], [out], f"x1o{c % 2}")
                else:
                    p.dma("sp", XO[:, c, g0:g0 + N], xo[:, 0:N], [xo], [XO], f"x1o{c % 2}")
    p.barrier(final=True)
    p.emit()
    return nc, p


def kernel(**inputs):
    inp = {k: np.asarray(v) for k, v in inputs.items()}
    consts = host_consts()
    nc, prog = build_program()
    wl = [prep_layer_weights(inp, l) for l in range(DEPTH)]
    in_maps = []
    for core in range(8):
        b = core // 2
        xfull = np.concatenate([inp["ctx"][b], inp["x"][b]], axis=0)
        xT = np.ascontiguousarray(xfull.reshape(T, NKC, 128).transpose(2, 1, 0))
        cc = np.stack([fm(inp["c"][b]), fm(inp["c_ctx"])], axis=-1)
        m = {"xT": xT, "cc": np.ascontiguousarray(cc), "ropetab": consts["ropetab"], "permm": consts["permm"],
             "misc": consts["misc"], "colc": consts["colc"], "hsel": consts["hsel"]}
        for l in range(DEPTH):
            for k in WKEYS:
                m[f"{k}{l}"] = wl[l][k]
        in_maps.append(m)
    res = run_bass_kernel_spmd(nc, in_maps, core_ids=list(range(8)))
    outp = np.empty((4, SEQ, D), np.float32)
    for core in range(8):
        b, half = core // 2, core % 2
        o = res.results[core]["out"]
        sl = slice(half * 2048, (half + 1) * 2048)
        outp[b, sl] = o[:, :, sl].transpose(2, 1, 0).reshape(2048, D)
    return outp
```

```python
import os
import numpy as np
import concourse.bass as bass
import concourse.mybir as mybir
from concourse.bass_utils import run_bass_kernel_spmd

F32 = mybir.dt.float32
BF16 = mybir.dt.bfloat16
ALU = mybir.AluOpType
AF = mybir.ActivationFunctionType

D = 2048
SEQ = 4096
CTX = 256
T = SEQ + CTX
DEPTH = 2
GRID_W = 64
NKC = 16
N_IN = 10016
ALPHA = (2.0 * DEPTH) ** 0.25
MLA_SCALE = 96 ** -0.5
RET_K_SCALE = 32 ** -0.5
GQA_SCALE = 128 ** -0.5
NORM_EPS = 1e-5
RMS_EPS = 1e-6
GROUPS = [(0, 256)] + [(256 + 512 * i, 512) for i in range(8)]
NCH = T // 128

ENGINES = ("pe", "dve", "act", "pool", "sp")
EPOCH = int(os.environ.get("KEPOCH", "16000"))
SAME_ENGINE_SYNC = True
NCHAN = int(os.environ.get('KNCHAN', '1000000007'))
ARENA_WORDS = 46 * 1024


class Tk:
    _n = 0

    def __init__(self, ap, name):
        self.t = ap
        self.name = name
        Tk._n += 1
        self.id = Tk._n

    def __getitem__(self, idx):
        return self.t[idx]


class Chan:
    def __init__(self, name):
        self.name = name
        self.count = 0
        self.sem = None
        self.id = name


class Op:
    __slots__ = ("eng", "fn", "deps", "idx", "signal", "seq", "is_dma", "chan", "chan_count", "line")


class Prog:
    def __init__(self, nc):
        self.nc = nc
        self.ops = []
        self.state = {}
        self._ctx = []
        self.chans = {}
        self.last_eng = {}
        self.last_chan = {}
        self.names = {}
        self.pending = {}
        cm = nc.sbuf_tensor("arena", [128, ARENA_WORDS], F32)
        self.arena = cm.__enter__()
        self._ctx.append(cm)
        self.arena_off = 0
        self.arena_base = 0

    def sbuf(self, name, shape, dt, parts=128):
        n = int(np.prod(shape[1:]))
        words = n if dt == F32 else (n + 1) // 2
        off = self.arena_off
        assert off + words <= ARENA_WORDS, f"arena overflow at {name}: {off}+{words}"
        self.arena_off += words
        v = self.arena[0:shape[0], off:off + words]
        if dt != F32:
            v = v.bitcast(dt)[:, 0:n]
        if len(shape) == 3:
            v = v.rearrange("p (a b) -> p a b", a=shape[1])
        elif len(shape) == 4:
            v = v.rearrange("p (a b c) -> p a b c", a=shape[1], b=shape[2])
        elif len(shape) == 5:
            v = v.rearrange("p (a b c d) -> p a b c d", a=shape[1], b=shape[2], c=shape[3])
        return Tk(v, name)

    def persist(self):
        self.arena_base = self.arena_off

    def reset(self):
        self.barrier()
        self.arena_off = self.arena_base

    def reset_to(self, base):
        self.barrier()
        if os.environ.get("KNOALIAS"):
            return
        self.arena_off = self.arena_base = base

    def psum(self, name, shape, dt):
        cm = self.nc.psum_tensor(name, list(shape), dt)
        h = cm.__enter__()
        self._ctx.append(cm)
        return Tk(h, name)

    def dram(self, name, shape, dt, kind="Internal"):
        if kind == "Internal" and name in getattr(self, "dump", ()):
            kind = "ExternalOutput"
        h = self.nc.dram_tensor(name, list(shape), dt, kind=kind)
        return Tk(h.ap(), name)

    @staticmethod
    def _tok(x):
        if isinstance(x, tuple):
            return x[0], x[1]
        return x, None

    def _conf(self, st, key):
        if key is None:
            return list(st.keys())
        ks = []
        if key in st:
            ks.append(key)
        if None in st:
            ks.append(None)
        return ks

    def op(self, eng, fn, R=(), W=(), dma=False, extra=()):
        o = Op()
        o.eng, o.fn, o.idx, o.signal, o.seq, o.is_dma = eng, fn, len(self.ops), False, None, dma
        o.chan, o.chan_count = None, 0
        import sys as _s
        f = _s._getframe(1)
        while f.f_code.co_name in ("op", "mm", "tr", "act", "tt", "ts", "stt", "cp", "memset", "dma", "barrier"):
            f = f.f_back
        o.line = f.f_lineno
        deps = set(extra) | self.pending.pop(eng, set())
        for x in R:
            tile, key = self._tok(x)
            st = self.state.setdefault(tile.id, {})
            for k in self._conf(st, key):
                if st[k][0] is not None:
                    deps.add(st[k][0])
        for x in W:
            tile, key = self._tok(x)
            st = self.state.setdefault(tile.id, {})
            for k in self._conf(st, key):
                w, rs = st[k]
                if w is not None:
                    deps.add(w)
                deps.update(rs)
        for x in R:
            tile, key = self._tok(x)
            st = self.state.setdefault(tile.id, {})
            if key not in st:
                st[key] = [st[None][0] if None in st else None, []]
            st[key][1].append(o.idx)
        for x in W:
            tile, key = self._tok(x)
            st = self.state.setdefault(tile.id, {})
            if key is None:
                st.clear()
            st[key] = [o.idx, []]
        deps.discard(o.idx)
        o.deps = deps
        self.ops.append(o)
        self.last_eng[eng] = o.idx
        return o

    def dma(self, eng, out_ap, in_ap, R, W, chan):
        import zlib as _z
        chan = f"q{_z.crc32(chan.encode()) % NCHAN}_{eng}"
        if chan not in self.chans:
            self.chans[chan] = Chan(chan)
        c = self.chans[chan]
        o = self.op(eng, lambda e: e.dma_start(out=out_ap, in_=in_ap), R, W, dma=True)
        o.chan = c
        c.count += 1
        o.chan_count = c.count
        self.last_chan[chan] = o.idx
        return o

    def barrier(self, final=False):
        deps = set(self.last_eng.values()) | set(self.last_chan.values())
        for pd in self.pending.values():
            deps |= pd
        if final:
            for e in ENGINES:
                self.op(e, lambda en: en.nop(nofuse=True), extra=deps)
        else:
            self.pending = {e: set(deps) for e in ENGINES}

    def mm(self, out, lhsT, rhs, start, stop, R, W):
        return self.op("pe", lambda e: e.matmul(out, lhsT=lhsT, rhs=rhs, start=start, stop=stop), R, W)

    def tr(self, out, in_, ident, R, W):
        return self.op("pe", lambda e: e.transpose(out, in_, ident), R, W)

    def act(self, out, in_, func, R, W, bias=None, scale=None, accum=None):
        kw = {}
        if bias is not None:
            kw["bias"] = bias
        if scale is not None:
            kw["scale"] = scale
        if accum is not None:
            kw["accum_out"] = accum
        return self.op("act", lambda e: e.activation(out=out, in_=in_, func=func, **kw), R, W)

    def tt(self, eng, out, in0, in1, op, R, W):
        return self.op(eng, lambda e: e.tensor_tensor(out=out, in0=in0, in1=in1, op=op), R, W)

    def ts(self, eng, out, in0, s1, s2, op0, op1, R, W):
        if op1 is None:
            return self.op(eng, lambda e: e.tensor_scalar(out=out, in0=in0, scalar1=s1, scalar2=None, op0=op0), R, W)
        return self.op(eng, lambda e: e.tensor_scalar(out=out, in0=in0, scalar1=s1, scalar2=s2, op0=op0, op1=op1), R, W)

    def stt(self, out, in0, scalar, in1, op0, op1, R, W):
        return self.op("dve", lambda e: e.scalar_tensor_tensor(out=out, in0=in0, scalar=scalar, in1=in1, op0=op0, op1=op1), R, W)

    def cp(self, eng, out, in_, R, W):
        if eng == "act":
            return self.op("act", lambda e: e.copy(out=out, in_=in_), R, W)
        return self.op(eng, lambda e: e.tensor_copy(out=out, in_=in_), R, W)

    def recip(self, out, in_, R, W):
        return self.op("dve", lambda e: e.reciprocal(out=out, in_=in_), R, W)

    def max8(self, out, in_, R, W):
        return self.op("dve", lambda e: e.max(out=out, in_=in_), R, W)

    def mrep(self, out, rep, vals, R, W):
        return self.op("dve", lambda e: e.match_replace(out=out, in_to_replace=rep, in_values=vals, imm_value=-1e30), R, W)

    def rsum(self, out, in_, R, W):
        return self.op("dve", lambda e: e.reduce_sum(out=out, in_=in_, axis=mybir.AxisListType.X), R, W)

    def memset(self, eng, ap, val, W):
        return self.op(eng, lambda e: e.memset(ap, val), (), W)

    def emit(self):
        nc, ops = self.nc, self.ops

        def skip(y, o):
            return y.eng == o.eng and (y.eng == "pe" or not SAME_ENGINE_SYNC)

        for o in ops:
            for d in o.deps:
                y = ops[d]
                if y.is_dma or skip(y, o):
                    continue
                y.signal = True
        cnt = {e: 0 for e in ENGINES}
        for o in ops:
            if o.signal and not o.is_dma:
                cnt[o.eng] += 1
                o.seq = cnt[o.eng]
        sems, sem_ctx, nsem = {}, [], 0
        for e in ENGINES:
            sems[e] = []
            for k in range(max(1, (cnt[e] + EPOCH - 1) // EPOCH)):
                cm = nc.semaphore(f"s_{e}_{k}")
                sems[e].append(cm.__enter__())
                sem_ctx.append(cm)
                nsem += 1
        for c in self.chans.values():
            cm = nc.semaphore(f"d_{c.name}")
            c.sem = cm.__enter__()
            sem_ctx.append(cm)
            nsem += 1
        for k in range(int(os.environ.get("KXSEM", "0"))):
            cm = nc.semaphore(f"dummy{k}")
            cm.__enter__()
            sem_ctx.append(cm)
            nsem += 1
        self.nsem = nsem
        assert nsem <= 140, f"too many semaphores: {nsem}"
        known = {e: {} for e in ENGINES}
        run = {}
        waits = []
        for o in ops:
            w = {}
            for d in o.deps:
                y = ops[d]
                if y.is_dma:
                    key = ("d", y.chan.id)
                    val = 16 * run[y.chan.id]
                    semh = y.chan.sem
                else:
                    if skip(y, o):
                        continue
                    ep = (y.seq - 1) // EPOCH
                    key = ("e", y.eng, ep)
                    val = (y.seq - 1) % EPOCH + 1
                    semh = sems[y.eng][ep]
                if known[o.eng].get(key, 0) >= val:
                    continue
                if key not in w or w[key][1] < val:
                    w[key] = (semh, val)
            for key, (semh, val) in w.items():
                known[o.eng][key] = val
            waits.append(list(w.values()))
            if o.is_dma:
                run[o.chan.id] = o.chan_count
        self.waits_dbg = waits
        engmap = {"pe": "tensor", "dve": "vector", "act": "scalar", "pool": "gpsimd", "sp": "sync"}
        by_eng = {e: [o for o in ops if o.eng == e] for e in ENGINES}
        self.stats = {e: len(by_eng[e]) for e in ENGINES}
        self.stats["waits"] = sum(len(w) for w in waits)
        with nc.Block() as block:
            for e in ENGINES:
                lst = by_eng[e]
                if not lst:
                    continue

                def body(engine, lst=lst, e=e):
                    for o in lst:
                        for semh, val in waits[o.idx]:
                            engine.wait_ge(semh, val)
                        ins = o.fn(engine)
                        if os.environ.get("KDBG"):
                            try:
                                self.names[ins.ins.name] = o.line
                            except Exception:
                                self.names[str(ins)[:40]] = o.line
                        if o.is_dma:
                            ins.then_inc(o.chan.sem, 16)
                        elif o.signal:
                            ins.then_inc(sems[e][(o.seq - 1) // EPOCH], 1)
                getattr(block, engmap[e])(body)
        for cm in reversed(sem_ctx):
            cm.__exit__(None, None, None)
        for cm in reversed(self._ctx):
            cm.__exit__(None, None, None)


OFF = dict(cq=0, ckv=512, kr=768, rq=800, rk=1056, rv=1312, rg=1824, gq=2336, gk=3360, gv=3616,
           ga=3872, gb=5920, gc=7968)
FM_TILES = []
FM_TILES.append(("kr", 0, OFF["kr"], 32))
for i in range(4):
    FM_TILES.append(("cq", i, OFF["cq"] + 128 * i, 128))
for i in range(2):
    FM_TILES.append(("ckv", i, OFF["ckv"] + 128 * i, 128))
for i in range(4):
    FM_TILES.append(("rq", i, OFF["rq"] + 64 * i, 64))
for i in range(4):
    FM_TILES.append(("rk", i, OFF["rk"] + 64 * i, 64))
for i in range(8):
    FM_TILES.append(("rg", i, OFF["rg"] + 64 * i, 64))
for i in range(8):
    FM_TILES.append(("gq", i, OFF["gq"] + 128 * i, 128))
for i in range(2):
    FM_TILES.append(("gk", i, OFF["gk"] + 128 * i, 128))
for i in range(48):
    FM_TILES.append(("gate", i, OFF["ga"] + 128 * i, 128))
NFM = len(FM_TILES)


def rope_tables(r, nblk, scale, nrows_pad=None, row0=0):
    ra, nf = r // 2, r // 4
    inv = 10000.0 ** (-np.arange(nf, dtype=np.float64) / nf)
    tl = np.arange(SEQ)
    row, col = tl // GRID_W, tl % GRID_W
    rows = nblk * r
    tot = nrows_pad or rows
    C = np.full((tot, T), scale, np.float64)
    S = np.zeros((tot, T), np.float64)
    P = np.zeros((tot, tot), np.float32)
    for b in range(nblk):
        for d in range(r):
            g = row0 + b * r + d
            half = d // ra
            dd = d % ra
            pos = row if half == 0 else col
            f = inv[dd % nf]
            C[g, CTX:] = scale * np.cos(pos * f)
            if dd < nf:
                S[g, CTX:] = -scale * np.sin(pos * f)
                P[g + nf, g] = 1.0
            else:
                S[g, CTX:] = scale * np.sin(pos * f)
                P[g - nf, g] = 1.0
    return C.astype(np.float32), S.astype(np.float32), P


_CONST = None


def host_consts():
    global _CONST
    if _CONST is not None:
        return _CONST
    c = {}
    Cq, Sq, Pq = rope_tables(32, 1, MLA_SCALE, nrows_pad=96, row0=64)
    Ckr, Skr, Pkr = rope_tables(32, 1, 1.0)
    Crq, Srq, Prr = rope_tables(32, 4, 1.0)
    Crk, Srk, _ = rope_tables(32, 4, RET_K_SCALE)
    Cgq, Sgq, Pg = rope_tables(128, 1, GQA_SCALE)
    Cgk, Sgk, _ = rope_tables(128, 1, 1.0)
    tab = np.zeros((12, 128, T), np.float32)
    for i, a in enumerate([Cq, Sq, Ckr, Skr, Crq, Srq, Crk, Srk, Cgq, Sgq, Cgk, Sgk]):
        tab[i, :a.shape[0]] = a
    c["ropetab"] = tab
    pm = np.zeros((4, 128, 128), np.float32)
    pm[0, :96, :96] = Pq
    pm[1, :32, :32] = Pkr
    pm[2] = Prr
    pm[3] = Pg
    c["permm"] = pm
    misc = np.zeros((128, 8, 128), np.float32)
    misc[:, 0, :] = np.eye(128)
    sh = np.zeros((128, 128), np.float32)
    for k in range(32):
        sh[k, 64 + k] = 1.0
    misc[:, 1, :] = sh
    i = np.arange(128)
    misc[:, 2, :] = np.maximum(i[None, :] - i[:, None], 0)
    misc[:, 3, :] = (i[None, :] >= i[:, None])
    misc[:, 4, :] = np.maximum(i[:, None] - i[None, :], 0)
    misc[:, 5, :] = (i[:, None] >= i[None, :])
    misc[:, 6, :] = (i[None, :] + 1.0)
    misc[:, 7, :] = (128.0 - i[None, :])
    c["misc"] = misc
    col = np.zeros((128, 4), np.float32)
    col[:, 0] = 127.0 - i
    col[:, 1] = i
    c["colc"] = col
    sel = np.zeros((128, 4, 8), np.float32)
    for p in range(64):
        for j in range(4):
            sel[p, j, j * 2 + p // 32] = 1.0
    c["hsel"] = sel
    _CONST = c
    return c


def fm(v):
    v = np.asarray(v, np.float32)
    return np.ascontiguousarray(v.reshape(-1, 128).T)


def prep_layer_weights(inp, l):
    w = {}
    w_in = inp["w_in"][l]
    wf = np.zeros((NFM, 128, NKC, 128), np.float32)
    for ti, (_, _, c0, nc_) in enumerate(FM_TILES):
        wf[ti, :, :, :nc_] = w_in[:, c0:c0 + nc_].reshape(NKC, 128, nc_).transpose(1, 0, 2)
    w["wf"] = wf
    w["wt_rv"] = np.ascontiguousarray(w_in[:, OFF["rv"]:OFF["rv"] + 512].reshape(NKC, 128, 512).transpose(1, 0, 2))
    w["wt_gv"] = np.ascontiguousarray(w_in[:, OFF["gv"]:OFF["gv"] + 256].reshape(NKC, 128, 256).transpose(1, 0, 2))
    w["wmod"] = np.ascontiguousarray(inp["w_mod"][l].reshape(NKC, 128, 96, 128).transpose(2, 1, 0, 3))
    w["bmod"] = fm(inp["b_mod"][l])
    w["wqup"] = np.ascontiguousarray(inp["mla_w_qup"][l].reshape(4, 128, 768).transpose(1, 0, 2))
    kv = inp["mla_w_kvup"][l].reshape(2, 128, 8, 128)
    wk = np.zeros((128, 2, 8, 96), np.float32)
    wk[:, :, :, :64] = kv[:, :, :, :64].transpose(1, 0, 2, 3)
    w["wkpad"] = wk
    w["wv"] = np.ascontiguousarray(kv[:, :, :, 64:].transpose(1, 0, 2, 3).reshape(128, 2, 512))
    nrm = np.zeros((128, 8), np.float32)
    nrm[:, 0:4] = fm(inp["mla_q_norm"][l])
    nrm[:, 4:6] = fm(inp["mla_kv_norm"][l])
    nrm[:, 6:7] = fm(inp["gqa_q_norm"][l])
    nrm[:, 7:8] = fm(inp["gqa_k_norm"][l])
    w["nrm"] = nrm
    w["decay"] = np.ascontiguousarray(inp["ret_decay_logit"][l].reshape(16))
    w["wbra"] = np.ascontiguousarray(inp["w_br_a"][l].reshape(8, 64, 16, 128).transpose(2, 1, 0, 3))
    w["wbrb"] = np.ascontiguousarray(inp["w_br_b"][l].reshape(8, 64, 16, 128).transpose(2, 1, 0, 3))
    w["wbrc"] = np.ascontiguousarray(inp["w_br_c"][l].reshape(8, 128, 16, 128).transpose(2, 1, 0, 3))
    w["wout"] = np.ascontiguousarray(inp["w_out"][l].reshape(16, 128, 16, 128).transpose(2, 1, 0, 3))
    ln = np.zeros((128, 4, 16), np.float32)
    ln[:, 0] = fm(inp["ln1_g"][l]); ln[:, 1] = fm(inp["ln1_b"][l])
    ln[:, 2] = fm(inp["ln2_g"][l]); ln[:, 3] = fm(inp["ln2_b"][l])
    w["ln"] = ln
    w["pwq"] = np.ascontiguousarray(inp["peer_w_q"][l].reshape(16, 128, 8, 128).transpose(2, 1, 0, 3))
    kk = np.zeros((128, 256), np.float32)
    kk[0:64, 0:128] = inp["peer_k1"][l].T
    kk[64:128, 128:256] = inp["peer_k2"][l].T
    w["pk"] = kk
    w["put"] = np.ascontiguousarray(inp["peer_u"][l].reshape(128, 128, 16, 128).transpose(0, 3, 2, 1))
    w["pvt"] = np.ascontiguousarray(inp["peer_v"][l].reshape(16, 8, 128, 16, 128).transpose(0, 3, 2, 1, 4))
    return w


WKEYS = ["wf", "wt_rv", "wt_gv", "wmod", "bmod", "wqup", "wkpad", "wv", "nrm", "decay", "wbra", "wbrb", "wbrc",
         "wout", "ln", "pwq", "pk", "put", "pvt"]
WSHAPES = dict(wf=[NFM, 128, NKC, 128], wt_rv=[128, NKC, 512], wt_gv=[128, NKC, 256], wmod=[96, 128, NKC, 128],
               bmod=[128, 96], wqup=[128, 4, 768], wkpad=[128, 2, 8, 96], wv=[128, 2, 512], nrm=[128, 8],
               decay=[16], wbra=[16, 64, 8, 128], wbrb=[16, 64, 8, 128], wbrc=[16, 128, 8, 128],
               wout=[16, 128, 16, 128], ln=[128, 4, 16], pwq=[8, 128, 16, 128], pk=[128, 256],
               put=[128, 128, 16, 128], pvt=[16, 16, 128, 8, 128])


def build_program(n_layers=DEPTH, debug=None):
    debug = debug or {}
    nc = bass.Bass("TRN2", target_bir_lowering=False)
    p = Prog(nc)
    p.dump = debug.get("dump", ()) if debug else ()
    EI = "ExternalInput"
    xT_in = p.dram("xT", [128, NKC, T], F32, EI)
    cc_in = p.dram("cc", [128, NKC, 2], F32, EI)
    ropetab = p.dram("ropetab", [12, 128, T], F32, EI)
    permm_in = p.dram("permm", [4, 128, 128], F32, EI)
    misc_in = p.dram("misc", [128, 8, 128], F32, EI)
    colc_in = p.dram("colc", [128, 4], F32, EI)
    hsel_in = p.dram("hsel", [128, 4, 8], F32, EI)
    Wd = [{k: p.dram(f"{k}{l}", WSHAPES[k], F32, EI) for k in WKEYS} for l in range(n_layers)]
    out = p.dram("out", [128, NKC, SEQ], F32, "ExternalOutput")

    XS = [p.dram(f"xs{i}", [128, NKC, T], F32) for i in range(2)]
    X1 = p.dram("x1", [128, NKC, T], F32)
    H2 = p.dram("h2", [128, NKC, T], BF16)
    QA = p.dram("qa", [8, 96, T], BF16)
    KA = p.dram("ka", [8, 96, T], BF16)
    VA = p.dram("va", [NCH, 128, 8 * 65], BF16)
    QB = p.dram("qb", [4, 64, T], BF16)
    KB = p.dram("kb", [4, 64, T], BF16)
    VB = p.dram("vb", [NCH, 128, 512], BF16)
    GB = p.dram("gb", [8, 64, T], BF16)
    QC = p.dram("qc", [8, 128, T], BF16)
    KC = p.dram("kc", [2, 128, T], BF16)
    VC = p.dram("vc", [NCH, 128, 256], BF16)
    GT = p.dram("gt", [48, 128, T], BF16)
    OA = p.dram("oa", [8, 64, T], BF16)
    OC = p.dram("oc", [8, 128, T], BF16)
    DR = p.dram("dr", [2, NCH, 32, 512], F32)
    PUTB = p.dram("putb", [128, 128, 2048], BF16)
    PVTB = p.dram("pvtb", [16, 16, 128, 1024], BF16)
    WFB = p.dram("wfbc", [NFM, 128, 2048], BF16)
    RS = p.dram("rs", [2, NCH, 32, 512], BF16)
    dbg = {}

    ps = [p.psum(f"ps{i}", [128, 512], F32) for i in range(8)]

    def psb(i):
        return ps[i][:, 0:256].bitcast(BF16)

    ident = p.sbuf("ident", [128, 128], BF16)
    identf = p.sbuf("identf", [128, 128], F32)
    shiftm = p.sbuf("shiftm", [128, 128], BF16)
    permm = p.sbuf("permm_sb", [128, 4, 128], BF16)
    ones_bf = p.sbuf("ones_bf", [128, 128], BF16)
    ones_f = p.sbuf("ones_f", [128, 128], F32)
    misc = p.sbuf("misc_sb", [128, 8, 128], F32)
    colc = p.sbuf("colc_sb", [128, 4], F32)
    hsel = p.sbuf("hsel_sb", [128, 4, 8], F32)
    silu_c = p.sbuf("silu_c", [128, NKC, 2], F32)
    epsc = p.sbuf("epsc", [128, 4], F32)
    p.dma("sp", misc[:], misc_in[:], [misc_in], [misc], "c_misc")
    p.dma("sp", colc[:], colc_in[:], [colc_in], [colc], "c_misc")
    p.dma("sp", hsel[:], hsel_in[:], [hsel_in], [hsel], "c_misc")
    p.dma("sp", silu_c[:], cc_in[:], [cc_in], [silu_c], "c_misc")
    p.dma("pool", permm[:], permm_in[:].rearrange("a p c -> p a c"), [permm_in], [permm], "c_perm")
    p.cp("dve", ident[:], misc[:, 0, :], [misc], [ident])
    p.cp("dve", identf[:], misc[:, 0, :], [misc], [identf])
    p.cp("dve", shiftm[:], misc[:, 1, :], [misc], [shiftm])
    p.memset("dve", ones_bf[:], 1.0, [ones_bf])
    p.memset("dve", epsc[:, 0:1], RMS_EPS, [epsc])
    p.memset("dve", epsc[:, 1:2], NORM_EPS, [epsc])
    p.memset("dve", epsc[:, 2:3], 1.0, [epsc])
    p.memset("dve", epsc[:, 3:4], 0.0, [epsc])
    p.memset("dve", ones_f[:], 1.0, [ones_f])
    p.act(silu_c[:], silu_c[:], AF.Silu, [silu_c], [silu_c])
    p.persist()
    base0 = p.arena_off

    for l in range(n_layers):
        W = Wd[l]
        XT = xT_in if l == 0 else XS[(l - 1) % 2]
        XO = XS[l % 2]
        last = l == n_layers - 1
        p.reset_to(base0)
        modv = p.sbuf("modv", [128, 96, 2], F32)
        bmod = p.sbuf("bmod", [128, 96], F32)
        nrm = p.sbuf("nrm", [128, 8], F32)
        lnp = p.sbuf("lnp", [128, 4, 16], F32)
        lg = p.sbuf("lg", [128, 16], F32)
        p.dma("sp", bmod[:], W["bmod"][:], [W["bmod"]], [bmod], "c_misc")
        p.dma("sp", nrm[:], W["nrm"][:], [W["nrm"]], [nrm], "c_misc")
        p.dma("sp", lnp[:], W["ln"][:], [W["ln"]], [lnp], "c_misc")
        p.dma("sp", lg[:], W["decay"][:].partition_broadcast(128), [W["decay"]], [lg], "c_misc")
        p.act(lg[:], lg[:], AF.Exp, [lg], [lg], scale=-1.0)
        p.act(lg[:], lg[:], AF.Ln, [lg, epsc], [lg], bias=epsc[:, 2:3])
        p.ts("dve", lg[:], lg[:], -1.0, None, ALU.mult, None, [lg], [lg])
        layer_base = p.arena_off
        p.arena_base = layer_base
        wmt = [p.sbuf(f"wmt{i}", [128, NKC, 128], F32) for i in range(3)]
        for j in range(96):
            wt = wmt[j % 3]
            p.dma("sp" if j % 2 == 0 else "act", wt[:], W["wmod"][j], [W["wmod"]], [wt], f"wmt{j % 3}")
            bank = ps[j % 2]
            for kc in range(NKC):
                p.mm(bank[:, 0:2], wt[:, kc, :], silu_c[:, kc, :], kc == 0, kc == NKC - 1, [wt, silu_c], [bank])
            p.tt("dve", modv[:, j, :], bank[:, 0:2], bmod[:, j:j + 1].to_broadcast([128, 2]), ALU.add,
                 [bank, bmod], [(modv, j)])
        for s in (1, 4):
            p.ts("dve", modv[:, 16 * s:16 * s + 16, :], modv[:, 16 * s:16 * s + 16, :], 1.0, None, ALU.add, None,
                 [modv], [modv])
        p.reset()

        def mod(kind, c, isctx):
            j = dict(sh1=0, sc1=1, g1=2, sh2=3, sc2=4, g2=5)[kind] * 16 + c
            return modv[:, j, (1 if isctx else 0):(2 if isctx else 1)]

        wq_sb = p.sbuf("wqup_sb", [128, 4, 768], BF16)
        wk_sb = p.sbuf("wkpad_sb", [128, 2, 8, 96], BF16)
        wv_sb = p.sbuf("wv_sb", [128, 2, 512], BF16)
        wrv_sb = p.sbuf("wrv_sb", [128, NKC, 512], BF16)
        wgv_sb = p.sbuf("wgv_sb", [128, NKC, 256], BF16)
        zf = p.sbuf("zf", [128, 8], F32)
        zb = p.sbuf("zb", [128, 8], F32)
        p.dma("pool", wq_sb[:], W["wqup"][:], [W["wqup"]], [wq_sb], "a_w0")
        p.dma("pool", wk_sb[:], W["wkpad"][:], [W["wkpad"]], [wk_sb], "a_w0")
        p.dma("pool", wv_sb[:], W["wv"][:], [W["wv"]], [wv_sb], "a_w0")
        p.dma("pool", wrv_sb[:], W["wt_rv"][:], [W["wt_rv"]], [wrv_sb], "a_w0")
        p.dma("pool", wgv_sb[:], W["wt_gv"][:], [W["wt_gv"]], [wgv_sb], "a_w0")
        p.ts("dve", zf[:], lg[:, 0:8], colc[:, 0:1], None, ALU.mult, None, [lg, colc], [zf])
        p.ts("dve", zb[:], lg[:, 8:16], colc[:, 1:2], None, ALU.mult, None, [lg, colc], [zb])
        p.act(zf[:], zf[:], AF.Exp, [zf], [zf])
        p.act(zb[:], zb[:], AF.Exp, [zb], [zb])
        xg = p.sbuf("xg", [128, NKC, 512], F32)
        hT = p.sbuf("hT", [128, NKC, 512], BF16)
        wfb = [p.sbuf(f"wfb{i}", [128, NKC, 128], BF16) for i in range(3)]
        tabs = [p.sbuf(f"tab{i}", [128, 2, 512], F32) for i in range(2)]
        raw = [p.sbuf(f"raw{i}", [128, 512], F32) for i in range(2)]
        rawb = [p.sbuf(f"rawb{i}", [128, 512], BF16) for i in range(2)]
        sqb = [p.sbuf(f"sqb{i}", [128, 512], BF16) for i in range(2)]
        cqT = p.sbuf("cqT", [128, 4, 512], F32)
        cqn = p.sbuf("cqn", [128, 4, 512], BF16)
        ckvn = p.sbuf("ckvn", [128, 2, 512], BF16)
        krr = p.sbuf("krr", [32, 512], BF16)
        rstd = p.sbuf("rstd", [128, 512], F32)
        t1 = [p.sbuf(f"t1_{i}", [128, 512], F32) for i in range(2)]
        t2 = [p.sbuf(f"t2_{i}", [128, 512], F32) for i in range(2)]
        outb = [p.sbuf(f"outb{i}", [128, 512], BF16) for i in range(3)]
        vaext = [p.sbuf(f"vaext{i}", [128, 8, 65], BF16) for i in range(2)]
        vtm = [p.sbuf(f"vtm{i}", [128, 512], BF16) for i in range(2)]
        kz = [p.sbuf(f"kz{i}", [128, 2, 256], BF16) for i in range(2)]
        kbT = p.sbuf("kbT", [64, 4, 512], BF16)
        drs = [p.sbuf(f"drs{i}", [32, 2, 512], F32) for i in range(2)]
        for i in range(2):
            p.memset("pool", vaext[i][:], 1.0, [vaext[i]])
        cnt = {"o": 0, "r": 0, "t": 0}

        def rope(xT_, xap, nrow, tabidx, permidx, g0, N, bank_r):
            i = cnt["r"] % 2
            cnt["r"] += 1
            tb = tabs[i]
            p.dma("sp", tb[0:nrow, :, 0:N], ropetab[tabidx:tabidx + 2, 0:nrow, g0:g0 + N].rearrange("a p n -> p a n"),
                  [ropetab], [tb], f"tab{i}")
            br = bank_r
            p.mm(br[0:nrow, 0:N], permm[0:nrow, permidx, 0:nrow], xap, True, True, [permm, xT_], [br])
            a, b = t1[i], t2[i]
            p.tt("dve", a[0:nrow, 0:N], xap, tb[0:nrow, 0, 0:N], ALU.mult, [xT_, tb], [a])
            p.tt("dve", b[0:nrow, 0:N], br[0:nrow, 0:N], tb[0:nrow, 1, 0:N], ALU.mult, [br, tb], [b])
            j = cnt["o"] % 3
            cnt["o"] += 1
            ob = outb[j]
            p.tt("pool", ob[0:nrow, 0:N], a[0:nrow, 0:N], b[0:nrow, 0:N], ALU.add, [a, b], [ob])
            return ob, j

        def to_bf(bank, nrow, N):
            i = cnt["t"] % 2
            cnt["t"] += 1
            xb = rawb[i]
            p.cp("act", xb[0:nrow, 0:N], bank[0:nrow, 0:N], [bank], [xb])
            return xb

        def rms_bcast(src_list, nfeat, N, bank):
            for k, (sT, sap) in enumerate(src_list):
                sq = sqb[k % 2]
                p.act(sq[:, 0:N], sap, AF.Square, [sT], [sq])
                p.mm(bank[:, 0:N], ones_bf[:], sq[:, 0:N], k == 0, k == len(src_list) - 1, [ones_bf, sq], [bank])
            p.act(rstd[:, 0:N], bank[:, 0:N], AF.Sqrt, [bank, epsc], [rstd], bias=epsc[:, 0:1], scale=1.0 / nfeat)
            p.recip(rstd[:, 0:N], rstd[:, 0:N], [rstd], [rstd])

        wf_cached = set()
        for gi, (g0, N) in enumerate(GROUPS):
            isctx = gi == 0
            nsub = N // 128
            for c in range(NKC):
                p.dma("sp" if c % 2 == 0 else "act", xg[:, c, 0:N], XT[:, c, g0:g0 + N], [XT], [(xg, c)], "xg")
            for c in range(NKC):
                p.ts("dve", hT[:, c, 0:N], xg[:, c, 0:N], mod("sc1", c, isctx), mod("sh1", c, isctx),
                     ALU.mult, ALU.add, [(xg, c), modv], [(hT, c)])
            for ti, (kind, idx, c0, ncol) in enumerate(FM_TILES):
                if last and isctx and kind in ("cq", "rq", "rg", "gq", "gate"):
                    continue
                wt = wfb[ti % 3]
                wtf = wt[:].rearrange("p a b -> p (a b)")
                if ti not in wf_cached:
                    p.dma("pool", wt[:], W["wf"][ti], [W["wf"]], [wt], f"wfb{ti % 3}")
                    p.dma("act", WFB[ti], wtf, [wt], [(WFB, ti)], f"wfb{ti % 3}")
                    wf_cached.add(ti)
                else:
                    p.dma("sp", wtf, WFB[ti], [(WFB, ti)], [wt], f"wfb{ti % 3}")
                bank = ps[ti % 2]
                for kc in range(NKC):
                    p.mm(bank[0:ncol, 0:N], wt[:, kc, 0:ncol], hT[:, kc, 0:N], kc == 0, kc == NKC - 1, [wt, (hT, kc)], [bank])
                if kind == "kr":
                    xb = to_bf(bank, 32, N)
                    ob, j = rope(xb, xb[0:32, 0:N], 32, 2, 1, g0, N, ps[2])
                    p.cp("dve", krr[:, 0:N], ob[0:32, 0:N], [ob], [krr])
                elif kind == "cq":
                    p.cp("act", cqT[:, idx, 0:N], bank[:, 0:N], [bank], [(cqT, idx)])
                    if idx == 3:
                        rms_bcast([((cqT, k), cqT[:, k, 0:N]) for k in range(4)], 512, N, ps[2])
                        for k in range(4):
                            p.stt(cqn[:, k, 0:N], cqT[:, k, 0:N], nrm[:, k:k + 1], rstd[:, 0:N], ALU.mult, ALU.mult,
                                  [(cqT, k), nrm, rstd], [(cqn, k)])
                        for h in range(8):
                            b2 = ps[3 + h % 2]
                            for k in range(4):
                                p.mm(b2[0:96, 0:N], wq_sb[:, k, h * 96:(h + 1) * 96], cqn[:, k, 0:N], k == 0, k == 3,
                                     [wq_sb, (cqn, k)], [b2])
                            xb = to_bf(b2, 96, N)
                            ob, j = rope(xb, xb[0:96, 0:N], 96, 0, 0, g0, N, ps[5 + h % 2])
                            p.dma("sp", QA[h, :, g0:g0 + N], ob[0:96, 0:N], [ob], [QA], f"outb{j}")
                elif kind == "ckv":
                    p.cp("act", cqT[:, idx, 0:N], bank[:, 0:N], [bank], [(cqT, idx)])
                    if idx == 1:
                        rms_bcast([((cqT, k), cqT[:, k, 0:N]) for k in range(2)], 256, N, ps[2])
                        for k in range(2):
                            p.stt(ckvn[:, k, 0:N], cqT[:, k, 0:N], nrm[:, 4 + k:5 + k], rstd[:, 0:N], ALU.mult, ALU.mult,
                                  [(cqT, k), nrm, rstd], [(ckvn, k)])
                        for h in range(8):
                            b2 = ps[3 + h % 2]
                            for k in range(2):
                                p.mm(b2[0:96, 0:N], wk_sb[:, k, h, :], ckvn[:, k, 0:N], k == 0, False, [wk_sb, (ckvn, k)], [b2])
                            p.mm(b2[0:96, 0:N], shiftm[0:32, 0:96], krr[:, 0:N], False, True, [shiftm, krr], [b2])
                            j = cnt["o"] % 3
                            cnt["o"] += 1
                            ob = outb[j]
                            p.cp("act", ob[0:96, 0:N], b2[0:96, 0:N], [b2], [ob])
                            p.dma("sp", KA[h, :, g0:g0 + N], ob[0:96, 0:N], [ob], [KA], f"outb{j}")
                        for s in range(nsub):
                            b2 = ps[5 + s % 2]
                            for k in range(2):
                                p.mm(b2[:, :], ckvn[:, k, s * 128:(s + 1) * 128], wv_sb[:, k, :], k == 0, k == 1,
                                     [(ckvn, k), wv_sb], [b2])
                            ve = vaext[s % 2]
                            p.cp("act", ve[:, :, 0:64], b2[:, :].rearrange("p (h v) -> p h v", h=8), [b2], [ve])
                            p.dma("sp", VA[g0 // 128 + s], ve[:].rearrange("p h v -> p (h v)"), [ve], [VA], f"vaext{s % 2}")
                elif kind == "rq":
                    xb = to_bf(bank, 64, N)
                    ob, j = rope(xb, xb[0:64, 0:N], 64, 4, 2, g0, N, ps[2])
                    p.dma("sp", QB[idx, :, g0:g0 + N], ob[0:64, 0:N], [ob], [QB], f"outb{j}")
                elif kind == "rk":
                    xb = to_bf(bank, 64, N)
                    ob, j = rope(xb, xb[0:64, 0:N], 64, 6, 2, g0, N, ps[2])
                    p.dma("sp", KB[idx, :, g0:g0 + N], ob[0:64, 0:N], [ob], [KB], f"outb{j}")
                    p.cp("pool", kbT[:, idx, 0:N], ob[0:64, 0:N], [ob], [(kbT, idx)])
                elif kind == "rg":
                    j = cnt["o"] % 3
                    cnt["o"] += 1
                    ob = outb[j]
                    p.act(ob[0:64, 0:N], bank[0:64, 0:N], AF.Silu, [bank], [ob])
                    p.dma("sp", GB[idx, :, g0:g0 + N], ob[0:64, 0:N], [ob], [GB], f"outb{j}")
                elif kind in ("gq", "gk"):
                    i = cnt["t"] % 2
                    cnt["t"] += 1
                    rw = raw[i]
                    p.cp("act", rw[:, 0:N], bank[:, 0:N], [bank], [rw])
                    rms_bcast([(rw, rw[:, 0:N])], 128, N, ps[2])
                    gcol = 6 if kind == "gq" else 7
                    qn = rawb[i]
                    p.stt(qn[:, 0:N], rw[:, 0:N], nrm[:, gcol:gcol + 1], rstd[:, 0:N], ALU.mult, ALU.mult, [rw, nrm, rstd], [qn])
                    dstT = QC if kind == "gq" else KC
                    ob, j = rope(qn, qn[:, 0:N], 128, 8 if kind == "gq" else 10, 3, g0, N, ps[3])
                    p.dma("sp", dstT[idx, :, g0:g0 + N], ob[:, 0:N], [ob], [dstT], f"outb{j}")
                elif kind == "gate":
                    j = cnt["o"] % 3
                    cnt["o"] += 1
                    ob = outb[j]
                    p.act(ob[:, 0:N], bank[:, 0:N], AF.Sigmoid, [bank], [ob])
                    p.dma("sp", GT[idx, :, g0:g0 + N], ob[:, 0:N], [ob], [GT], f"outb{j}")
            for s in range(nsub):
                ch = g0 // 128 + s
                b2 = ps[4 + s % 2]
                for kc in range(NKC):
                    p.mm(b2[:, :], hT[:, kc, s * 128:(s + 1) * 128], wrv_sb[:, kc, :], kc == 0, kc == NKC - 1,
                         [(hT, kc), wrv_sb], [b2])
                vt = vtm[s % 2]
                p.cp("act", vt[:], b2[:, :], [b2], [vt])
                p.dma("sp", VB[ch], vt[:], [vt], [VB], f"vtm{s % 2}")
                b3 = ps[6]
                for kc in range(NKC):
                    p.mm(b3[:, 0:256], hT[:, kc, s * 128:(s + 1) * 128], wgv_sb[:, kc, :], kc == 0, kc == NKC - 1,
                         [(hT, kc), wgv_sb], [b3])
                j = cnt["o"] % 3
                cnt["o"] += 1
                ob = outb[j]
                p.cp("act", ob[:, 0:256], b3[:, 0:256], [b3], [ob])
                p.dma("sp", VC[ch], ob[:, 0:256], [ob], [VC], f"outb{j}")
                b4 = ps[7]
                b4b = psb(7)
                for j2 in range(4):
                    p.tr(b4b[:, j2 * 64:(j2 + 1) * 64], kbT[:, j2, s * 128:(s + 1) * 128], ident[0:64, 0:64], [(kbT, j2), ident], [b4])
                kzt = kz[s % 2]
                p.tt("dve", kzt[:, 0, :].rearrange("p (h d) -> p h d", h=8), b4b[:, 0:256].rearrange("p (h d) -> p h d", h=8),
                     zf[:].unsqueeze(2).to_broadcast([128, 8, 32]), ALU.mult, [b4, zf], [(kzt, 0)])
                p.tt("dve", kzt[:, 1, :].rearrange("p (h d) -> p h d", h=8), b4b[:, 0:256].rearrange("p (h d) -> p h d", h=8),
                     zb[:].unsqueeze(2).to_broadcast([128, 8, 32]), ALU.mult, [b4, zb], [(kzt, 1)])
                dd = drs[s % 2]
                for dr_ in range(2):
                    b5 = ps[2 + dr_]
                    for h in range(8):
                        p.mm(b5[0:32, h * 64:(h + 1) * 64], kzt[:, dr_, h * 32:(h + 1) * 32], vt[:, h * 64:(h + 1) * 64],
                             True, True, [(kzt, dr_), vt], [b5])
                    p.cp("act" if dr_ == 0 else "dve", dd[:, dr_, :], b5[0:32, :], [b5], [(dd, dr_)])
                    p.dma("sp", DR[dr_, ch], dd[:, dr_, :], [(dd, dr_)], [DR], f"drs{s % 2}")
        p.reset()
        if "A" in debug and l == debug["A"]:
            break

        gtab = p.sbuf("gtab", [32, 2, 8], F32)
        p.act(gtab[:].rearrange("p a h -> p (a h)"), lg[0:32, :], AF.Exp, [lg], [gtab], scale=128.0)
        st = p.sbuf("st", [32, 512], F32)
        stb = [p.sbuf(f"stb{i}", [32, 512], BF16) for i in range(2)]
        dl = [p.sbuf(f"dl{i}", [32, 512], F32) for i in range(2)]
        zero = p.sbuf("zero32", [32, 512], BF16)
        p.memset("dve", zero[:], 0.0, [zero])
        sc = {"n": 0}

        def st_store(dr_, ch, src=None):
            i = sc["n"] % 2
            sc["n"] += 1
            if src is None:
                p.cp("act", stb[i][:], st[:], [st], [stb[i]])
                src = stb[i]
                p.dma("sp", RS[dr_, ch], src[:], [src], [RS], f"stb{i}")
            else:
                p.dma("sp", RS[dr_, ch], src[:], [src], [RS], "zero32")

        def st_load(dr_, ch, into_state):
            i = sc["n"] % 2
            d_ = dl[i]
            p.dma("act", d_[:], DR[dr_, ch], [DR], [d_], f"dl{i}")
            return d_

        def st_step(dr_, ch, first=False):
            d_ = st_load(dr_, ch, False)
            if first:
                p.cp("dve", st[:], d_[:], [d_], [st])
            else:
                g3 = gtab[:, dr_, :].unsqueeze(2).to_broadcast([32, 8, 64])
                p.tt("dve", st[:].rearrange("p (h v) -> p h v", h=8), st[:].rearrange("p (h v) -> p h v", h=8), g3, ALU.mult,
                     [st, gtab], [st])
                p.tt("dve", st[:], st[:], d_[:], ALU.add, [st, d_], [st])

        st_store(0, 0, zero)
        st_step(0, 0, first=True)
        st_store(0, 1)
        st_step(0, 1)
        for ch in range(2, NCH):
            st_store(0, ch)
            if ch < NCH - 1:
                st_step(0, ch)
        st_store(1, 1, zero)
        st_step(1, 1, first=True)
        st_store(1, 0)
        st_step(1, 0)
        for ch in range(NCH - 1, 1, -1):
            st_store(1, ch)
            if ch > 2:
                st_step(1, ch)
        p.reset()

        ka = p.sbuf("ka_sb", [96, 8, T], BF16)
        va = p.sbuf("va_sb", [128, NCH, 520], BF16)
        for h in range(8):
            p.dma("sp" if h % 2 == 0 else "act", ka[:, h, :], KA[h], [KA], [(ka, h)], "b_kv")
        for c4 in range(0, NCH, 2):
            p.dma("sp" if c4 % 4 == 0 else "act", va[:, c4:c4 + 2, :], VA[c4:c4 + 2].rearrange("c p f -> p c f"), [VA], [(va, c4)], "b_kv")
        qsb = [p.sbuf(f"qsb{i}", [128, 512], BF16) for i in range(2)]
        pT = [p.sbuf(f"pT{i}", [128, 512], BF16) for i in range(3)]
        rden = p.sbuf("rden", [128, 512], F32)
        accs = p.sbuf("accs", [128, 512], F32)
        oo = [p.sbuf(f"oo{i}", [128, 512], BF16) for i in range(2)]
        for gi, (g0, N) in enumerate(GROUPS):
            isctx = gi == 0
            if last and isctx:
                continue
            kt_list = list(range(2)) if isctx else list(range(NCH))
            for h in range(8):
                q_ = qsb[h % 2]
                p.dma("sp", q_[0:96, 0:N], QA[h, :, g0:g0 + N], [QA], [q_], f"qsb{h % 2}")
                acc = ps[4 + h % 2]
                den = ps[7] if h % 2 == 0 else ps[3]
                nk = len(kt_list)
                LOOK = 2
                for step in range(nk + LOOK):
                    if step < nk:
                        ki, kt = step, kt_list[step]
                        sb_ = ps[ki % 3]
                        p.mm(sb_[:, 0:N], ka[:, h, kt * 128:(kt + 1) * 128], q_[0:96, 0:N], True, True, [ka, q_], [sb_])
                        pt = pT[ki % 3]
                        p.act(pt[:, 0:N], sb_[:, 0:N], AF.Exp, [sb_], [pt])
                    if step >= LOOK:
                        ki = step - LOOK
                        kt = kt_list[ki]
                        pt = pT[ki % 3]
                        fl = (ki == 0, ki == nk - 1)
                        p.mm(acc[0:64, 0:N], va[:, kt, h * 65:h * 65 + 64], pt[:, 0:N], fl[0], fl[1], [va, pt], [acc])
                        p.mm(den[0:1, 0:N], ones_bf[:, 0:1], pt[:, 0:N], fl[0], fl[1], [ones_bf, pt], [den])
                p.recip(rden[0:1, 0:N], den[0:1, 0:N], [den], [rden])
                p.cp("act", accs[0:64, 0:N], acc[0:64, 0:N], [acc], [accs])
                p.mm(ps[6][0:64, 0:N], ones_f[0:1, 0:64], rden[0:1, 0:N], True, True, [ones_f, rden], [ps[6]])
                o_ = oo[h % 2]
                p.tt("dve", o_[0:64, 0:N], accs[0:64, 0:N], ps[6][0:64, 0:N], ALU.mult, [accs, ps[6]], [o_])
                p.dma("pool", OA[h, :, g0:g0 + N], o_[0:64, 0:N], [o_], [OA], f"oo{h % 2}")
        p.reset_to(layer_base)
        if "B1" in debug and l == debug["B1"]:
            break
        kcs = p.sbuf("kc_sb", [128, 2, T], BF16)
        vcs = p.sbuf("vc_sb", [128, NCH, 256], BF16)
        for c4 in range(0, NCH, 2):
            p.dma("act", vcs[:, c4:c4 + 2, :], VC[c4:c4 + 2].rearrange("c p f -> p c f"), [VC], [(vcs, c4)], "b_kv")
        for g in range(2):
            p.dma("sp", kcs[:, g, :], KC[g], [KC], [(kcs, g)], "b_kv")
        qsb = [p.sbuf(f"qsb{i}", [128, 512], BF16) for i in range(2)]
        pT = [p.sbuf(f"pT{i}", [128, 512], BF16) for i in range(3)]
        rden = p.sbuf("rden", [128, 512], F32)
        accs = p.sbuf("accs", [128, 512], F32)
        oo = [p.sbuf(f"oo{i}", [128, 512], BF16) for i in range(2)]
        for gi, (g0, N) in enumerate(GROUPS):
            isctx = gi == 0
            if last and isctx:
                continue
            kt_list = list(range(2)) if isctx else list(range(NCH))
            for h in range(8):
                g = h // 4
                q_ = qsb[h % 2]
                p.dma("sp", q_[:, 0:N], QC[h, :, g0:g0 + N], [QC], [q_], f"qsb{h % 2}")
                acc = ps[4 + h % 2]
                den = ps[7] if h % 2 == 0 else ps[3]
                nk = len(kt_list)
                LOOK = 2
                for step in range(nk + LOOK):
                    if step < nk:
                        ki, kt = step, kt_list[step]
                        sb_ = ps[ki % 3]
                        p.mm(sb_[:, 0:N], kcs[:, g, kt * 128:(kt + 1) * 128], q_[:, 0:N], True, True, [kcs, q_], [sb_])
                        pt = pT[ki % 3]
                        p.act(pt[:, 0:N], sb_[:, 0:N], AF.Exp, [sb_], [pt])
                    if step >= LOOK:
                        ki = step - LOOK
                        kt = kt_list[ki]
                        pt = pT[ki % 3]
                        fl = (ki == 0, ki == nk - 1)
                        p.mm(acc[:, 0:N], vcs[:, kt, g * 128:(g + 1) * 128], pt[:, 0:N], fl[0], fl[1], [vcs, pt], [acc])
                        p.mm(den[0:1, 0:N], ones_bf[:, 0:1], pt[:, 0:N], fl[0], fl[1], [ones_bf, pt], [den])
                p.recip(rden[0:1, 0:N], den[0:1, 0:N], [den], [rden])
                p.cp("act", accs[:, 0:N], acc[:, 0:N], [acc], [accs])
                p.mm(ps[6][:, 0:N], ones_f[0:1, :], rden[0:1, 0:N], True, True, [ones_f, rden], [ps[6]])
                o_ = oo[h % 2]
                p.tt("dve", o_[:, 0:N], accs[:, 0:N], ps[6][:, 0:N], ALU.mult, [accs, ps[6]], [o_])
                p.dma("pool", OC[h, :, g0:g0 + N], o_[:, 0:N], [o_], [OC], f"oo{h % 2}")
        p.reset_to(layer_base)
        if "B2" in debug and l == debug["B2"]:
            break
        lgs = p.sbuf("lgs", [128, 2, 4], F32)
        tmp8 = p.sbuf("tmp8", [128, 4, 8], F32)
        for dr_ in range(2):
            p.tt("dve", tmp8[:], hsel[:], lg[:, dr_ * 8:dr_ * 8 + 8].unsqueeze(1).to_broadcast([128, 4, 8]), ALU.mult,
                 [hsel, lg], [tmp8])
            p.rsum(lgs[:, dr_, :], tmp8[:], [tmp8], [lgs])
        xi = p.sbuf("xi", [128, 2, 4, 128], BF16)
        xif = p.sbuf("xif", [128, 128], F32)
        for dr_ in range(2):
            for j in range(4):
                p.act(xif[:], misc[:, 6 + dr_, :], AF.Exp, [misc, lgs], [xif], scale=lgs[:, dr_, j:j + 1])
                p.cp("dve", xi[:, dr_, j, :], xif[:], [xif], [xi])
        mcomb = p.sbuf("mcomb", [128, 8, 128], F32)
        mtmp = p.sbuf("mtmp", [128, 128], F32)
        for h in range(8):
            p.act(mcomb[:, h, :], misc[:, 2, :], AF.Exp, [misc, lg], [(mcomb, h)], scale=lg[:, h:h + 1])
            p.tt("dve", mcomb[:, h, :], mcomb[:, h, :], misc[:, 3, :], ALU.mult, [(mcomb, h), misc], [(mcomb, h)])
            p.act(mtmp[:], misc[:, 4, :], AF.Exp, [misc, lg], [mtmp], scale=lg[:, 8 + h:9 + h])
            p.tt("dve", mtmp[:], mtmp[:], misc[:, 5, :], ALU.mult, [mtmp, misc], [mtmp])
            p.tt("dve", mcomb[:, h, :], mcomb[:, h, :], mtmp[:], ALU.add, [(mcomb, h), mtmp], [(mcomb, h)])
        ones64 = p.sbuf("ones64", [64, 64], F32)
        p.memset("dve", ones64[:], 1.0 / 64.0, [ones64])
        onesD = p.sbuf("onesD", [128, 128], F32)
        p.memset("dve", onesD[:], 1.0 / D, [onesD])
        oA = p.sbuf("oA", [64, 8, 512], BF16)
        oB = p.sbuf("oB", [64, 8, 512], BF16)
        oC = p.sbuf("oC", [128, 8, 512], BF16)
        qb_sb = p.sbuf("qb_sb", [64, 4, 512], BF16)
        kb_sb = p.sbuf("kb_sb", [64, 4, 512], BF16)
        vb_sb = p.sbuf("vb_sb", [128, 4, 512], BF16)
        qxi = p.sbuf("qxi", [64, 2, 4, 512], BF16)
        rsb = p.sbuf("rsb", [64, 2, 4, 4, 64], BF16)
        pr = [p.sbuf(f"pr{i}", [128, 128], BF16) for i in range(2)]
        ysb = p.sbuf("ysb", [64, 512], F32)
        ysq = p.sbuf("ysq", [64, 512], F32)
        mean = p.sbuf("mean", [128, 512], F32)
        var = p.sbuf("var", [128, 512], F32)
        gbs = [p.sbuf(f"gbs{i}", [64, 512], BF16) for i in range(2)]
        wa_sb = [p.sbuf(f"wa_sb{i}", [64, 8, 128], BF16) for i in range(2)]
        wb_sb = [p.sbuf(f"wb_sb{i}", [64, 8, 128], BF16) for i in range(2)]
        wc_sb = [p.sbuf(f"wc_sb{i}", [128, 8, 128], BF16) for i in range(2)]
        wo_sb = [p.sbuf(f"wo_sb{i}", [128, 16, 128], BF16) for i in range(2)]
        gts = [p.sbuf(f"gts{i}", [128, 3, 512], BF16) for i in range(2)]
        mT = p.sbuf("mT", [128, 16, 512], BF16)
        tT = p.sbuf("tT", [128, 16, 512], F32)
        xa = [p.sbuf(f"xa{i}", [128, 512], F32) for i in range(2)]
        u1 = [p.sbuf(f"u1_{i}", [128, 512], F32) for i in range(2)]
        u2 = [p.sbuf(f"u2_{i}", [128, 512], F32) for i in range(2)]
        sq32 = [p.sbuf(f"sq32_{i}", [128, 512], F32) for i in range(2)]
        x1o = [p.sbuf(f"x1o{i}", [128, 512], F32) for i in range(2)]
        h2o = [p.sbuf(f"h2o{i}", [128, 512], BF16) for i in range(2)]

        for gi, (g0, N) in enumerate(GROUPS):
            isctx = gi == 0
            if last and isctx:
                continue
            nsub = N // 128
            for h in range(8):
                p.dma("sp" if h % 2 == 0 else "act", oA[:, h, 0:N], OA[h, :, g0:g0 + N], [OA], [(oA, h)], "oA")
                p.dma("act" if h % 2 == 0 else "sp", oC[:, h, 0:N], OC[h, :, g0:g0 + N], [OC], [(oC, h)], "oC")
            for j in range(4):
                p.dma("sp", qb_sb[:, j, 0:N], QB[j, :, g0:g0 + N], [QB], [(qb_sb, j)], "qb_sb")
                p.dma("act", kb_sb[:, j, 0:N], KB[j, :, g0:g0 + N], [KB], [(kb_sb, j)], "kb_sb")
            p.dma("sp", vb_sb[:, 0:nsub, :], VB[g0 // 128:g0 // 128 + nsub].rearrange("c p f -> p c f"), [VB], [vb_sb], "vb_sb")
            for dr_ in range(2):
                for s in range(nsub):
                    for hh in range(2):
                        p.dma("act", rsb[hh * 32:(hh + 1) * 32, dr_, s, :, :],
                              RS[dr_, g0 // 128 + s].rearrange("d (j h v) -> h d j v", j=4, h=2)[hh], [RS], [(rsb, (dr_, s, hh))], "rsb")
                for j in range(4):
                    p.tt("pool", qxi[:, dr_, j, 0:N].rearrange("p (s i) -> p s i", i=128),
                         qb_sb[:, j, 0:N].rearrange("p (s i) -> p s i", i=128),
                         xi[0:64, dr_, j, :].unsqueeze(1).to_broadcast([64, nsub, 128]), ALU.mult, [qb_sb, xi], [qxi])
            for h in range(8):
                j, hh = h // 2, h % 2
                pb = slice(hh * 32, hh * 32 + 32)
                yb = ps[4 + h % 2]
                for s in range(nsub):
                    cs = slice(s * 128, (s + 1) * 128)
                    sb_ = ps[(h * 4 + s) % 3]
                    p.mm(sb_[:, 0:128], kb_sb[pb, j, cs], qb_sb[pb, j, cs], True, True, [kb_sb, qb_sb], [sb_])
                    pr_ = pr[(h * 4 + s) % 2]
                    p.tt("dve", pr_[:], sb_[:, 0:128], mcomb[:, h, :], ALU.mult, [sb_, (mcomb, h)], [pr_])
                    p.mm(yb[0:64, cs], vb_sb[:, s, h * 64:(h + 1) * 64], pr_[:], True, False, [vb_sb, pr_], [yb])
                    p.mm(yb[0:64, cs], rsb[pb, 0, s, j, :], qxi[pb, 0, j, cs], False, False, [rsb, qxi], [yb])
                    p.mm(yb[0:64, cs], rsb[pb, 1, s, j, :], qxi[pb, 1, j, cs], False, True, [rsb, qxi], [yb])
                p.cp("act", ysb[:, 0:N], yb[0:64, 0:N], [yb], [ysb])
                p.act(ysq[:, 0:N], yb[0:64, 0:N], AF.Square, [yb], [ysq])
                p.mm(ps[6][0:64, 0:N], ones64[:], ysb[:, 0:N], True, True, [ones64, ysb], [ps[6]])
                p.mm(ps[7][0:64, 0:N], ones64[:], ysq[:, 0:N], True, True, [ones64, ysq], [ps[7]])
                gb_ = gbs[h % 2]
                p.dma("sp", gb_[:, 0:N], GB[h, :, g0:g0 + N], [GB], [gb_], f"gbs{h % 2}")
                p.cp("act", mean[0:64, 0:N], ps[6][0:64, 0:N], [ps[6]], [mean])
                p.tt("dve", var[0:64, 0:N], mean[0:64, 0:N], mean[0:64, 0:N], ALU.mult, [mean], [var])
                p.tt("dve", var[0:64, 0:N], ps[7][0:64, 0:N], var[0:64, 0:N], ALU.subtract, [ps[7], var], [var])
                p.act(var[0:64, 0:N], var[0:64, 0:N], AF.Sqrt, [var, epsc], [var], bias=epsc[0:64, 1:2], scale=1.0)
                p.recip(var[0:64, 0:N], var[0:64, 0:N], [var], [var])
                p.tt("pool", ysb[:, 0:N], ysb[:, 0:N], mean[0:64, 0:N], ALU.subtract, [ysb, mean], [ysb])
                p.tt("dve", ysb[:, 0:N], ysb[:, 0:N], var[0:64, 0:N], ALU.mult, [ysb, var], [ysb])
                p.tt("dve", oB[:, h, 0:N], ysb[:, 0:N], gb_[:, 0:N], ALU.mult, [ysb, gb_], [(oB, h)])
            for ct in range(16):
                i = ct % 2
                p.dma("pool", wa_sb[i][:], W["wbra"][ct], [W["wbra"]], [wa_sb[i]], f"wa_sb{i}")
                p.dma("pool", wb_sb[i][:], W["wbrb"][ct], [W["wbrb"]], [wb_sb[i]], f"wb_sb{i}")
                p.dma("pool", wc_sb[i][:], W["wbrc"][ct], [W["wbrc"]], [wc_sb[i]], f"wc_sb{i}")
                for b3_ in range(3):
                    p.dma("act", gts[i][:, b3_, 0:N], GT[b3_ * 16 + ct, :, g0:g0 + N], [GT], [(gts[i], b3_)], f"gts{i}")
                for h in range(8):
                    p.mm(ps[0][:, 0:N], wa_sb[i][:, h, :], oA[:, h, 0:N], h == 0, h == 7, [wa_sb[i], (oA, h)], [ps[0]])
                for h in range(8):
                    p.mm(ps[1][:, 0:N], wb_sb[i][:, h, :], oB[:, h, 0:N], h == 0, h == 7, [wb_sb[i], (oB, h)], [ps[1]])
                for h in range(8):
                    p.mm(ps[2][:, 0:N], wc_sb[i][:, h, :], oC[:, h, 0:N], h == 0, h == 7, [wc_sb[i], (oC, h)], [ps[2]])
                a, b = u1[i], u2[i]
                p.tt("dve", a[:, 0:N], ps[0][:, 0:N], gts[i][:, 0, 0:N], ALU.mult, [ps[0], gts[i]], [a])
                p.tt("dve", b[:, 0:N], ps[1][:, 0:N], gts[i][:, 1, 0:N], ALU.mult, [ps[1], gts[i]], [b])
                p.tt("pool", a[:, 0:N], a[:, 0:N], b[:, 0:N], ALU.add, [a, b], [a])
                p.tt("dve", b[:, 0:N], ps[2][:, 0:N], gts[i][:, 2, 0:N], ALU.mult, [ps[2], gts[i]], [b])
                p.tt("pool", mT[:, ct, 0:N], a[:, 0:N], b[:, 0:N], ALU.add, [a, b], [(mT, ct)])
            for ct in range(16):
                i = ct % 2
                p.dma("pool", wo_sb[i][:], W["wout"][ct], [W["wout"]], [wo_sb[i]], f"wo_sb{i}")
                p.dma("sp", xa[i][:, 0:N], XT[:, ct, g0:g0 + N], [XT], [xa[i]], f"xa{i}")
                p.act(xa[i][:, 0:N], xa[i][:, 0:N], AF.Copy, [xa[i]], [xa[i]], scale=float(ALPHA))
                bk = ps[3 + i]
                for kc in range(16):
                    p.mm(bk[:, 0:N], wo_sb[i][:, kc, :], mT[:, kc, 0:N], kc == 0, kc == 15, [wo_sb[i], (mT, kc)], [bk])
                p.stt(tT[:, ct, 0:N], bk[:, 0:N], mod("g1", ct, isctx), xa[i][:, 0:N], ALU.mult, ALU.add, [bk, modv, xa[i]], [(tT, ct)])
            for c in range(NKC):
                p.mm(ps[6][:, 0:N], onesD[:], tT[:, c, 0:N], c == 0, c == NKC - 1, [onesD, (tT, c)], [ps[6]])
            for c in range(NKC):
                s_ = sq32[c % 2]
                p.act(s_[:, 0:N], tT[:, c, 0:N], AF.Square, [(tT, c)], [s_])
                p.mm(ps[7][:, 0:N], onesD[:], s_[:, 0:N], c == 0, c == NKC - 1, [onesD, s_], [ps[7]])
            p.cp("act", mean[:, 0:N], ps[6][:, 0:N], [ps[6]], [mean])
            p.tt("dve", var[:, 0:N], mean[:, 0:N], mean[:, 0:N], ALU.mult, [mean], [var])
            p.tt("dve", var[:, 0:N], ps[7][:, 0:N], var[:, 0:N], ALU.subtract, [ps[7], var], [var])
            p.act(var[:, 0:N], var[:, 0:N], AF.Sqrt, [var, epsc], [var], bias=epsc[:, 1:2], scale=1.0)
            p.recip(var[:, 0:N], var[:, 0:N], [var], [var])
            for c in range(NKC):
                a = u1[c % 2]
                p.tt("pool", a[:, 0:N], tT[:, c, 0:N], mean[:, 0:N], ALU.subtract, [(tT, c), mean], [a])
                p.tt("dve", a[:, 0:N], a[:, 0:N], var[:, 0:N], ALU.mult, [a, var], [a])
                xo = x1o[c % 2]
                p.act(xo[:, 0:N], a[:, 0:N], AF.Identity, [a, lnp], [xo], scale=lnp[:, 0, c:c + 1], bias=lnp[:, 1, c:c + 1])
                p.dma("sp", X1[:, c, g0:g0 + N], xo[:, 0:N], [xo], [X1], f"x1o{c % 2}")
                ho = h2o[c % 2]
                p.ts("dve", ho[:, 0:N], xo[:, 0:N], mod("sc2", c, isctx), mod("sh2", c, isctx), ALU.mult, ALU.add, [xo, modv], [ho])
                p.dma("act", H2[:, c, g0:g0 + N], ho[:, 0:N], [ho], [H2], f"h2o{c % 2}")
        p.reset_to(layer_base)
        if "B" in debug and l == debug["B"]:
            break


        peer_seen = False
        onesD = p.sbuf("onesD", [128, 128], F32)
        p.memset("dve", onesD[:], 1.0 / D, [onesD])
        pk = p.sbuf("pk", [128, 256], F32)
        p.dma("sp", pk[:], W["pk"][:], [W["pk"]], [pk], "c_misc")
        h2 = p.sbuf("h2_sb", [128, NKC, 512], BF16)
        acc = p.sbuf("acc_sb", [128, NKC, 512], F32)
        wqs = [p.sbuf(f"wqs{i}", [128, 16, 128], BF16) for i in range(1)] * 2
        qf = [p.sbuf(f"qf{i}", [128, 512], F32) for i in range(1)] * 2
        aS = p.sbuf("aS", [128, 4, 8, 128], F32)
        s2S = p.sbuf("s2S", [128, 4, 8, 128], F32)
        b2S = p.sbuf("b2S", [128, 4, 8], F32)
        s1t = p.sbuf("s1t", [128, 128], F32)
        wk1 = p.sbuf("wk1", [128, 256], F32)
        wk2 = p.sbuf("wk2", [128, 256], F32)
        v16 = p.sbuf("v16", [128, 2, 16], F32)
        cand = p.sbuf("cand", [128, 16, 16], F32)
        c24 = p.sbuf("c24", [128, 24], F32)
        e16 = p.sbuf("e16", [128, 16], F32)
        sm = p.sbuf("sm", [128, 8], F32)
        NI = 8
        Db = [p.sbuf(f"Db{i}", [128, NI, 128], F32) for i in range(2)]
        Eb = [p.sbuf(f"Eb{i}", [128, NI * 128], F32) for i in range(2)]
        Gacc = [p.sbuf(f"Gacc{i}", [128, NI * 128], F32) for i in range(4)]
        Gbf = [[p.sbuf(f"Gbf{b}_{i}", [128, NI * 128], BF16) for i in range(4)] for b in range(2)]
        ut = [p.sbuf(f"ut{i}", [128, 16, 128], BF16) for i in range(2)]
        vt_ = [p.sbuf(f"vts{i}", [128, NI, 128], BF16) for i in range(2)]
        gl = [p.sbuf(f"gl{i}", [128, 512], BF16) for i in range(2)]
        AT = [p.sbuf(f"AT{b}", [128, NI, 512], BF16) for b in range(2)]
        xa = [Eb[0], Eb[1]]
        tT = acc
        mean = Gacc[0]
        var = Gacc[1]
        u1 = [Gacc[2], Gacc[3]]
        sq32 = [Tk(Db[0][:].rearrange("p a b -> p (a b)"), "Db0v"), Tk(Db[1][:].rearrange("p a b -> p (a b)"), "Db1v")]
        sq32[0].id, sq32[1].id = Db[0].id, Db[1].id
        x1o = [Tk(Gbf[0][0].t.bitcast(F32), "g0v"), Tk(Gbf[0][1].t.bitcast(F32), "g1v")]
        x1o[0].id, x1o[1].id = Gbf[0][0].id, Gbf[0][1].id

        for gi, (g0, N) in enumerate(GROUPS):
            isctx = gi == 0
            if last and isctx:
                continue
            nsub = N // 128
            peer_first = not peer_seen
            peer_seen = True
            for c in range(NKC):
                p.dma("sp" if c % 2 == 0 else "act", h2[:, c, 0:N], H2[:, c, g0:g0 + N], [H2], [(h2, c)], "h2_sb")
            for h in range(8):
                w_ = wqs[h % 2]
                p.dma("pool", w_[:], W["pwq"][h], [W["pwq"]], [w_], f"wqs{h % 2}")
                bk = ps[h % 2]
                for kc in range(NKC):
                    p.mm(bk[:, 0:N], w_[:, kc, :], h2[:, kc, 0:N], kc == 0, kc == NKC - 1, [w_, (h2, kc)], [bk])
                q_ = qf[h % 2]
                p.cp("act", q_[:, 0:N], bk[:, 0:N], [bk], [q_])
                for tt_ in range(nsub):
                    cs = slice(tt_ * 128, (tt_ + 1) * 128)
                    sb_ = ps[2 + tt_ % 2]
                    p.mm(sb_[:, 0:256], q_[:, cs], pk[:, :], True, True, [q_, pk], [sb_])
                    p.cp("act", s1t[:], sb_[:, 0:128], [sb_], [s1t])
                    p.cp("act", s2S[:, tt_, h, :], sb_[:, 128:256], [sb_], [(s2S, (tt_, h))])
                    for w2, src in ((0, s1t[:]), (1, s2S[:, tt_, h, :])):
                        p.max8(v16[:, w2, 0:8], src, [s1t, (s2S, (tt_, h))], [v16])
                        p.mrep(wk1[:, 0:128], v16[:, w2, 0:8], src, [s1t, (s2S, (tt_, h)), v16], [wk1])
                        p.max8(v16[:, w2, 8:16], wk1[:, 0:128], [wk1], [v16])
                    p.tt("dve", cand[:], v16[:, 0, :].unsqueeze(2).to_broadcast([128, 16, 16]),
                         v16[:, 1, :].unsqueeze(1).to_broadcast([128, 16, 16]), ALU.add, [v16], [cand])
                    cf = cand[:].rearrange("p a b -> p (a b)")
                    p.max8(c24[:, 0:8], cf, [cand], [c24])
                    p.mrep(wk1[:], c24[:, 0:8], cf, [cand, c24], [wk1])
                    p.max8(c24[:, 8:16], wk1[:], [wk1], [c24])
                    p.ts("dve", sm[:, 0:1], c24[:, 0:1], -1.0, None, ALU.mult, None, [c24], [sm])
                    p.act(e16[:], c24[:, 0:16], AF.Exp, [c24, sm], [e16, sm], bias=sm[:, 0:1], accum=sm[:, 1:2])
                    p.ts("dve", sm[:, 2:3], c24[:, 15:16], -2e-5, None, ALU.add, None, [c24], [sm])
                    p.act(sm[:, 3:4], sm[:, 1:2], AF.Ln, [sm], [sm])
                    p.tt("dve", sm[:, 4:5], sm[:, 2:3], sm[:, 0:1], ALU.add, [sm], [sm])
                    p.tt("dve", b2S[:, tt_, h:h + 1], sm[:, 4:5], sm[:, 3:4], ALU.subtract, [sm], [(b2S, (tt_, h))])
                    p.ts("dve", aS[:, tt_, h, :], s1t[:], sm[:, 2:3], None, ALU.subtract, None, [s1t, sm], [(aS, (tt_, h))])
            if debug.get("Cstop") == 1:
                p.barrier(final=True)
                p.emit()
                return nc, p
            nchunk = 128 // NI
            for ic in range(nchunk):
                gb = ic % 2
                for tt_ in range(nsub):
                    for h in range(8):
                        k = (tt_ * 8 + h) % 2
                        p.tt("pool", Db[k][:], aS[:, tt_, h, ic * NI:(ic + 1) * NI].unsqueeze(2).to_broadcast([128, NI, 128]),
                             s2S[:, tt_, h, :].unsqueeze(1).to_broadcast([128, NI, 128]), ALU.add,
                             [(aS, (tt_, h)), (s2S, (tt_, h))], [Db[k]])
                        df = Db[k][:].rearrange("p a b -> p (a b)")
                        p.stt(df, df, 1e30, df, ALU.mult, ALU.min, [Db[k]], [Db[k]])
                        if h == 0:
                            p.act(Gacc[tt_][:], df, AF.Exp, [Db[k], (b2S, (tt_, h))], [Gacc[tt_]], bias=b2S[:, tt_, h:h + 1])
                        else:
                            p.act(Eb[k][:], df, AF.Exp, [Db[k], (b2S, (tt_, h))], [Eb[k]], bias=b2S[:, tt_, h:h + 1])
                            dst = Gbf[gb][tt_] if h == 7 else Gacc[tt_]
                            p.tt("dve", dst[:], Gacc[tt_][:], Eb[k][:], ALU.add, [Gacc[tt_], Eb[k]], [dst])
                if debug.get("Cstop") == 2:
                    p.barrier(final=True)
                    p.emit()
                    return nc, p
                for e in range(NI):
                    eg = ic * NI + e
                    u_ = ut[eg % 2]
                    uf = u_[:].rearrange("p a b -> p (a b)")
                    if peer_first:
                        p.dma("pool", u_[:], W["put"][eg], [W["put"]], [u_], f"ut{eg % 2}")
                        p.dma("sp", PUTB[eg], uf, [u_], [(PUTB, eg)], f"ut{eg % 2}")
                    else:
                        p.dma("sp", uf, PUTB[eg], [(PUTB, eg)], [u_], f"ut{eg % 2}")
                    hb = ps[eg % 2]
                    for kc in range(NKC):
                        p.mm(hb[:, 0:N], u_[:, kc, :], h2[:, kc, 0:N], kc == 0, kc == NKC - 1, [u_, (h2, kc)], [hb])
                    g_ = gl[eg % 2]
                    p.act(g_[:, 0:N], hb[:, 0:N], AF.Gelu, [hb], [g_])
                    gtb = ps[2 + eg % 2]
                    gtv = psb(2 + eg % 2)
                    for tt_ in range(nsub):
                        p.tr(gtv[:, tt_ * 128:(tt_ + 1) * 128], Gbf[gb][tt_][:, e * 128:(e + 1) * 128], ident[:],
                             [Gbf[gb][tt_], ident], [gtb])
                    p.tt("dve", AT[gb][:, e, 0:N], g_[:, 0:N], gtv[:, 0:N], ALU.mult, [g_, gtb], [(AT[gb], e)])
                if debug.get("Cstop") == 3:
                    p.barrier(final=True)
                    p.emit()
                    return nc, p
                for dt in range(16):
                    k = (ic * 16 + dt) % 2
                    v_ = vt_[k]
                    vf = v_[:].rearrange("p a b -> p (a b)")
                    if peer_first:
                        p.dma("pool", v_[:], W["pvt"][ic, dt], [W["pvt"]], [v_], f"vts{k}")
                        p.dma("sp", PVTB[ic, dt], vf, [v_], [(PVTB, (ic, dt))], f"vts{k}")
                    else:
                        p.dma("sp", vf, PVTB[ic, dt], [(PVTB, (ic, dt))], [v_], f"vts{k}")
                    ab = ps[4 + dt % 2]
                    for e in range(NI):
                        p.mm(ab[:, 0:N], v_[:, e, :], AT[gb][:, e, 0:N], e == 0, e == NI - 1, [v_, (AT[gb], e)], [ab])
                    if ic == 0:
                        p.cp("act", acc[:, dt, 0:N], ab[:, 0:N], [ab], [(acc, dt)])
                    else:
                        p.tt("dve", acc[:, dt, 0:N], acc[:, dt, 0:N], ab[:, 0:N], ALU.add, [(acc, dt), ab], [(acc, dt)])
            if debug.get("Cstop") == 5:
                p.barrier(final=True)
                p.emit()
                return nc, p
            for ct in range(16):
                i = ct % 2
                p.dma("sp", xa[i][:, 0:N], X1[:, ct, g0:g0 + N], [X1], [xa[i]], f"xa{i}")
                p.act(xa[i][:, 0:N], xa[i][:, 0:N], AF.Copy, [xa[i]], [xa[i]], scale=float(ALPHA))
                p.stt(tT[:, ct, 0:N], acc[:, ct, 0:N], mod("g2", ct, isctx), xa[i][:, 0:N], ALU.mult, ALU.add,
                      [(acc, ct), modv, xa[i]], [(tT, ct)])
            for c in range(NKC):
                p.mm(ps[6][:, 0:N], onesD[:], tT[:, c, 0:N], c == 0, c == NKC - 1, [onesD, (tT, c)], [ps[6]])
            for c in range(NKC):
                s = sq32[c % 2]
                p.act(s[:, 0:N], tT[:, c, 0:N], AF.Square, [(tT, c)], [s])
                p.mm(ps[7][:, 0:N], onesD[:], s[:, 0:N], c == 0, c == NKC - 1, [onesD, s], [ps[7]])
            p.cp("act", mean[:, 0:N], ps[6][:, 0:N], [ps[6]], [mean])
            p.tt("dve", var[:, 0:N], mean[:, 0:N], mean[:, 0:N], ALU.mult, [mean], [var])
            p.tt("dve", var[:, 0:N], ps[7][:, 0:N], var[:, 0:N], ALU.subtract, [ps[7], var], [var])
            p.act(var[:, 0:N], var[:, 0:N], AF.Sqrt, [var, epsc], [var], bias=epsc[:, 1:2], scale=1.0)
            p.recip(var[:, 0:N], var[:, 0:N], [var], [var])
            for c in range(NKC):
                a = u1[c % 2]
                p.tt("pool", a[:, 0:N], tT[:, c, 0:N], mean[:, 0:N], ALU.subtract, [(tT, c), mean], [a])
                p.tt("dve", a[:, 0:N], a[:, 0:N], var[:, 0:N], ALU.mult, [a, var], [a])
                xo = x1o[c % 2]
                p.act(xo[:, 0:N], a[:, 0:N], AF.Identity, [a, lnp], [xo], scale=lnp[:, 2, c:c + 1], bias=lnp[:, 3, c:c + 1])
                if last:
                    p.dma("sp", out[:, c, g0 - CTX:g0 - CTX + N], xo[:, 0:N], [xo], [out], f"x1o{c % 2}")
                else:
                    p.dma("sp", XO[:, c, g0:g0 + N], xo[:, 0:N], [xo], [XO], f"x1o{c % 2}")
    p.barrier(final=True)
    p.emit()
    return nc, p


def kernel(**inputs):
    inp = {k: np.asarray(v) for k, v in inputs.items()}
    consts = host_consts()
    nc, prog = build_program()
    wl = [prep_layer_weights(inp, l) for l in range(DEPTH)]
    in_maps = []
    for core in range(8):
        b = core // 2
        xfull = np.concatenate([inp["ctx"][b], inp["x"][b]], axis=0)
        xT = np.ascontiguousarray(xfull.reshape(T, NKC, 128).transpose(2, 1, 0))
        cc = np.stack([fm(inp["c"][b]), fm(inp["c_ctx"])], axis=-1)
        m = {"xT": xT, "cc": np.ascontiguousarray(cc), "ropetab": consts["ropetab"], "permm": consts["permm"],
             "misc": consts["misc"], "colc": consts["colc"], "hsel": consts["hsel"]}
        for l in range(DEPTH):
            for k in WKEYS:
                m[f"{k}{l}"] = wl[l][k]
        in_maps.append(m)
    res = run_bass_kernel_spmd(nc, in_maps, core_ids=list(range(8)))
    outp = np.empty((4, SEQ, D), np.float32)
    for core in range(8):
        b, half = core // 2, core % 2
        o = res.results[core]["out"]
        sl = slice(half * 2048, (half + 1) * 2048)
        outp[b, sl] = o[:, :, sl].transpose(2, 1, 0).reshape(2048, D)
    return outp
```

```python
import os
import numpy as np
import concourse.bass as bass
import concourse.mybir as mybir
from concourse.bass_utils import run_bass_kernel_spmd

F32 = mybir.dt.float32
BF16 = mybir.dt.bfloat16
ALU = mybir.AluOpType
AF = mybir.ActivationFunctionType

D = 2048
SEQ = 4096
CTX = 256
T = SEQ + CTX
DEPTH = 2
GRID_W = 64
NKC = 16
N_IN = 10016
ALPHA = (2.0 * DEPTH) ** 0.25
MLA_SCALE = 96 ** -0.5
RET_K_SCALE = 32 ** -0.5
GQA_SCALE = 128 ** -0.5
NORM_EPS = 1e-5
RMS_EPS = 1e-6
GROUPS = [(0, 256)] + [(256 + 512 * i, 512) for i in range(8)]
NCH = T // 128

ENGINES = ("pe", "dve", "act", "pool", "sp")
EPOCH = int(os.environ.get("KEPOCH", "16000"))
SAME_ENGINE_SYNC = True
NCHAN = int(os.environ.get('KNCHAN', '1000000007'))
ARENA_WORDS = 46 * 1024


class Tk:
    _n = 0

    def __init__(self, ap, name):
        self.t = ap
        self.name = name
        Tk._n += 1
        self.id = Tk._n

    def __getitem__(self, idx):
        return self.t[idx]


class Chan:
    def __init__(self, name):
        self.name = name
        self.count = 0
        self.sem = None
        self.id = name


class Op:
    __slots__ = ("eng", "fn", "deps", "idx", "signal", "seq", "is_dma", "chan", "chan_count", "line")


class Prog:
    def __init__(self, nc):
        self.nc = nc
        self.ops = []
        self.state = {}
        self._ctx = []
        self.chans = {}
        self.last_eng = {}
        self.last_chan = {}
        self.names = {}
        self.pending = {}
        cm = nc.sbuf_tensor("arena", [128, ARENA_WORDS], F32)
        self.arena = cm.__enter__()
        self._ctx.append(cm)
        self.arena_off = 0
        self.arena_base = 0

    def sbuf(self, name, shape, dt, parts=128):
        n = int(np.prod(shape[1:]))
        words = n if dt == F32 else (n + 1) // 2
        off = self.arena_off
        assert off + words <= ARENA_WORDS, f"arena overflow at {name}: {off}+{words}"
        self.arena_off += words
        v = self.arena[0:shape[0], off:off + words]
        if dt != F32:
            v = v.bitcast(dt)[:, 0:n]
        if len(shape) == 3:
            v = v.rearrange("p (a b) -> p a b", a=shape[1])
        elif len(shape) == 4:
            v = v.rearrange("p (a b c) -> p a b c", a=shape[1], b=shape[2])
        elif len(shape) == 5:
            v = v.rearrange("p (a b c d) -> p a b c d", a=shape[1], b=shape[2], c=shape[3])
        return Tk(v, name)

    def persist(self):
        self.arena_base = self.arena_off

    def reset(self):
        self.barrier()
        self.arena_off = self.arena_base

    def reset_to(self, base):
        self.barrier()
        if os.environ.get("KNOALIAS"):
            return
        self.arena_off = self.arena_base = base

    def psum(self, name, shape, dt):
        cm = self.nc.psum_tensor(name, list(shape), dt)
        h = cm.__enter__()
        self._ctx.append(cm)
        return Tk(h, name)

    def dram(self, name, shape, dt, kind="Internal"):
        if kind == "Internal" and name in getattr(self, "dump", ()):
            kind = "ExternalOutput"
        h = self.nc.dram_tensor(name, list(shape), dt, kind=kind)
        return Tk(h.ap(), name)

    @staticmethod
    def _tok(x):
        if isinstance(x, tuple):
            return x[0], x[1]
        return x, None

    def _conf(self, st, key):
        if key is None:
            return list(st.keys())
        ks = []
        if key in st:
            ks.append(key)
        if None in st:
            ks.append(None)
        return ks

    def op(self, eng, fn, R=(), W=(), dma=False, extra=()):
        o = Op()
        o.eng, o.fn, o.idx, o.signal, o.seq, o.is_dma = eng, fn, len(self.ops), False, None, dma
        o.chan, o.chan_count = None, 0
        import sys as _s
        f = _s._getframe(1)
        while f.f_code.co_name in ("op", "mm", "tr", "act", "tt", "ts", "stt", "cp", "memset", "dma", "barrier"):
            f = f.f_back
        o.line = f.f_lineno
        deps = set(extra) | self.pending.pop(eng, set())
        for x in R:
            tile, key = self._tok(x)
            st = self.state.setdefault(tile.id, {})
            for k in self._conf(st, key):
                if st[k][0] is not None:
                    deps.add(st[k][0])
        for x in W:
            tile, key = self._tok(x)
            st = self.state.setdefault(tile.id, {})
            for k in self._conf(st, key):
                w, rs = st[k]
                if w is not None:
                    deps.add(w)
                deps.update(rs)
        for x in R:
            tile, key = self._tok(x)
            st = self.state.setdefault(tile.id, {})
            if key not in st:
                st[key] = [st[None][0] if None in st else None, []]
            st[key][1].append(o.idx)
        for x in W:
            tile, key = self._tok(x)
            st = self.state.setdefault(tile.id, {})
            if key is None:
                st.clear()
            st[key] = [o.idx, []]
        deps.discard(o.idx)
        o.deps = deps
        self.ops.append(o)
        self.last_eng[eng] = o.idx
        return o

    def dma(self, eng, out_ap, in_ap, R, W, chan):
        import zlib as _z
        chan = f"q{_z.crc32(chan.encode()) % NCHAN}_{eng}"
        if chan not in self.chans:
            self.chans[chan] = Chan(chan)
        c = self.chans[chan]
        o = self.op(eng, lambda e: e.dma_start(out=out_ap, in_=in_ap), R, W, dma=True)
        o.chan = c
        c.count += 1
        o.chan_count = c.count
        self.last_chan[chan] = o.idx
        return o

    def barrier(self, final=False):
        deps = set(self.last_eng.values()) | set(self.last_chan.values())
        for pd in self.pending.values():
            deps |= pd
        if final:
            for e in ENGINES:
                self.op(e, lambda en: en.nop(nofuse=True), extra=deps)
        else:
            self.pending = {e: set(deps) for e in ENGINES}

    def mm(self, out, lhsT, rhs, start, stop, R, W):
        return self.op("pe", lambda e: e.matmul(out, lhsT=lhsT, rhs=rhs, start=start, stop=stop), R, W)

    def tr(self, out, in_, ident, R, W):
        return self.op("pe", lambda e: e.transpose(out, in_, ident), R, W)

    def act(self, out, in_, func, R, W, bias=None, scale=None, accum=None):
        kw = {}
        if bias is not None:
            kw["bias"] = bias
        if scale is not None:
            kw["scale"] = scale
        if accum is not None:
            kw["accum_out"] = accum
        return self.op("act", lambda e: e.activation(out=out, in_=in_, func=func, **kw), R, W)

    def tt(self, eng, out, in0, in1, op, R, W):
        return self.op(eng, lambda e: e.tensor_tensor(out=out, in0=in0, in1=in1, op=op), R, W)

    def ts(self, eng, out, in0, s1, s2, op0, op1, R, W):
        if op1 is None:
            return self.op(eng, lambda e: e.tensor_scalar(out=out, in0=in0, scalar1=s1, scalar2=None, op0=op0), R, W)
        return self.op(eng, lambda e: e.tensor_scalar(out=out, in0=in0, scalar1=s1, scalar2=s2, op0=op0, op1=op1), R, W)

    def stt(self, out, in0, scalar, in1, op0, op1, R, W):
        return self.op("dve", lambda e: e.scalar_tensor_tensor(out=out, in0=in0, scalar=scalar, in1=in1, op0=op0, op1=op1), R, W)

    def cp(self, eng, out, in_, R, W):
        if eng == "act":
            return self.op("act", lambda e: e.copy(out=out, in_=in_), R, W)
        return self.op(eng, lambda e: e.tensor_copy(out=out, in_=in_), R, W)

    def recip(self, out, in_, R, W):
        return self.op("dve", lambda e: e.reciprocal(out=out, in_=in_), R, W)

    def max8(self, out, in_, R, W):
        return self.op("dve", lambda e: e.max(out=out, in_=in_), R, W)

    def mrep(self, out, rep, vals, R, W):
        return self.op("dve", lambda e: e.match_replace(out=out, in_to_replace=rep, in_values=vals, imm_value=-1e30), R, W)

    def rsum(self, out, in_, R, W):
        return self.op("dve", lambda e: e.reduce_sum(out=out, in_=in_, axis=mybir.AxisListType.X), R, W)

    def memset(self, eng, ap, val, W):
        return self.op(eng, lambda e: e.memset(ap, val), (), W)

    def emit(self):
        nc, ops = self.nc, self.ops

        def skip(y, o):
            return y.eng == o.eng and (y.eng == "pe" or not SAME_ENGINE_SYNC)

        for o in ops:
            for d in o.deps:
                y = ops[d]
                if y.is_dma or skip(y, o):
                    continue
                y.signal = True
        cnt = {e: 0 for e in ENGINES}
        for o in ops:
            if o.signal and not o.is_dma:
                cnt[o.eng] += 1
                o.seq = cnt[o.eng]
        sems, sem_ctx, nsem = {}, [], 0
        for e in ENGINES:
            sems[e] = []
            for k in range(max(1, (cnt[e] + EPOCH - 1) // EPOCH)):
                cm = nc.semaphore(f"s_{e}_{k}")
                sems[e].append(cm.__enter__())
                sem_ctx.append(cm)
                nsem += 1
        for c in self.chans.values():
            cm = nc.semaphore(f"d_{c.name}")
            c.sem = cm.__enter__()
            sem_ctx.append(cm)
            nsem += 1
        for k in range(int(os.environ.get("KXSEM", "0"))):
            cm = nc.semaphore(f"dummy{k}")
            cm.__enter__()
            sem_ctx.append(cm)
            nsem += 1
        self.nsem = nsem
        assert nsem <= 140, f"too many semaphores: {nsem}"
        known = {e: {} for e in ENGINES}
        run = {}
        waits = []
        for o in ops:
            w = {}
            for d in o.deps:
                y = ops[d]
                if y.is_dma:
                    key = ("d", y.chan.id)
                    val = 16 * run[y.chan.id]
                    semh = y.chan.sem
                else:
                    if skip(y, o):
                        continue
                    ep = (y.seq - 1) // EPOCH
                    key = ("e", y.eng, ep)
                    val = (y.seq - 1) % EPOCH + 1
                    semh = sems[y.eng][ep]
                if known[o.eng].get(key, 0) >= val:
                    continue
                if key not in w or w[key][1] < val:
                    w[key] = (semh, val)
            for key, (semh, val) in w.items():
                known[o.eng][key] = val
            waits.append(list(w.values()))
            if o.is_dma:
                run[o.chan.id] = o.chan_count
        self.waits_dbg = waits
        engmap = {"pe": "tensor", "dve": "vector", "act": "scalar", "pool": "gpsimd", "sp": "sync"}
        by_eng = {e: [o for o in ops if o.eng == e] for e in ENGINES}
        self.stats = {e: len(by_eng[e]) for e in ENGINES}
        self.stats["waits"] = sum(len(w) for w in waits)
        with nc.Block() as block:
            for e in ENGINES:
                lst = by_eng[e]
                if not lst:
                    continue

                def body(engine, lst=lst, e=e):
                    for o in lst:
                        for semh, val in waits[o.idx]:
                            engine.wait_ge(semh, val)
                        ins = o.fn(engine)
                        if os.environ.get("KDBG"):
                            try:
                                self.names[ins.ins.name] = o.line
                            except Exception:
                                self.names[str(ins)[:40]] = o.line
                        if o.is_dma:
                            ins.then_inc(o.chan.sem, 16)
                        elif o.signal:
                            ins.then_inc(sems[e][(o.seq - 1) // EPOCH], 1)
                getattr(block, engmap[e])(body)
        for cm in reversed(sem_ctx):
            cm.__exit__(None, None, None)
        for cm in reversed(self._ctx):
            cm.__exit__(None, None, None)


OFF = dict(cq=0, ckv=512, kr=768, rq=800, rk=1056, rv=1312, rg=1824, gq=2336, gk=3360, gv=3616,
           ga=3872, gb=5920, gc=7968)
FM_TILES = []
FM_TILES.append(("kr", 0, OFF["kr"], 32))
for i in range(4):
    FM_TILES.append(("cq", i, OFF["cq"] + 128 * i, 128))
for i in range(2):
    FM_TILES.append(("ckv", i, OFF["ckv"] + 128 * i, 128))
for i in range(4):
    FM_TILES.append(("rq", i, OFF["rq"] + 64 * i, 64))
for i in range(4):
    FM_TILES.append(("rk", i, OFF["rk"] + 64 * i, 64))
for i in range(8):
    FM_TILES.append(("rg", i, OFF["rg"] + 64 * i, 64))
for i in range(8):
    FM_TILES.append(("gq", i, OFF["gq"] + 128 * i, 128))
for i in range(2):
    FM_TILES.append(("gk", i, OFF["gk"] + 128 * i, 128))
for i in range(48):
    FM_TILES.append(("gate", i, OFF["ga"] + 128 * i, 128))
NFM = len(FM_TILES)


def rope_tables(r, nblk, scale, nrows_pad=None, row0=0):
    ra, nf = r // 2, r // 4
    inv = 10000.0 ** (-np.arange(nf, dtype=np.float64) / nf)
    tl = np.arange(SEQ)
    row, col = tl // GRID_W, tl % GRID_W
    rows = nblk * r
    tot = nrows_pad or rows
    C = np.full((tot, T), scale, np.float64)
    S = np.zeros((tot, T), np.float64)
    P = np.zeros((tot, tot), np.float32)
    for b in range(nblk):
        for d in range(r):
            g = row0 + b * r + d
            half = d // ra
            dd = d % ra
            pos = row if half == 0 else col
            f = inv[dd % nf]
            C[g, CTX:] = scale * np.cos(pos * f)
            if dd < nf:
                S[g, CTX:] = -scale * np.sin(pos * f)
                P[g + nf, g] = 1.0
            else:
                S[g, CTX:] = scale * np.sin(pos * f)
                P[g - nf, g] = 1.0
    return C.astype(np.float32), S.astype(np.float32), P


_CONST = None


def host_consts():
    global _CONST
    if _CONST is not None:
        return _CONST
    c = {}
    Cq, Sq, Pq = rope_tables(32, 1, MLA_SCALE, nrows_pad=96, row0=64)
    Ckr, Skr, Pkr = rope_tables(32, 1, 1.0)
    Crq, Srq, Prr = rope_tables(32, 4, 1.0)
    Crk, Srk, _ = rope_tables(32, 4, RET_K_SCALE)
    Cgq, Sgq, Pg = rope_tables(128, 1, GQA_SCALE)
    Cgk, Sgk, _ = rope_tables(128, 1, 1.0)
    tab = np.zeros((12, 128, T), np.float32)
    for i, a in enumerate([Cq, Sq, Ckr, Skr, Crq, Srq, Crk, Srk, Cgq, Sgq, Cgk, Sgk]):
        tab[i, :a.shape[0]] = a
    c["ropetab"] = tab
    pm = np.zeros((4, 128, 128), np.float32)
    pm[0, :96, :96] = Pq
    pm[1, :32, :32] = Pkr
    pm[2] = Prr
    pm[3] = Pg
    c["permm"] = pm
    misc = np.zeros((128, 8, 128), np.float32)
    misc[:, 0, :] = np.eye(128)
    sh = np.zeros((128, 128), np.float32)
    for k in range(32):
        sh[k, 64 + k] = 1.0
    misc[:, 1, :] = sh
    i = np.arange(128)
    misc[:, 2, :] = np.maximum(i[None, :] - i[:, None], 0)
    misc[:, 3, :] = (i[None, :] >= i[:, None])
    misc[:, 4, :] = np.maximum(i[:, None] - i[None, :], 0)
    misc[:, 5, :] = (i[:, None] >= i[None, :])
    misc[:, 6, :] = (i[None, :] + 1.0)
    misc[:, 7, :] = (128.0 - i[None, :])
    c["misc"] = misc
    col = np.zeros((128, 4), np.float32)
    col[:, 0] = 127.0 - i
    col[:, 1] = i
    c["colc"] = col
    sel = np.zeros((128, 4, 8), np.float32)
    for p in range(64):
        for j in range(4):
            sel[p, j, j * 2 + p // 32] = 1.0
    c["hsel"] = sel
    _CONST = c
    return c


def fm(v):
    v = np.asarray(v, np.float32)
    return np.ascontiguousarray(v.reshape(-1, 128).T)


def prep_layer_weights(inp, l):
    w = {}
    w_in = inp["w_in"][l]
    wf = np.zeros((NFM, 128, NKC, 128), np.float32)
    for ti, (_, _, c0, nc_) in enumerate(FM_TILES):
        wf[ti, :, :, :nc_] = w_in[:, c0:c0 + nc_].reshape(NKC, 128, nc_).transpose(1, 0, 2)
    w["wf"] = wf
    w["wt_rv"] = np.ascontiguousarray(w_in[:, OFF["rv"]:OFF["rv"] + 512].reshape(NKC, 128, 512).transpose(1, 0, 2))
    w["wt_gv"] = np.ascontiguousarray(w_in[:, OFF["gv"]:OFF["gv"] + 256].reshape(NKC, 128, 256).transpose(1, 0, 2))
    w["wmod"] = np.ascontiguousarray(inp["w_mod"][l].reshape(NKC, 128, 96, 128).transpose(2, 1, 0, 3))
    w["bmod"] = fm(inp["b_mod"][l])
    w["wqup"] = np.ascontiguousarray(inp["mla_w_qup"][l].reshape(4, 128, 768).transpose(1, 0, 2))
    kv = inp["mla_w_kvup"][l].reshape(2, 128, 8, 128)
    wk = np.zeros((128, 2, 8, 96), np.float32)
    wk[:, :, :, :64] = kv[:, :, :, :64].transpose(1, 0, 2, 3)
    w["wkpad"] = wk
    w["wv"] = np.ascontiguousarray(kv[:, :, :, 64:].transpose(1, 0, 2, 3).reshape(128, 2, 512))
    nrm = np.zeros((128, 8), np.float32)
    nrm[:, 0:4] = fm(inp["mla_q_norm"][l])
    nrm[:, 4:6] = fm(inp["mla_kv_norm"][l])
    nrm[:, 6:7] = fm(inp["gqa_q_norm"][l])
    nrm[:, 7:8] = fm(inp["gqa_k_norm"][l])
    w["nrm"] = nrm
    w["decay"] = np.ascontiguousarray(inp["ret_decay_logit"][l].reshape(16))
    w["wbra"] = np.ascontiguousarray(inp["w_br_a"][l].reshape(8, 64, 16, 128).transpose(2, 1, 0, 3))
    w["wbrb"] = np.ascontiguousarray(inp["w_br_b"][l].reshape(8, 64, 16, 128).transpose(2, 1, 0, 3))
    w["wbrc"] = np.ascontiguousarray(inp["w_br_c"][l].reshape(8, 128, 16, 128).transpose(2, 1, 0, 3))
    w["wout"] = np.ascontiguousarray(inp["w_out"][l].reshape(16, 128, 16, 128).transpose(2, 1, 0, 3))
    ln = np.zeros((128, 4, 16), np.float32)
    ln[:, 0] = fm(inp["ln1_g"][l]); ln[:, 1] = fm(inp["ln1_b"][l])
    ln[:, 2] = fm(inp["ln2_g"][l]); ln[:, 3] = fm(inp["ln2_b"][l])
    w["ln"] = ln
    w["pwq"] = np.ascontiguousarray(inp["peer_w_q"][l].reshape(16, 128, 8, 128).transpose(2, 1, 0, 3))
    kk = np.zeros((128, 256), np.float32)
    kk[0:64, 0:128] = inp["peer_k1"][l].T
    kk[64:128, 128:256] = inp["peer_k2"][l].T
    w["pk"] = kk
    w["put"] = np.ascontiguousarray(inp["peer_u"][l].reshape(128, 128, 16, 128).transpose(0, 3, 2, 1))
    w["pvt"] = np.ascontiguousarray(inp["peer_v"][l].reshape(16, 8, 128, 16, 128).transpose(0, 3, 2, 1, 4))
    return w


WKEYS = ["wf", "wt_rv", "wt_gv", "wmod", "bmod", "wqup", "wkpad", "wv", "nrm", "decay", "wbra", "wbrb", "wbrc",
         "wout", "ln", "pwq", "pk", "put", "pvt"]
WSHAPES = dict(wf=[NFM, 128, NKC, 128], wt_rv=[128, NKC, 512], wt_gv=[128, NKC, 256], wmod=[96, 128, NKC, 128],
               bmod=[128, 96], wqup=[128, 4, 768], wkpad=[128, 2, 8, 96], wv=[128, 2, 512], nrm=[128, 8],
               decay=[16], wbra=[16, 64, 8, 128], wbrb=[16, 64, 8, 128], wbrc=[16, 128, 8, 128],
               wout=[16, 128, 16, 128], ln=[128, 4, 16], pwq=[8, 128, 16, 128], pk=[128, 256],
               put=[128, 128, 16, 128], pvt=[16, 16, 128, 8, 128])


def build_program(n_layers=DEPTH, debug=None):
    debug = debug or {}
    nc = bass.Bass("TRN2", target_bir_lowering=False)
    p = Prog(nc)
    p.dump = debug.get("dump", ()) if debug else ()
    EI = "ExternalInput"
    xT_in = p.dram("xT", [128, NKC, T], F32, EI)
    cc_in = p.dram("cc", [128, NKC, 2], F32, EI)
    ropetab = p.dram("ropetab", [12, 128, T], F32, EI)
    permm_in = p.dram("permm", [4, 128, 128], F32, EI)
    misc_in = p.dram("misc", [128, 8, 128], F32, EI)
    colc_in = p.dram("colc", [128, 4], F32, EI)
    hsel_in = p.dram("hsel", [128, 4, 8], F32, EI)
    Wd = [{k: p.dram(f"{k}{l}", WSHAPES[k], F32, EI) for k in WKEYS} for l in range(n_layers)]
    out = p.dram("out", [128, NKC, SEQ], F32, "ExternalOutput")

    XS = [p.dram(f"xs{i}", [128, NKC, T], F32) for i in range(2)]
    X1 = p.dram("x1", [128, NKC, T], F32)
    H2 = p.dram("h2", [128, NKC, T], BF16)
    QA = p.dram("qa", [8, 96, T], BF16)
    KA = p.dram("ka", [8, 96, T], BF16)
    VA = p.dram("va", [NCH, 128, 8 * 65], BF16)
    QB = p.dram("qb", [4, 64, T], BF16)
    KB = p.dram("kb", [4, 64, T], BF16)
    VB = p.dram("vb", [NCH, 128, 512], BF16)
    GB = p.dram("gb", [8, 64, T], BF16)
    QC = p.dram("qc", [8, 128, T], BF16)
    KC = p.dram("kc", [2, 128, T], BF16)
    VC = p.dram("vc", [NCH, 128, 256], BF16)
    GT = p.dram("gt", [48, 128, T], BF16)
    OA = p.dram("oa", [8, 64, T], BF16)
    OC = p.dram("oc", [8, 128, T], BF16)
    DR = p.dram("dr", [2, NCH, 32, 512], F32)
    PUTB = p.dram("putb", [128, 128, 2048], BF16)
    PVTB = p.dram("pvtb", [16, 16, 128, 1024], BF16)
    WFB = p.dram("wfbc", [NFM, 128, 2048], BF16)
    RS = p.dram("rs", [2, NCH, 32, 512], BF16)
    dbg = {}

    ps = [p.psum(f"ps{i}", [128, 512], F32) for i in range(8)]

    def psb(i):
        return ps[i][:, 0:256].bitcast(BF16)

    ident = p.sbuf("ident", [128, 128], BF16)
    identf = p.sbuf("identf", [128, 128], F32)
    shiftm = p.sbuf("shiftm", [128, 128], BF16)
    permm = p.sbuf("permm_sb", [128, 4, 128], BF16)
    ones_bf = p.sbuf("ones_bf", [128, 128], BF16)
    ones_f = p.sbuf("ones_f", [128, 128], F32)
    misc = p.sbuf("misc_sb", [128, 8, 128], F32)
    colc = p.sbuf("colc_sb", [128, 4], F32)
    hsel = p.sbuf("hsel_sb", [128, 4, 8], F32)
    silu_c = p.sbuf("silu_c", [128, NKC, 2], F32)
    epsc = p.sbuf("epsc", [128, 4], F32)
    p.dma("sp", misc[:], misc_in[:], [misc_in], [misc], "c_misc")
    p.dma("sp", colc[:], colc_in[:], [colc_in], [colc], "c_misc")
    p.dma("sp", hsel[:], hsel_in[:], [hsel_in], [hsel], "c_misc")
    p.dma("sp", silu_c[:], cc_in[:], [cc_in], [silu_c], "c_misc")
    p.dma("pool", permm[:], permm_in[:].rearrange("a p c -> p a c"), [permm_in], [permm], "c_perm")
    p.cp("dve", ident[:], misc[:, 0, :], [misc], [ident])
    p.cp("dve", identf[:], misc[:, 0, :], [misc], [identf])
    p.cp("dve", shiftm[:], misc[:, 1, :], [misc], [shiftm])
    p.memset("dve", ones_bf[:], 1.0, [ones_bf])
    p.memset("dve", epsc[:, 0:1], RMS_EPS, [epsc])
    p.memset("dve", epsc[:, 1:2], NORM_EPS, [epsc])
    p.memset("dve", epsc[:, 2:3], 1.0, [epsc])
    p.memset("dve", epsc[:, 3:4], 0.0, [epsc])
    p.memset("dve", ones_f[:], 1.0, [ones_f])
    p.act(silu_c[:], silu_c[:], AF.Silu, [silu_c], [silu_c])
    p.persist()
    base0 = p.arena_off

    for l in range(n_layers):
        W = Wd[l]
        XT = xT_in if l == 0 else XS[(l - 1) % 2]
        XO = XS[l % 2]
        last = l == n_layers - 1
        p.reset_to(base0)
        modv = p.sbuf("modv", [128, 96, 2], F32)
        bmod = p.sbuf("bmod", [128, 96], F32)
        nrm = p.sbuf("nrm", [128, 8], F32)
        lnp = p.sbuf("lnp", [128, 4, 16], F32)
        lg = p.sbuf("lg", [128, 16], F32)
        p.dma("sp", bmod[:], W["bmod"][:], [W["bmod"]], [bmod], "c_misc")
        p.dma("sp", nrm[:], W["nrm"][:], [W["nrm"]], [nrm], "c_misc")
        p.dma("sp", lnp[:], W["ln"][:], [W["ln"]], [lnp], "c_misc")
        p.dma("sp", lg[:], W["decay"][:].partition_broadcast(128), [W["decay"]], [lg], "c_misc")
        p.act(lg[:], lg[:], AF.Exp, [lg], [lg], scale=-1.0)
        p.act(lg[:], lg[:], AF.Ln, [lg, epsc], [lg], bias=epsc[:, 2:3])
        p.ts("dve", lg[:], lg[:], -1.0, None, ALU.mult, None, [lg], [lg])
        layer_base = p.arena_off
        p.arena_base = layer_base
        wmt = [p.sbuf(f"wmt{i}", [128, NKC, 128], F32) for i in range(3)]
        for j in range(96):
            wt = wmt[j % 3]
            p.dma("sp" if j % 2 == 0 else "act", wt[:], W["wmod"][j], [W["wmod"]], [wt], f"wmt{j % 3}")
            bank = ps[j % 2]
            for kc in range(NKC):
                p.mm(bank[:, 0:2], wt[:, kc, :], silu_c[:, kc, :], kc == 0, kc == NKC - 1, [wt, silu_c], [bank])
            p.tt("dve", modv[:, j, :], bank[:, 0:2], bmod[:, j:j + 1].to_broadcast([128, 2]), ALU.add,
                 [bank, bmod], [(modv, j)])
        for s in (1, 4):
            p.ts("dve", modv[:, 16 * s:16 * s + 16, :], modv[:, 16 * s:16 * s + 16, :], 1.0, None, ALU.add, None,
                 [modv], [modv])
        p.reset()

        def mod(kind, c, isctx):
            j = dict(sh1=0, sc1=1, g1=2, sh2=3, sc2=4, g2=5)[kind] * 16 + c
            return modv[:, j, (1 if isctx else 0):(2 if isctx else 1)]

        wq_sb = p.sbuf("wqup_sb", [128, 4, 768], BF16)
        wk_sb = p.sbuf("wkpad_sb", [128, 2, 8, 96], BF16)
        wv_sb = p.sbuf("wv_sb", [128, 2, 512], BF16)
        wrv_sb = p.sbuf("wrv_sb", [128, NKC, 512], BF16)
        wgv_sb = p.sbuf("wgv_sb", [128, NKC, 256], BF16)
        zf = p.sbuf("zf", [128, 8], F32)
        zb = p.sbuf("zb", [128, 8], F32)
        p.dma("pool", wq_sb[:], W["wqup"][:], [W["wqup"]], [wq_sb], "a_w0")
        p.dma("pool", wk_sb[:], W["wkpad"][:], [W["wkpad"]], [wk_sb], "a_w0")
        p.dma("pool", wv_sb[:], W["wv"][:], [W["wv"]], [wv_sb], "a_w0")
        p.dma("pool", wrv_sb[:], W["wt_rv"][:], [W["wt_rv"]], [wrv_sb], "a_w0")
        p.dma("pool", wgv_sb[:], W["wt_gv"][:], [W["wt_gv"]], [wgv_sb], "a_w0")
        p.ts("dve", zf[:], lg[:, 0:8], colc[:, 0:1], None, ALU.mult, None, [lg, colc], [zf])
        p.ts("dve", zb[:], lg[:, 8:16], colc[:, 1:2], None, ALU.mult, None, [lg, colc], [zb])
        p.act(zf[:], zf[:], AF.Exp, [zf], [zf])
        p.act(zb[:], zb[:], AF.Exp, [zb], [zb])
        xg = p.sbuf("xg", [128, NKC, 512], F32)
        hT = p.sbuf("hT", [128, NKC, 512], BF16)
        wfb = [p.sbuf(f"wfb{i}", [128, NKC, 128], BF16) for i in range(3)]
        tabs = [p.sbuf(f"tab{i}", [128, 2, 512], F32) for i in range(2)]
        raw = [p.sbuf(f"raw{i}", [128, 512], F32) for i in range(2)]
        rawb = [p.sbuf(f"rawb{i}", [128, 512], BF16) for i in range(2)]
        sqb = [p.sbuf(f"sqb{i}", [128, 512], BF16) for i in range(2)]
        cqT = p.sbuf("cqT", [128, 4, 512], F32)
        cqn = p.sbuf("cqn", [128, 4, 512], BF16)
        ckvn = p.sbuf("ckvn", [128, 2, 512], BF16)
        krr = p.sbuf("krr", [32, 512], BF16)
        rstd = p.sbuf("rstd", [128, 512], F32)
        t1 = [p.sbuf(f"t1_{i}", [128, 512], F32) for i in range(2)]
        t2 = [p.sbuf(f"t2_{i}", [128, 512], F32) for i in range(2)]
        outb = [p.sbuf(f"outb{i}", [128, 512], BF16) for i in range(3)]
        vaext = [p.sbuf(f"vaext{i}", [128, 8, 65], BF16) for i in range(2)]
        vtm = [p.sbuf(f"vtm{i}", [128, 512], BF16) for i in range(2)]
        kz = [p.sbuf(f"kz{i}", [128, 2, 256], BF16) for i in range(2)]
        kbT = p.sbuf("kbT", [64, 4, 512], BF16)
        drs = [p.sbuf(f"drs{i}", [32, 2, 512], F32) for i in range(2)]
        for i in range(2):
            p.memset("pool", vaext[i][:], 1.0, [vaext[i]])
        cnt = {"o": 0, "r": 0, "t": 0}

        def rope(xT_, xap, nrow, tabidx, permidx, g0, N, bank_r):
            i = cnt["r"] % 2
            cnt["r"] += 1
            tb = tabs[i]
            p.dma("sp", tb[0:nrow, :, 0:N], ropetab[tabidx:tabidx + 2, 0:nrow, g0:g0 + N].rearrange("a p n -> p a n"),
                  [ropetab], [tb], f"tab{i}")
            br = bank_r
            p.mm(br[0:nrow, 0:N], permm[0:nrow, permidx, 0:nrow], xap, True, True, [permm, xT_], [br])
            a, b = t1[i], t2[i]
            p.tt("dve", a[0:nrow, 0:N], xap, tb[0:nrow, 0, 0:N], ALU.mult, [xT_, tb], [a])
            p.tt("dve", b[0:nrow, 0:N], br[0:nrow, 0:N], tb[0:nrow, 1, 0:N], ALU.mult, [br, tb], [b])
            j = cnt["o"] % 3
            cnt["o"] += 1
            ob = outb[j]
            p.tt("pool", ob[0:nrow, 0:N], a[0:nrow, 0:N], b[0:nrow, 0:N], ALU.add, [a, b], [ob])
            return ob, j

        def to_bf(bank, nrow, N):
            i = cnt["t"] % 2
            cnt["t"] += 1
            xb = rawb[i]
            p.cp("act", xb[0:nrow, 0:N], bank[0:nrow, 0:N], [bank], [xb])
            return xb

        def rms_bcast(src_list, nfeat, N, bank):
            for k, (sT, sap) in enumerate(src_list):
                sq = sqb[k % 2]
                p.act(sq[:, 0:N], sap, AF.Square, [sT], [sq])
                p.mm(bank[:, 0:N], ones_bf[:], sq[:, 0:N], k == 0, k == len(src_list) - 1, [ones_bf, sq], [bank])
            p.act(rstd[:, 0:N], bank[:, 0:N], AF.Sqrt, [bank, epsc], [rstd], bias=epsc[:, 0:1], scale=1.0 / nfeat)
            p.recip(rstd[:, 0:N], rstd[:, 0:N], [rstd], [rstd])

        wf_cached = set()
        for gi, (g0, N) in enumerate(GROUPS):
            isctx = gi == 0
            nsub = N // 128
            for c in range(NKC):
                p.dma("sp" if c % 2 == 0 else "act", xg[:, c, 0:N], XT[:, c, g0:g0 + N], [XT], [(xg, c)], "xg")
            for c in range(NKC):
                p.ts("dve", hT[:, c, 0:N], xg[:, c, 0:N], mod("sc1", c, isctx), mod("sh1", c, isctx),
                     ALU.mult, ALU.add, [(xg, c), modv], [(hT, c)])
            for ti, (kind, idx, c0, ncol) in enumerate(FM_TILES):
                if last and isctx and kind in ("cq", "rq", "rg", "gq", "gate"):
                    continue
                wt = wfb[ti % 3]
                wtf = wt[:].rearrange("p a b -> p (a b)")
                if ti not in wf_cached:
                    p.dma("pool", wt[:], W["wf"][ti], [W["wf"]], [wt], f"wfb{ti % 3}")
                    p.dma("act", WFB[ti], wtf, [wt], [(WFB, ti)], f"wfb{ti % 3}")
                    wf_cached.add(ti)
                else:
                    p.dma("sp", wtf, WFB[ti], [(WFB, ti)], [wt], f"wfb{ti % 3}")
                bank = ps[ti % 2]
                for kc in range(NKC):
                    p.mm(bank[0:ncol, 0:N], wt[:, kc, 0:ncol], hT[:, kc, 0:N], kc == 0, kc == NKC - 1, [wt, (hT, kc)], [bank])
                if kind == "kr":
                    xb = to_bf(bank, 32, N)
                    ob, j = rope(xb, xb[0:32, 0:N], 32, 2, 1, g0, N, ps[2])
                    p.cp("dve", krr[:, 0:N], ob[0:32, 0:N], [ob], [krr])
                elif kind == "cq":
                    p.cp("act", cqT[:, idx, 0:N], bank[:, 0:N], [bank], [(cqT, idx)])
                    if idx == 3:
                        rms_bcast([((cqT, k), cqT[:, k, 0:N]) for k in range(4)], 512, N, ps[2])
                        for k in range(4):
                            p.stt(cqn[:, k, 0:N], cqT[:, k, 0:N], nrm[:, k:k + 1], rstd[:, 0:N], ALU.mult, ALU.mult,
                                  [(cqT, k), nrm, rstd], [(cqn, k)])
                        for h in range(8):
                            b2 = ps[3 + h % 2]
                            for k in range(4):
                                p.mm(b2[0:96, 0:N], wq_sb[:, k, h * 96:(h + 1) * 96], cqn[:, k, 0:N], k == 0, k == 3,
                                     [wq_sb, (cqn, k)], [b2])
                            xb = to_bf(b2, 96, N)
                            ob, j = rope(xb, xb[0:96, 0:N], 96, 0, 0, g0, N, ps[5 + h % 2])
                            p.dma("sp", QA[h, :, g0:g0 + N], ob[0:96, 0:N], [ob], [QA], f"outb{j}")
                elif kind == "ckv":
                    p.cp("act", cqT[:, idx, 0:N], bank[:, 0:N], [bank], [(cqT, idx)])
                    if idx == 1:
                        rms_bcast([((cqT, k), cqT[:, k, 0:N]) for k in range(2)], 256, N, ps[2])
                        for k in range(2):
                            p.stt(ckvn[:, k, 0:N], cqT[:, k, 0:N], nrm[:, 4 + k:5 + k], rstd[:, 0:N], ALU.mult, ALU.mult,
                                  [(cqT, k), nrm, rstd], [(ckvn, k)])
                        for h in range(8):
                            b2 = ps[3 + h % 2]
                            for k in range(2):
                                p.mm(b2[0:96, 0:N], wk_sb[:, k, h, :], ckvn[:, k, 0:N], k == 0, False, [wk_sb, (ckvn, k)], [b2])
                            p.mm(b2[0:96, 0:N], shiftm[0:32, 0:96], krr[:, 0:N], False, True, [shiftm, krr], [b2])
                            j = cnt["o"] % 3
                            cnt["o"] += 1
                            ob = outb[j]
                            p.cp("act", ob[0:96, 0:N], b2[0:96, 0:N], [b2], [ob])
                            p.dma("sp", KA[h, :, g0:g0 + N], ob[0:96, 0:N], [ob], [KA], f"outb{j}")
                        for s in range(nsub):
                            b2 = ps[5 + s % 2]
                            for k in range(2):
                                p.mm(b2[:, :], ckvn[:, k, s * 128:(s + 1) * 128], wv_sb[:, k, :], k == 0, k == 1,
                                     [(ckvn, k), wv_sb], [b2])
                            ve = vaext[s % 2]
                            p.cp("act", ve[:, :, 0:64], b2[:, :].rearrange("p (h v) -> p h v", h=8), [b2], [ve])
                            p.dma("sp", VA[g0 // 128 + s], ve[:].rearrange("p h v -> p (h v)"), [ve], [VA], f"vaext{s % 2}")
                elif kind == "rq":
                    xb = to_bf(bank, 64, N)
                    ob, j = rope(xb, xb[0:64, 0:N], 64, 4, 2, g0, N, ps[2])
                    p.dma("sp", QB[idx, :, g0:g0 + N], ob[0:64, 0:N], [ob], [QB], f"outb{j}")
                elif kind == "rk":
                    xb = to_bf(bank, 64, N)
                    ob, j = rope(xb, xb[0:64, 0:N], 64, 6, 2, g0, N, ps[2])
                    p.dma("sp", KB[idx, :, g0:g0 + N], ob[0:64, 0:N], [ob], [KB], f"outb{j}")
                    p.cp("pool", kbT[:, idx, 0:N], ob[0:64, 0:N], [ob], [(kbT, idx)])
                elif kind == "rg":
                    j = cnt["o"] % 3
                    cnt["o"] += 1
                    ob = outb[j]
                    p.act(ob[0:64, 0:N], bank[0:64, 0:N], AF.Silu, [bank], [ob])
                    p.dma("sp", GB[idx, :, g0:g0 + N], ob[0:64, 0:N], [ob], [GB], f"outb{j}")
                elif kind in ("gq", "gk"):
                    i = cnt["t"] % 2
                    cnt["t"] += 1
                    rw = raw[i]
                    p.cp("act", rw[:, 0:N], bank[:, 0:N], [bank], [rw])
                    rms_bcast([(rw, rw[:, 0:N])], 128, N, ps[2])
                    gcol = 6 if kind == "gq" else 7
                    qn = rawb[i]
                    p.stt(qn[:, 0:N], rw[:, 0:N], nrm[:, gcol:gcol + 1], rstd[:, 0:N], ALU.mult, ALU.mult, [rw, nrm, rstd], [qn])
                    dstT = QC if kind == "gq" else KC
                    ob, j = rope(qn, qn[:, 0:N], 128, 8 if kind == "gq" else 10, 3, g0, N, ps[3])
                    p.dma("sp", dstT[idx, :, g0:g0 + N], ob[:, 0:N], [ob], [dstT], f"outb{j}")
                elif kind == "gate":
                    j = cnt["o"] % 3
                    cnt["o"] += 1
                    ob = outb[j]
                    p.act(ob[:, 0:N], bank[:, 0:N], AF.Sigmoid, [bank], [ob])
                    p.dma("sp", GT[idx, :, g0:g0 + N], ob[:, 0:N], [ob], [GT], f"outb{j}")
            for s in range(nsub):
                ch = g0 // 128 + s
                b2 = ps[4 + s % 2]
                for kc in range(NKC):
                    p.mm(b2[:, :], hT[:, kc, s * 128:(s + 1) * 128], wrv_sb[:, kc, :], kc == 0, kc == NKC - 1,
                         [(hT, kc), wrv_sb], [b2])
                vt = vtm[s % 2]
                p.cp("act", vt[:], b2[:, :], [b2], [vt])
                p.dma("sp", VB[ch], vt[:], [vt], [VB], f"vtm{s % 2}")
                b3 = ps[6]
                for kc in range(NKC):
                    p.mm(b3[:, 0:256], hT[:, kc, s * 128:(s + 1) * 128], wgv_sb[:, kc, :], kc == 0, kc == NKC - 1,
                         [(hT, kc), wgv_sb], [b3])
                j = cnt["o"] % 3
                cnt["o"] += 1
                ob = outb[j]
                p.cp("act", ob[:, 0:256], b3[:, 0:256], [b3], [ob])
                p.dma("sp", VC[ch], ob[:, 0:256], [ob], [VC], f"outb{j}")
                b4 = ps[7]
                b4b = psb(7)
                for j2 in range(4):
                    p.tr(b4b[:, j2 * 64:(j2 + 1) * 64], kbT[:, j2, s * 128:(s + 1) * 128], ident[0:64, 0:64], [(kbT, j2), ident], [b4])
                kzt = kz[s % 2]
                p.tt("dve", kzt[:, 0, :].rearrange("p (h d) -> p h d", h=8), b4b[:, 0:256].rearrange("p (h d) -> p h d", h=8),
                     zf[:].unsqueeze(2).to_broadcast([128, 8, 32]), ALU.mult, [b4, zf], [(kzt, 0)])
                p.tt("dve", kzt[:, 1, :].rearrange("p (h d) -> p h d", h=8), b4b[:, 0:256].rearrange("p (h d) -> p h d", h=8),
                     zb[:].unsqueeze(2).to_broadcast([128, 8, 32]), ALU.mult, [b4, zb], [(kzt, 1)])
                dd = drs[s % 2]
                for dr_ in range(2):
                    b5 = ps[2 + dr_]
                    for h in range(8):
                        p.mm(b5[0:32, h * 64:(h + 1) * 64], kzt[:, dr_, h * 32:(h + 1) * 32], vt[:, h * 64:(h + 1) * 64],
                             True, True, [(kzt, dr_), vt], [b5])
                    p.cp("act" if dr_ == 0 else "dve", dd[:, dr_, :], b5[0:32, :], [b5], [(dd, dr_)])
                    p.dma("sp", DR[dr_, ch], dd[:, dr_, :], [(dd, dr_)], [DR], f"drs{s % 2}")
        p.reset()
        if "A" in debug and l == debug["A"]:
            break

        gtab = p.sbuf("gtab", [32, 2, 8], F32)
        p.act(gtab[:].rearrange("p a h -> p (a h)"), lg[0:32, :], AF.Exp, [lg], [gtab], scale=128.0)
        st = p.sbuf("st", [32, 512], F32)
        stb = [p.sbuf(f"stb{i}", [32, 512], BF16) for i in range(2)]
        dl = [p.sbuf(f"dl{i}", [32, 512], F32) for i in range(2)]
        zero = p.sbuf("zero32", [32, 512], BF16)
        p.memset("dve", zero[:], 0.0, [zero])
        sc = {"n": 0}

        def st_store(dr_, ch, src=None):
            i = sc["n"] % 2
            sc["n"] += 1
            if src is None:
                p.cp("act", stb[i][:], st[:], [st], [stb[i]])
                src = stb[i]
                p.dma("sp", RS[dr_, ch], src[:], [src], [RS], f"stb{i}")
            else:
                p.dma("sp", RS[dr_, ch], src[:], [src], [RS], "zero32")

        def st_load(dr_, ch, into_state):
            i = sc["n"] % 2
            d_ = dl[i]
            p.dma("act", d_[:], DR[dr_, ch], [DR], [d_], f"dl{i}")
            return d_

        def st_step(dr_, ch, first=False):
            d_ = st_load(dr_, ch, False)
            if first:
                p.cp("dve", st[:], d_[:], [d_], [st])
            else:
                g3 = gtab[:, dr_, :].unsqueeze(2).to_broadcast([32, 8, 64])
                p.tt("dve", st[:].rearrange("p (h v) -> p h v", h=8), st[:].rearrange("p (h v) -> p h v", h=8), g3, ALU.mult,
                     [st, gtab], [st])
                p.tt("dve", st[:], st[:], d_[:], ALU.add, [st, d_], [st])

        st_store(0, 0, zero)
        st_step(0, 0, first=True)
        st_store(0, 1)
        st_step(0, 1)
        for ch in range(2, NCH):
            st_store(0, ch)
            if ch < NCH - 1:
                st_step(0, ch)
        st_store(1, 1, zero)
        st_step(1, 1, first=True)
        st_store(1, 0)
        st_step(1, 0)
        for ch in range(NCH - 1, 1, -1):
            st_store(1, ch)
            if ch > 2:
                st_step(1, ch)
        p.reset()

        ka = p.sbuf("ka_sb", [96, 8, T], BF16)
        va = p.sbuf("va_sb", [128, NCH, 520], BF16)
        for h in range(8):
            p.dma("sp" if h % 2 == 0 else "act", ka[:, h, :], KA[h], [KA], [(ka, h)], "b_kv")
        for c4 in range(0, NCH, 2):
            p.dma("sp" if c4 % 4 == 0 else "act", va[:, c4:c4 + 2, :], VA[c4:c4 + 2].rearrange("c p f -> p c f"), [VA], [(va, c4)], "b_kv")
        qsb = [p.sbuf(f"qsb{i}", [128, 512], BF16) for i in range(2)]
        pT = [p.sbuf(f"pT{i}", [128, 512], BF16) for i in range(3)]
        rden = p.sbuf("rden", [128, 512], F32)
        accs = p.sbuf("accs", [128, 512], F32)
        oo = [p.sbuf(f"oo{i}", [128, 512], BF16) for i in range(2)]
        for gi, (g0, N) in enumerate(GROUPS):
            isctx = gi == 0
            if last and isctx:
                continue
            kt_list = list(range(2)) if isctx else list(range(NCH))
            for h in range(8):
                q_ = qsb[h % 2]
                p.dma("sp", q_[0:96, 0:N], QA[h, :, g0:g0 + N], [QA], [q_], f"qsb{h % 2}")
                acc = ps[4 + h % 2]
                den = ps[7] if h % 2 == 0 else ps[3]
                nk = len(kt_list)
                LOOK = 2
                for step in range(nk + LOOK):
                    if step < nk:
                        ki, kt = step, kt_list[step]
                        sb_ = ps[ki % 3]
                        p.mm(sb_[:, 0:N], ka[:, h, kt * 128:(kt + 1) * 128], q_[0:96, 0:N], True, True, [ka, q_], [sb_])
                        pt = pT[ki % 3]
                        p.act(pt[:, 0:N], sb_[:, 0:N], AF.Exp, [sb_], [pt])
                    if step >= LOOK:
                        ki = step - LOOK
                        kt = kt_list[ki]
                        pt = pT[ki % 3]
                        fl = (ki == 0, ki == nk - 1)
                        p.mm(acc[0:64, 0:N], va[:, kt, h * 65:h * 65 + 64], pt[:, 0:N], fl[0], fl[1], [va, pt], [acc])
                        p.mm(den[0:1, 0:N], ones_bf[:, 0:1], pt[:, 0:N], fl[0], fl[1], [ones_bf, pt], [den])
                p.recip(rden[0:1, 0:N], den[0:1, 0:N], [den], [rden])
                p.cp("act", accs[0:64, 0:N], acc[0:64, 0:N], [acc], [accs])
                p.mm(ps[6][0:64, 0:N], ones_f[0:1, 0:64], rden[0:1, 0:N], True, True, [ones_f, rden], [ps[6]])
                o_ = oo[h % 2]
                p.tt("dve", o_[0:64, 0:N], accs[0:64, 0:N], ps[6][0:64, 0:N], ALU.mult, [accs, ps[6]], [o_])
                p.dma("pool", OA[h, :, g0:g0 + N], o_[0:64, 0:N], [o_], [OA], f"oo{h % 2}")
        p.reset_to(layer_base)
        if "B1" in debug and l == debug["B1"]:
            break
        kcs = p.sbuf("kc_sb", [128, 2, T], BF16)
        vcs = p.sbuf("vc_sb", [128, NCH, 256], BF16)
        for c4 in range(0, NCH, 2):
            p.dma("act", vcs[:, c4:c4 + 2, :], VC[c4:c4 + 2].rearrange("c p f -> p c f"), [VC], [(vcs, c4)], "b_kv")
        for g in range(2):
            p.dma("sp", kcs[:, g, :], KC[g], [KC], [(kcs, g)], "b_kv")
        qsb = [p.sbuf(f"qsb{i}", [128, 512], BF16) for i in range(2)]
        pT = [p.sbuf(f"pT{i}", [128, 512], BF16) for i in range(3)]
        rden = p.sbuf("rden", [128, 512], F32)
        accs = p.sbuf("accs", [128, 512], F32)
        oo = [p.sbuf(f"oo{i}", [128, 512], BF16) for i in range(2)]
        for gi, (g0, N) in enumerate(GROUPS):
            isctx = gi == 0
            if last and isctx:
                continue
            kt_list = list(range(2)) if isctx else list(range(NCH))
            for h in range(8):
                g = h // 4
                q_ = qsb[h % 2]
                p.dma("sp", q_[:, 0:N], QC[h, :, g0:g0 + N], [QC], [q_], f"qsb{h % 2}")
                acc = ps[4 + h % 2]
                den = ps[7] if h % 2 == 0 else ps[3]
                nk = len(kt_list)
                LOOK = 2
                for step in range(nk + LOOK):
                    if step < nk:
                        ki, kt = step, kt_list[step]
                        sb_ = ps[ki % 3]
                        p.mm(sb_[:, 0:N], kcs[:, g, kt * 128:(kt + 1) * 128], q_[:, 0:N], True, True, [kcs, q_], [sb_])
                        pt = pT[ki % 3]
                        p.act(pt[:, 0:N], sb_[:, 0:N], AF.Exp, [sb_], [pt])
                    if step >= LOOK:
                        ki = step - LOOK
                        kt = kt_list[ki]
                        pt = pT[ki % 3]
                        fl = (ki == 0, ki == nk - 1)
                        p.mm(acc[:, 0:N], vcs[:, kt, g * 128:(g + 1) * 128], pt[:, 0:N], fl[0], fl[1], [vcs, pt], [acc])
                        p.mm(den[0:1, 0:N], ones_bf[:, 0:1], pt[:, 0:N], fl[0], fl[1], [ones_bf, pt], [den])
                p.recip(rden[0:1, 0:N], den[0:1, 0:N], [den], [rden])
                p.cp("act", accs[:, 0:N], acc[:, 0:N], [acc], [accs])
                p.mm(ps[6][:, 0:N], ones_f[0:1, :], rden[0:1, 0:N], True, True, [ones_f, rden], [ps[6]])
                o_ = oo[h % 2]
                p.tt("dve", o_[:, 0:N], accs[:, 0:N], ps[6][:, 0:N], ALU.mult, [accs, ps[6]], [o_])
                p.dma("pool", OC[h, :, g0:g0 + N], o_[:, 0:N], [o_], [OC], f"oo{h % 2}")
        p.reset_to(layer_base)
        if "B2" in debug and l == debug["B2"]:
            break
        lgs = p.sbuf("lgs", [128, 2, 4], F32)
        tmp8 = p.sbuf("tmp8", [128, 4, 8], F32)
        for dr_ in range(2):
            p.tt("dve", tmp8[:], hsel[:], lg[:, dr_ * 8:dr_ * 8 + 8].unsqueeze(1).to_broadcast([128, 4, 8]), ALU.mult,
                 [hsel, lg], [tmp8])
            p.rsum(lgs[:, dr_, :], tmp8[:], [tmp8], [lgs])
        xi = p.sbuf("xi", [128, 2, 4, 128], BF16)
        xif = p.sbuf("xif", [128, 128], F32)
        for dr_ in range(2):
            for j in range(4):
                p.act(xif[:], misc[:, 6 + dr_, :], AF.Exp, [misc, lgs], [xif], scale=lgs[:, dr_, j:j + 1])
                p.cp("dve", xi[:, dr_, j, :], xif[:], [xif], [xi])
        mcomb = p.sbuf("mcomb", [128, 8, 128], F32)
        mtmp = p.sbuf("mtmp", [128, 128], F32)
        for h in range(8):
            p.act(mcomb[:, h, :], misc[:, 2, :], AF.Exp, [misc, lg], [(mcomb, h)], scale=lg[:, h:h + 1])
            p.tt("dve", mcomb[:, h, :], mcomb[:, h, :], misc[:, 3, :], ALU.mult, [(mcomb, h), misc], [(mcomb, h)])
            p.act(mtmp[:], misc[:, 4, :], AF.Exp, [misc, lg], [mtmp], scale=lg[:, 8 + h:9 + h])
            p.tt("dve", mtmp[:], mtmp[:], misc[:, 5, :], ALU.mult, [mtmp, misc], [mtmp])
            p.tt("dve", mcomb[:, h, :], mcomb[:, h, :], mtmp[:], ALU.add, [(mcomb, h), mtmp], [(mcomb, h)])
        ones64 = p.sbuf("ones64", [64, 64], F32)
        p.memset("dve", ones64[:], 1.0 / 64.0, [ones64])
        onesD = p.sbuf("onesD", [128, 128], F32)
        p.memset("dve", onesD[:], 1.0 / D, [onesD])
        oA = p.sbuf("oA", [64, 8, 512], BF16)
        oB = p.sbuf("oB", [64, 8, 512], BF16)
        oC = p.sbuf("oC", [128, 8, 512], BF16)
        qb_sb = p.sbuf("qb_sb", [64, 4, 512], BF16)
        kb_sb = p.sbuf("kb_sb", [64, 4, 512], BF16)
        vb_sb = p.sbuf("vb_sb", [128, 4, 512], BF16)
        qxi = p.sbuf("qxi", [64, 2, 4, 512], BF16)
        rsb = p.sbuf("rsb", [64, 2, 4, 4, 64], BF16)
        pr = [p.sbuf(f"pr{i}", [128, 128], BF16) for i in range(2)]
        ysb = p.sbuf("ysb", [64, 512], F32)
        ysq = p.sbuf("ysq", [64, 512], F32)
        mean = p.sbuf("mean", [128, 512], F32)
        var = p.sbuf("var", [128, 512], F32)
        gbs = [p.sbuf(f"gbs{i}", [64, 512], BF16) for i in range(2)]
        wa_sb = [p.sbuf(f"wa_sb{i}", [64, 8, 128], BF16) for i in range(2)]
        wb_sb = [p.sbuf(f"wb_sb{i}", [64, 8, 128], BF16) for i in range(2)]
        wc_sb = [p.sbuf(f"wc_sb{i}", [128, 8, 128], BF16) for i in range(2)]
        wo_sb = [p.sbuf(f"wo_sb{i}", [128, 16, 128], BF16) for i in range(2)]
        gts = [p.sbuf(f"gts{i}", [128, 3, 512], BF16) for i in range(2)]
        mT = p.sbuf("mT", [128, 16, 512], BF16)
        tT = p.sbuf("tT", [128, 16, 512], F32)
        xa = [p.sbuf(f"xa{i}", [128, 512], F32) for i in range(2)]
        u1 = [p.sbuf(f"u1_{i}", [128, 512], F32) for i in range(2)]
        u2 = [p.sbuf(f"u2_{i}", [128, 512], F32) for i in range(2)]
        sq32 = [p.sbuf(f"sq32_{i}", [128, 512], F32) for i in range(2)]
        x1o = [p.sbuf(f"x1o{i}", [128, 512], F32) for i in range(2)]
        h2o = [p.sbuf(f"h2o{i}", [128, 512], BF16) for i in range(2)]

        for gi, (g0, N) in enumerate(GROUPS):
            isctx = gi == 0
            if last and isctx:
                continue
            nsub = N // 128
            for h in range(8):
                p.dma("sp" if h % 2 == 0 else "act", oA[:, h, 0:N], OA[h, :, g0:g0 + N], [OA], [(oA, h)], "oA")
                p.dma("act" if h % 2 == 0 else "sp", oC[:, h, 0:N], OC[h, :, g0:g0 + N], [OC], [(oC, h)], "oC")
            for j in range(4):
                p.dma("sp", qb_sb[:, j, 0:N], QB[j, :, g0:g0 + N], [QB], [(qb_sb, j)], "qb_sb")
                p.dma("act", kb_sb[:, j, 0:N], KB[j, :, g0:g0 + N], [KB], [(kb_sb, j)], "kb_sb")
            p.dma("sp", vb_sb[:, 0:nsub, :], VB[g0 // 128:g0 // 128 + nsub].rearrange("c p f -> p c f"), [VB], [vb_sb], "vb_sb")
            for dr_ in range(2):
                for s in range(nsub):
                    for hh in range(2):
                        p.dma("act", rsb[hh * 32:(hh + 1) * 32, dr_, s, :, :],
                              RS[dr_, g0 // 128 + s].rearrange("d (j h v) -> h d j v", j=4, h=2)[hh], [RS], [(rsb, (dr_, s, hh))], "rsb")
                for j in range(4):
                    p.tt("pool", qxi[:, dr_, j, 0:N].rearrange("p (s i) -> p s i", i=128),
                         qb_sb[:, j, 0:N].rearrange("p (s i) -> p s i", i=128),
                         xi[0:64, dr_, j, :].unsqueeze(1).to_broadcast([64, nsub, 128]), ALU.mult, [qb_sb, xi], [qxi])
            for h in range(8):
                j, hh = h // 2, h % 2
                pb = slice(hh * 32, hh * 32 + 32)
                yb = ps[4 + h % 2]
                for s in range(nsub):
                    cs = slice(s * 128, (s + 1) * 128)
                    sb_ = ps[(h * 4 + s) % 3]
                    p.mm(sb_[:, 0:128], kb_sb[pb, j, cs], qb_sb[pb, j, cs], True, True, [kb_sb, qb_sb], [sb_])
                    pr_ = pr[(h * 4 + s) % 2]
                    p.tt("dve", pr_[:], sb_[:, 0:128], mcomb[:, h, :], ALU.mult, [sb_, (mcomb, h)], [pr_])
                    p.mm(yb[0:64, cs], vb_sb[:, s, h * 64:(h + 1) * 64], pr_[:], True, False, [vb_sb, pr_], [yb])
                    p.mm(yb[0:64, cs], rsb[pb, 0, s, j, :], qxi[pb, 0, j, cs], False, False, [rsb, qxi], [yb])
                    p.mm(yb[0:64, cs], rsb[pb, 1, s, j, :], qxi[pb, 1, j, cs], False, True, [rsb, qxi], [yb])
                p.cp("act", ysb[:, 0:N], yb[0:64, 0:N], [yb], [ysb])
                p.act(ysq[:, 0:N], yb[0:64, 0:N], AF.Square, [yb], [ysq])
                p.mm(ps[6][0:64, 0:N], ones64[:], ysb[:, 0:N], True, True, [ones64, ysb], [ps[6]])
                p.mm(ps[7][0:64, 0:N], ones64[:], ysq[:, 0:N], True, True, [ones64, ysq], [ps[7]])
                gb_ = gbs[h % 2]
                p.dma("sp", gb_[:, 0:N], GB[h, :, g0:g0 + N], [GB], [gb_], f"gbs{h % 2}")
                p.cp("act", mean[0:64, 0:N], ps[6][0:64, 0:N], [ps[6]], [mean])
                p.tt("dve", var[0:64, 0:N], mean[0:64, 0:N], mean[0:64, 0:N], ALU.mult, [mean], [var])
                p.tt("dve", var[0:64, 0:N], ps[7][0:64, 0:N], var[0:64, 0:N], ALU.subtract, [ps[7], var], [var])
                p.act(var[0:64, 0:N], var[0:64, 0:N], AF.Sqrt, [var, epsc], [var], bias=epsc[0:64, 1:2], scale=1.0)
                p.recip(var[0:64, 0:N], var[0:64, 0:N], [var], [var])
                p.tt("pool", ysb[:, 0:N], ysb[:, 0:N], mean[0:64, 0:N], ALU.subtract, [ysb, mean], [ysb])
                p.tt("dve", ysb[:, 0:N], ysb[:, 0:N], var[0:64, 0:N], ALU.mult, [ysb, var], [ysb])
                p.tt("dve", oB[:, h, 0:N], ysb[:, 0:N], gb_[:, 0:N], ALU.mult, [ysb, gb_], [(oB, h)])
            for ct in range(16):
                i = ct % 2
                p.dma("pool", wa_sb[i][:], W["wbra"][ct], [W["wbra"]], [wa_sb[i]], f"wa_sb{i}")
                p.dma("pool", wb_sb[i][:], W["wbrb"][ct], [W["wbrb"]], [wb_sb[i]], f"wb_sb{i}")
                p.dma("pool", wc_sb[i][:], W["wbrc"][ct], [W["wbrc"]], [wc_sb[i]], f"wc_sb{i}")
                for b3_ in range(3):
                    p.dma("act", gts[i][:, b3_, 0:N], GT[b3_ * 16 + ct, :, g0:g0 + N], [GT], [(gts[i], b3_)], f"gts{i}")
                for h in range(8):
                    p.mm(ps[0][:, 0:N], wa_sb[i][:, h, :], oA[:, h, 0:N], h == 0, h == 7, [wa_sb[i], (oA, h)], [ps[0]])
                for h in range(8):
                    p.mm(ps[1][:, 0:N], wb_sb[i][:, h, :], oB[:, h, 0:N], h == 0, h == 7, [wb_sb[i], (oB, h)], [ps[1]])
                for h in range(8):
                    p.mm(ps[2][:, 0:N], wc_sb[i][:, h, :], oC[:, h, 0:N], h == 0, h == 7, [wc_sb[i], (oC, h)], [ps[2]])
                a, b = u1[i], u2[i]
                p.tt("dve", a[:, 0:N], ps[0][:, 0:N], gts[i][:, 0, 0:N], ALU.mult, [ps[0], gts[i]], [a])
                p.tt("dve", b[:, 0:N], ps[1][:, 0:N], gts[i][:, 1, 0:N], ALU.mult, [ps[1], gts[i]], [b])
                p.tt("pool", a[:, 0:N], a[:, 0:N], b[:, 0:N], ALU.add, [a, b], [a])
                p.tt("dve", b[:, 0:N], ps[2][:, 0:N], gts[i][:, 2, 0:N], ALU.mult, [ps[2], gts[i]], [b])
                p.tt("pool", mT[:, ct, 0:N], a[:, 0:N], b[:, 0:N], ALU.add, [a, b], [(mT, ct)])
            for ct in range(16):
                i = ct % 2
                p.dma("pool", wo_sb[i][:], W["wout"][ct], [W["wout"]], [wo_sb[i]], f"wo_sb{i}")
                p.dma("sp", xa[i][:, 0:N], XT[:, ct, g0:g0 + N], [XT], [xa[i]], f"xa{i}")
                p.act(xa[i][:, 0:N], xa[i][:, 0:N], AF.Copy, [xa[i]], [xa[i]], scale=float(ALPHA))
                bk = ps[3 + i]
                for kc in range(16):
                    p.mm(bk[:, 0:N], wo_sb[i][:, kc, :], mT[:, kc, 0:N], kc == 0, kc == 15, [wo_sb[i], (mT, kc)], [bk])
                p.stt(tT[:, ct, 0:N], bk[:, 0:N], mod("g1", ct, isctx), xa[i][:, 0:N], ALU.mult, ALU.add, [bk, modv, xa[i]], [(tT, ct)])
            for c in range(NKC):
                p.mm(ps[6][:, 0:N], onesD[:], tT[:, c, 0:N], c == 0, c == NKC - 1, [onesD, (tT, c)], [ps[6]])
            for c in range(NKC):
                s_ = sq32[c % 2]
                p.act(s_[:, 0:N], tT[:, c, 0:N], AF.Square, [(tT, c)], [s_])
                p.mm(ps[7][:, 0:N], onesD[:], s_[:, 0:N], c == 0, c == NKC - 1, [onesD, s_], [ps[7]])
            p.cp("act", mean[:, 0:N], ps[6][:, 0:N], [ps[6]], [mean])
            p.tt("dve", var[:, 0:N], mean[:, 0:N], mean[:, 0:N], ALU.mult, [mean], [var])
            p.tt("dve", var[:, 0:N], ps[7][:, 0:N], var[:, 0:N], ALU.subtract, [ps[7], var], [var])
            p.act(var[:, 0:N], var[:, 0:N], AF.Sqrt, [var, epsc], [var], bias=epsc[:, 1:2], scale=1.0)
            p.recip(var[:, 0:N], var[:, 0:N], [var], [var])
            for c in range(NKC):
                a = u1[c % 2]
                p.tt("pool", a[:, 0:N], tT[:, c, 0:N], mean[:, 0:N], ALU.subtract, [(tT, c), mean], [a])
                p.tt("dve", a[:, 0:N], a[:, 0:N], var[:, 0:N], ALU.mult, [a, var], [a])
                xo = x1o[c % 2]
                p.act(xo[:, 0:N], a[:, 0:N], AF.Identity, [a, lnp], [xo], scale=lnp[:, 0, c:c + 1], bias=lnp[:, 1, c:c + 1])
                p.dma("sp", X1[:, c, g0:g0 + N], xo[:, 0:N], [xo], [X1], f"x1o{c % 2}")
                ho = h2o[c % 2]
                p.ts("dve", ho[:, 0:N], xo[:, 0:N], mod("sc2", c, isctx), mod("sh2", c, isctx), ALU.mult, ALU.add, [xo, modv], [ho])
                p.dma("act", H2[:, c, g0:g0 + N], ho[:, 0:N], [ho], [H2], f"h2o{c % 2}")
        p.reset_to(layer_base)
        if "B" in debug and l == debug["B"]:
            break


        peer_seen = False
        onesD = p.sbuf("onesD", [128, 128], F32)
        p.memset("dve", onesD[:], 1.0 / D, [onesD])
        pk = p.sbuf("pk", [128, 256], F32)
        p.dma("sp", pk[:], W["pk"][:], [W["pk"]], [pk], "c_misc")
        h2 = p.sbuf("h2_sb", [128, NKC, 512], BF16)
        acc = p.sbuf("acc_sb", [128, NKC, 512], F32)
        wqs = [p.sbuf(f"wqs{i}", [128, 16, 128], BF16) for i in range(1)] * 2
        qf = [p.sbuf(f"qf{i}", [128, 512], F32) for i in range(1)] * 2
        aS = p.sbuf("aS", [128, 4, 8, 128], F32)
        s2S = p.sbuf("s2S", [128, 4, 8, 128], F32)
        b2S = p.sbuf("b2S", [128, 4, 8], F32)
        s1t = p.sbuf("s1t", [128, 128], F32)
        wk1 = p.sbuf("wk1", [128, 256], F32)
        wk2 = p.sbuf("wk2", [128, 256], F32)
        v16 = p.sbuf("v16", [128, 2, 16], F32)
        cand = p.sbuf("cand", [128, 16, 16], F32)
        c24 = p.sbuf("c24", [128, 24], F32)
        e16 = p.sbuf("e16", [128, 16], F32)
        sm = p.sbuf("sm", [128, 8], F32)
        NI = 8
        Db = [p.sbuf(f"Db{i}", [128, NI, 128], F32) for i in range(2)]
        Eb = [p.sbuf(f"Eb{i}", [128, NI * 128], F32) for i in range(2)]
        Gacc = [p.sbuf(f"Gacc{i}", [128, NI * 128], F32) for i in range(4)]
        Gbf = [[p.sbuf(f"Gbf{b}_{i}", [128, NI * 128], BF16) for i in range(4)] for b in range(2)]
        ut = [p.sbuf(f"ut{i}", [128, 16, 128], BF16) for i in range(2)]
        vt_ = [p.sbuf(f"vts{i}", [128, NI, 128], BF16) for i in range(2)]
        gl = [p.sbuf(f"gl{i}", [128, 512], BF16) for i in range(2)]
        AT = [p.sbuf(f"AT{b}", [128, NI, 512], BF16) for b in range(2)]
        xa = [Eb[0], Eb[1]]
        tT = acc
        mean = Gacc[0]
        var = Gacc[1]
        u1 = [Gacc[2], Gacc[3]]
        sq32 = [Tk(Db[0][:].rearrange("p a b -> p (a b)"), "Db0v"), Tk(Db[1][:].rearrange("p a b -> p (a b)"), "Db1v")]
        sq32[0].id, sq32[1].id = Db[0].id, Db[1].id
        x1o = [Tk(Gbf[0][0].t.bitcast(F32), "g0v"), Tk(Gbf[0][1].t.bitcast(F32), "g1v")]
        x1o[0].id, x1o[1].id = Gbf[0][0].id, Gbf[0][1].id

        for gi, (g0, N) in enumerate(GROUPS):
            isctx = gi == 0
            if last and isctx:
                continue
            nsub = N // 128
            peer_first = not peer_seen
            peer_seen = True
            for c in range(NKC):
                p.dma("sp" if c % 2 == 0 else "act", h2[:, c, 0:N], H2[:, c, g0:g0 + N], [H2], [(h2, c)], "h2_sb")
            for h in range(8):
                w_ = wqs[h % 2]
                p.dma("pool", w_[:], W["pwq"][h], [W["pwq"]], [w_], f"wqs{h % 2}")
                bk = ps[h % 2]
                for kc in range(NKC):
                    p.mm(bk[:, 0:N], w_[:, kc, :], h2[:, kc, 0:N], kc == 0, kc == NKC - 1, [w_, (h2, kc)], [bk])
                q_ = qf[h % 2]
                p.cp("act", q_[:, 0:N], bk[:, 0:N], [bk], [q_])
                for tt_ in range(nsub):
                    cs = slice(tt_ * 128, (tt_ + 1) * 128)
                    sb_ = ps[2 + tt_ % 2]
                    p.mm(sb_[:, 0:256], q_[:, cs], pk[:, :], True, True, [q_, pk], [sb_])
                    p.cp("act", s1t[:], sb_[:, 0:128], [sb_], [s1t])
                    p.cp("act", s2S[:, tt_, h, :], sb_[:, 128:256], [sb_], [(s2S, (tt_, h))])
                    for w2, src in ((0, s1t[:]), (1, s2S[:, tt_, h, :])):
                        p.max8(v16[:, w2, 0:8], src, [s1t, (s2S, (tt_, h))], [v16])
                        p.mrep(wk1[:, 0:128], v16[:, w2, 0:8], src, [s1t, (s2S, (tt_, h)), v16], [wk1])
                        p.max8(v16[:, w2, 8:16], wk1[:, 0:128], [wk1], [v16])
                    p.tt("dve", cand[:], v16[:, 0, :].unsqueeze(2).to_broadcast([128, 16, 16]),
                         v16[:, 1, :].unsqueeze(1).to_broadcast([128, 16, 16]), ALU.add, [v16], [cand])
                    cf = cand[:].rearrange("p a b -> p (a b)")
                    p.max8(c24[:, 0:8], cf, [cand], [c24])
                    p.mrep(wk1[:], c24[:, 0:8], cf, [cand, c24], [wk1])
                    p.max8(c24[:, 8:16], wk1[:], [wk1], [c24])
                    p.ts("dve", sm[:, 0:1], c24[:, 0:1], -1.0, None, ALU.mult, None, [c24], [sm])
                    p.act(e16[:], c24[:, 0:16], AF.Exp, [c24, sm], [e16, sm], bias=sm[:, 0:1], accum=sm[:, 1:2])
                    p.ts("dve", sm[:, 2:3], c24[:, 15:16], -2e-5, None, ALU.add, None, [c24], [sm])
                    p.act(sm[:, 3:4], sm[:, 1:2], AF.Ln, [sm], [sm])
                    p.tt("dve", sm[:, 4:5], sm[:, 2:3], sm[:, 0:1], ALU.add, [sm], [sm])
                    p.tt("dve", b2S[:, tt_, h:h + 1], sm[:, 4:5], sm[:, 3:4], ALU.subtract, [sm], [(b2S, (tt_, h))])
                    p.ts("dve", aS[:, tt_, h, :], s1t[:], sm[:, 2:3], None, ALU.subtract, None, [s1t, sm], [(aS, (tt_, h))])
            if debug.get("Cstop") == 1:
                p.barrier(final=True)
                p.emit()
                return nc, p
            nchunk = 128 // NI
            for ic in range(nchunk):
                gb = ic % 2
                pend_add = None
                for tt_ in range(nsub):
                    for h in range(8):
                        k = (tt_ * 8 + h) % 2
                        p.tt("pool", Db[k][:], aS[:, tt_, h, ic * NI:(ic + 1) * NI].unsqueeze(2).to_broadcast([128, NI, 128]),
                             s2S[:, tt_, h, :].unsqueeze(1).to_broadcast([128, NI, 128]), ALU.add,
                             [(aS, (tt_, h)), (s2S, (tt_, h))], [Db[k]])
                        df = Db[k][:].rearrange("p a b -> p (a b)")
                        p.stt(df, df, 1e30, df, ALU.mult, ALU.min, [Db[k]], [Db[k]])
                        if h == 0:
                            p.act(Gacc[tt_][:], df, AF.Exp, [Db[k], (b2S, (tt_, h))], [Gacc[tt_]], bias=b2S[:, tt_, h:h + 1])
                            new_add = None
                        else:
                            p.act(Eb[k][:], df, AF.Exp, [Db[k], (b2S, (tt_, h))], [Eb[k]], bias=b2S[:, tt_, h:h + 1])
                            dst = Gbf[gb][tt_] if h == 7 else Gacc[tt_]
                            new_add = (dst, Gacc[tt_], Eb[k])
                        if pend_add is not None:
                            d_, g_a, e_b = pend_add
                            p.tt("dve", d_[:], g_a[:], e_b[:], ALU.add, [g_a, e_b], [d_])
                        pend_add = new_add
                if pend_add is not None:
                    d_, g_a, e_b = pend_add
                    p.tt("dve", d_[:], g_a[:], e_b[:], ALU.add, [g_a, e_b], [d_])
                    pend_add = None
                if debug.get("Cstop") == 2:
                    p.barrier(final=True)
                    p.emit()
                    return nc, p
                for e in range(NI):
                    eg = ic * NI + e
                    u_ = ut[eg % 2]
                    uf = u_[:].rearrange("p a b -> p (a b)")
                    if peer_first:
                        p.dma("pool", u_[:], W["put"][eg], [W["put"]], [u_], f"ut{eg % 2}")
                        p.dma("sp", PUTB[eg], uf, [u_], [(PUTB, eg)], f"ut{eg % 2}")
                    else:
                        p.dma("sp", uf, PUTB[eg], [(PUTB, eg)], [u_], f"ut{eg % 2}")
                    hb = ps[eg % 2]
                    for kc in range(NKC):
                        p.mm(hb[:, 0:N], u_[:, kc, :], h2[:, kc, 0:N], kc == 0, kc == NKC - 1, [u_, (h2, kc)], [hb])
                    g_ = gl[eg % 2]
                    p.act(g_[:, 0:N], hb[:, 0:N], AF.Gelu, [hb], [g_])
                    gtb = ps[2 + eg % 2]
                    gtv = psb(2 + eg % 2)
                    for tt_ in range(nsub):
                        p.tr(gtv[:, tt_ * 128:(tt_ + 1) * 128], Gbf[gb][tt_][:, e * 128:(e + 1) * 128], ident[:],
                             [Gbf[gb][tt_], ident], [gtb])
                    p.tt("dve", AT[gb][:, e, 0:N], g_[:, 0:N], gtv[:, 0:N], ALU.mult, [g_, gtb], [(AT[gb], e)])
                if debug.get("Cstop") == 3:
                    p.barrier(final=True)
                    p.emit()
                    return nc, p
                for dt in range(16):
                    k = (ic * 16 + dt) % 2
                    v_ = vt_[k]
                    vf = v_[:].rearrange("p a b -> p (a b)")
                    if peer_first:
                        p.dma("pool", v_[:], W["pvt"][ic, dt], [W["pvt"]], [v_], f"vts{k}")
                        p.dma("sp", PVTB[ic, dt], vf, [v_], [(PVTB, (ic, dt))], f"vts{k}")
                    else:
                        p.dma("sp", vf, PVTB[ic, dt], [(PVTB, (ic, dt))], [v_], f"vts{k}")
                    ab = ps[4 + dt % 2]
                    for e in range(NI):
                        p.mm(ab[:, 0:N], v_[:, e, :], AT[gb][:, e, 0:N], e == 0, e == NI - 1, [v_, (AT[gb], e)], [ab])
                    if ic == 0:
                        p.cp("act", acc[:, dt, 0:N], ab[:, 0:N], [ab], [(acc, dt)])
                    else:
                        p.tt("dve", acc[:, dt, 0:N], acc[:, dt, 0:N], ab[:, 0:N], ALU.add, [(acc, dt), ab], [(acc, dt)])
            if debug.get("Cstop") == 5:
                p.barrier(final=True)
                p.emit()
                return nc, p
            for ct in range(16):
                i = ct % 2
                p.dma("sp", xa[i][:, 0:N], X1[:, ct, g0:g0 + N], [X1], [xa[i]], f"xa{i}")
                p.act(xa[i][:, 0:N], xa[i][:, 0:N], AF.Copy, [xa[i]], [xa[i]], scale=float(ALPHA))
                p.stt(tT[:, ct, 0:N], acc[:, ct, 0:N], mod("g2", ct, isctx), xa[i][:, 0:N], ALU.mult, ALU.add,
                      [(acc, ct), modv, xa[i]], [(tT, ct)])
            for c in range(NKC):
                p.mm(ps[6][:, 0:N], onesD[:], tT[:, c, 0:N], c == 0, c == NKC - 1, [onesD, (tT, c)], [ps[6]])
            for c in range(NKC):
                s = sq32[c % 2]
                p.act(s[:, 0:N], tT[:, c, 0:N], AF.Square, [(tT, c)], [s])
                p.mm(ps[7][:, 0:N], onesD[:], s[:, 0:N], c == 0, c == NKC - 1, [onesD, s], [ps[7]])
            p.cp("act", mean[:, 0:N], ps[6][:, 0:N], [ps[6]], [mean])
            p.tt("dve", var[:, 0:N], mean[:, 0:N], mean[:, 0:N], ALU.mult, [mean], [var])
            p.tt("dve", var[:, 0:N], ps[7][:, 0:N], var[:, 0:N], ALU.subtract, [ps[7], var], [var])
            p.act(var[:, 0:N], var[:, 0:N], AF.Sqrt, [var, epsc], [var], bias=epsc[:, 1:2], scale=1.0)
            p.recip(var[:, 0:N], var[:, 0:N], [var], [var])
            for c in range(NKC):
                a = u1[c % 2]
                p.tt("pool", a[:, 0:N], tT[:, c, 0:N], mean[:, 0:N], ALU.subtract, [(tT, c), mean], [a])
                p.tt("dve", a[:, 0:N], a[:, 0:N], var[:, 0:N], ALU.mult, [a, var], [a])
                xo = x1o[c % 2]
                p.act(xo[:, 0:N], a[:, 0:N], AF.Identity, [a, lnp], [xo], scale=lnp[:, 2, c:c + 1], bias=lnp[:, 3, c:c + 1])
                if last:
                    p.dma("sp", out[:, c, g0 - CTX:g0 - CTX + N], xo[:, 0:N], [xo], [out], f"x1o{c % 2}")
                else:
                    p.dma("sp", XO[:, c, g0:g0 + N], xo[:, 0:N], [xo], [XO], f"x1o{c % 2}")
    p.barrier(final=True)
    p.emit()
    return nc, p


def kernel(**inputs):
    inp = {k: np.asarray(v) for k, v in inputs.items()}
    consts = host_consts()
    nc, prog = build_program()
    wl = [prep_layer_weights(inp, l) for l in range(DEPTH)]
    in_maps = []
    for core in range(8):
        b = core // 2
        xfull = np.concatenate([inp["ctx"][b], inp["x"][b]], axis=0)
        xT = np.ascontiguousarray(xfull.reshape(T, NKC, 128).transpose(2, 1, 0))
        cc = np.stack([fm(inp["c"][b]), fm(inp["c_ctx"])], axis=-1)
        m = {"xT": xT, "cc": np.ascontiguousarray(cc), "ropetab": consts["ropetab"], "permm": consts["permm"],
             "misc": consts["misc"], "colc": consts["colc"], "hsel": consts["hsel"]}
        for l in range(DEPTH):
            for k in WKEYS:
                m[f"{k}{l}"] = wl[l][k]
        in_maps.append(m)
    res = run_bass_kernel_spmd(nc, in_maps, core_ids=list(range(8)))
    outp = np.empty((4, SEQ, D), np.float32)
    for core in range(8):
        b, half = core // 2, core % 2
        o = res.results[core]["out"]
        sl = slice(half * 2048, (half + 1) * 2048)
        outp[b, sl] = o[:, :, sl].transpose(2, 1, 0).reshape(2048, D)
    return outp
```
